# Optimizing a Trainium2 kernel written in Bass

```python
import math
import jax, jax.numpy as jnp
from jax import lax
import numpy as np

D_MODEL = 1024
BATCH = 4
SEQ = 8192
DEPTH = 2

BLOCK = 128
BRANCH_W = D_MODEL // 2
N_BRANCH = 4
RET_HEADS = 4
RET_DK = BRANCH_W // RET_HEADS
RET_DV = BRANCH_W // RET_HEADS
ROPE_THETA = 10000.0
LRU_HEADS = 8
LRU_HD = BRANCH_W // LRU_HEADS
CONV_W = 4
LRU_C = 8.0
SB_HEADS = 4
SB_HD = BRANCH_W // SB_HEADS
SG_GROUPS = 4
SG_GD = BRANCH_W // SG_GROUPS
D_FF = 4 * D_MODEL
ALPHA = (2 * DEPTH) ** 0.25
BETA = (8 * DEPTH) ** -0.25
LN_EPS = 1e-5

SPLIT_SIZES = (BRANCH_W,) * 9 + (2 * BRANCH_W, N_BRANCH * D_MODEL)
D_IN = int(sum(SPLIT_SIZES))
SPLIT_POINTS = tuple(int(s) for s in np.cumsum(SPLIT_SIZES)[:-1])

kernel_name = "hybrid_retention_rglru_stickbreak_gmlp"


def layer_norm(x, g, b):
    xf = x.astype(jnp.float32)
    mu = jnp.mean(xf, axis=-1, keepdims=True)
    var = jnp.mean(jnp.square(xf - mu), axis=-1, keepdims=True)
    y = (xf - mu) * lax.rsqrt(var + LN_EPS)
    return (y * g.astype(jnp.float32) + b.astype(jnp.float32)).astype(x.dtype)


def rotary(x, pos):
    half = x.shape[-1] // 2
    inv_freq = ROPE_THETA ** (-jnp.arange(half, dtype=jnp.float32) / half)
    ang = pos[:, None] * inv_freq[None, :]
    cos = jnp.cos(ang)[None, :, None, :]
    sin = jnp.sin(ang)[None, :, None, :]
    x1, x2 = x[..., :half], x[..., half:]
    return jnp.concatenate([x1 * cos - x2 * sin, x2 * cos + x1 * sin], axis=-1)


def retention(q, k, v, g):
    dtype = q.dtype
    B, S, _ = q.shape
    N = S // BLOCK
    pos = jnp.arange(S, dtype=jnp.float32)
    qh = rotary(q.astype(jnp.float32).reshape(B, S, RET_HEADS, RET_DK), pos)
    kh = rotary(k.astype(jnp.float32).reshape(B, S, RET_HEADS, RET_DK), pos) * (RET_DK ** -0.5)
    vh = v.astype(jnp.float32).reshape(B, S, RET_HEADS, RET_DV)

    def chunk(t):
        return t.reshape(B, N, BLOCK, RET_HEADS, -1).transpose(0, 3, 1, 2, 4)

    qc, kc, vc = chunk(qh), chunk(kh), chunk(vh)
    log_g = jnp.log1p(-(2.0 ** (-5.0 - jnp.arange(RET_HEADS, dtype=jnp.float32))))
    idx = jnp.arange(BLOCK, dtype=jnp.float32)
    diff = idx[:, None] - idx[None, :]
    decay = jnp.where(diff >= 0, jnp.exp(log_g[:, None, None] * jnp.maximum(diff, 0.0)), 0.0)
    scores = jnp.einsum('bhncd,bhnmd->bhncm', qc, kc) * decay[None, :, None]
    inner = jnp.einsum('bhncm,bhnme->bhnce', scores, vc)
    k_dec = jnp.exp(log_g[:, None] * (BLOCK - 1.0 - idx)[None, :])
    kv = jnp.einsum('bhnmd,bhnme->nbhde', kc * k_dec[None, :, None, :, None], vc)
    chunk_decay = jnp.exp(log_g * BLOCK)[None, :, None, None]

    def step(state, kv_n):
        return chunk_decay * state + kv_n, state

    _, prev = lax.scan(step, jnp.zeros((B, RET_HEADS, RET_DK, RET_DV), jnp.float32), kv)
    q_dec = jnp.exp(log_g[:, None] * (idx + 1.0)[None, :])
    cross = jnp.einsum('bhncd,nbhde->bhnce', qc * q_dec[None, :, None, :, None], prev)
    y = (inner + cross).transpose(0, 2, 3, 1, 4).reshape(B, S, RET_HEADS, RET_DV)
    mu = jnp.mean(y, axis=-1, keepdims=True)
    var = jnp.mean(jnp.square(y - mu), axis=-1, keepdims=True)
    y = ((y - mu) * lax.rsqrt(var + LN_EPS)).reshape(B, S, BRANCH_W)
    return (jax.nn.silu(g.astype(jnp.float32)) * y).astype(dtype)


def rg_lru_branch(xb, gate_in, conv_w, conv_b, w_a, b_a, w_x, b_x, lam):
    dtype = xb.dtype
    B, S, W = xb.shape
    x = xb.astype(jnp.float32)
    rhs = conv_w.astype(jnp.float32).reshape(CONV_W, 1, W)
    xc = lax.conv_general_dilated(x, rhs, window_strides=(1,), padding=[(CONV_W - 1, 0)],
                                  dimension_numbers=('NWC', 'WIO', 'NWC'),
                                  feature_group_count=W) + conv_b.astype(jnp.float32)
    xh = xc.reshape(B, S, LRU_HEADS, LRU_HD)
    r = jax.nn.sigmoid(jnp.einsum('bshi,hij->bshj', xh, w_a.astype(jnp.float32)).reshape(B, S, W)
                       + b_a.astype(jnp.float32))
    i = jax.nn.sigmoid(jnp.einsum('bshi,hij->bshj', xh, w_x.astype(jnp.float32)).reshape(B, S, W)
                       + b_x.astype(jnp.float32))
    log_a = -LRU_C * r * jax.nn.softplus(-lam.astype(jnp.float32))
    a = jnp.exp(log_a)
    u = jnp.sqrt(-jnp.expm1(2.0 * log_a)) * (i * xc)

    def combine(c1, c2):
        a1, b1 = c1
        a2, b2 = c2
        return a1 * a2, a2 * b1 + b2

    _, h = lax.associative_scan(combine, (a, u), axis=1)
    return (h * jax.nn.gelu(gate_in.astype(jnp.float32))).astype(dtype)


def stick_breaking(q, k, v):
    dtype = q.dtype
    B, S, _ = q.shape
    N = S // BLOCK

    def heads(t):
        return t.astype(jnp.float32).reshape(B, S, SB_HEADS, SB_HD).transpose(0, 2, 1, 3)

    qh = heads(q) * (SB_HD ** -0.5)
    kh, vh = heads(k), heads(v)
    qb = qh.reshape(B, SB_HEADS, N, BLOCK, SB_HD).transpose(2, 0, 1, 3, 4)
    key_pos = jnp.arange(S)

    def one_block(args):
        q_blk, n = args
        z = jnp.einsum('bhid,bhjd->bhij', q_blk, kh)
        q_pos = n * BLOCK + jnp.arange(BLOCK)
        mask = key_pos[None, :] < q_pos[:, None]
        log_1m = jnp.where(mask, jax.nn.log_sigmoid(-z), 0.0)
        after = lax.cumsum(log_1m, axis=3, reverse=True) - log_1m
        w = jnp.where(mask, jnp.exp(jax.nn.log_sigmoid(z) + after), 0.0)
        return jnp.einsum('bhij,bhjd->bhid', w, vh)

    out = lax.map(one_block, (qb, jnp.arange(N)))
    return out.transpose(1, 0, 3, 2, 4).reshape(B, S, BRANCH_W).astype(dtype)


def spatial_gating(uv, ln_g, ln_b, w_s, b_s):
    dtype = uv.dtype
    B, S, _ = uv.shape
    N = S // BLOCK
    zz = jax.nn.gelu(uv.astype(jnp.float32))
    u, v = zz[..., :BRANCH_W], zz[..., BRANCH_W:]
    v = layer_norm(v, ln_g, ln_b).reshape(B, N, BLOCK, SG_GROUPS, SG_GD)
    tri = jnp.tril(jnp.ones((BLOCK, BLOCK), jnp.float32))
    w = w_s.astype(jnp.float32) * tri[None]
    s = jnp.einsum('gij,bnjgc->bnigc', w, v) + b_s.astype(jnp.float32).T[None, None, :, :, None]
    return (u * s.reshape(B, S, BRANCH_W)).astype(dtype)


def hybrid_layer(x, w_in, b_in, conv_w, conv_b, lru_wa, lru_ba, lru_wx, lru_bx, lru_lambda,
                 sg_ln_g, sg_ln_b, sg_ws, sg_bs, w_branch, w_out, ln1_g, ln1_b,
                 w1, b1, w2, b2, ln2_g, ln2_b):
    B, S, D = x.shape
    z = x @ w_in + b_in
    (rq, rk, rv, rg, lx, lg, sq, sk, sv, suv, gl) = jnp.split(z, SPLIT_POINTS, axis=-1)
    y_a = retention(rq, rk, rv, rg)
    y_b = rg_lru_branch(lx, lg, conv_w, conv_b, lru_wa, lru_ba, lru_wx, lru_bx, lru_lambda)
    y_c = stick_breaking(sq, sk, sv)
    y_d = spatial_gating(suv, sg_ln_g, sg_ln_b, sg_ws, sg_bs)
    ys = jnp.stack([y_a, y_b, y_c, y_d], axis=2)
    proj = jnp.einsum('bsnw,nwd->bsnd', ys, w_branch)
    gates = jax.nn.sigmoid(gl.astype(jnp.float32)).reshape(B, S, N_BRANCH, D)
    merged = jnp.sum(gates * proj.astype(jnp.float32), axis=2).astype(x.dtype)
    x = layer_norm(ALPHA * x + merged @ w_out, ln1_g, ln1_b)
    hid = jnp.square(jax.nn.relu(x @ w1 + b1))
    x = layer_norm(ALPHA * x + (hid @ w2 + b2), ln2_g, ln2_b)
    return x


def setup_inputs(seed: int = 0) -> dict:
    key = jax.random.key(seed)
    ks = jax.random.split(key, 24)
    L = DEPTH
    f32 = jnp.float32

    def nrm(k, shape, scale):
        return jax.random.normal(k, shape, f32) * scale

    u = jax.random.uniform(ks[9], (L, BRANCH_W), f32, 0.9, 0.999)
    s = u ** (1.0 / LRU_C)
    return {
        "x": nrm(ks[0], (BATCH, SEQ, D_MODEL), 1.0),
        "w_in": nrm(ks[1], (L, D_MODEL, D_IN), D_MODEL ** -0.5),
        "b_in": nrm(ks[2], (L, D_IN), 0.01),
        "conv_w": nrm(ks[3], (L, CONV_W, BRANCH_W), CONV_W ** -0.5),
        "conv_b": nrm(ks[4], (L, BRANCH_W), 0.01),
        "lru_wa": nrm(ks[5], (L, LRU_HEADS, LRU_HD, LRU_HD), LRU_HD ** -0.5),
        "lru_ba": nrm(ks[6], (L, BRANCH_W), 0.01),
        "lru_wx": nrm(ks[7], (L, LRU_HEADS, LRU_HD, LRU_HD), LRU_HD ** -0.5),
        "lru_bx": nrm(ks[8], (L, BRANCH_W), 0.01),
        "lru_lambda": jnp.log(s) - jnp.log1p(-s),
        "sg_ln_g": 1.0 + nrm(ks[10], (L, BRANCH_W), 0.01),
        "sg_ln_b": nrm(ks[11], (L, BRANCH_W), 0.01),
        "sg_ws": nrm(ks[12], (L, SG_GROUPS, BLOCK, BLOCK), BLOCK ** -0.5),
        "sg_bs": 1.0 + nrm(ks[13], (L, SG_GROUPS, BLOCK), 0.01),
        "w_branch": nrm(ks[14], (L, N_BRANCH, BRANCH_W, D_MODEL), BRANCH_W ** -0.5),
        "w_out": nrm(ks[15], (L, D_MODEL, D_MODEL), D_MODEL ** -0.5 * BETA),
        "ln1_g": 1.0 + nrm(ks[16], (L, D_MODEL), 0.01),
        "ln1_b": nrm(ks[17], (L, D_MODEL), 0.01),
        "w1": nrm(ks[18], (L, D_MODEL, D_FF), D_MODEL ** -0.5),
        "b1": nrm(ks[19], (L, D_FF), 0.01),
        "w2": nrm(ks[20], (L, D_FF, D_MODEL), D_FF ** -0.5 * BETA),
        "b2": nrm(ks[21], (L, D_MODEL), 0.01),
        "ln2_g": 1.0 + nrm(ks[22], (L, D_MODEL), 0.01),
        "ln2_b": nrm(ks[23], (L, D_MODEL), 0.01),
    }


def reference(x, w_in, b_in, conv_w, conv_b, lru_wa, lru_ba, lru_wx, lru_bx, lru_lambda,
              sg_ln_g, sg_ln_b, sg_ws, sg_bs, w_branch, w_out, ln1_g, ln1_b,
              w1, b1, w2, b2, ln2_g, ln2_b):
    for l in range(DEPTH):
        x = hybrid_layer(x, w_in[l], b_in[l], conv_w[l], conv_b[l], lru_wa[l], lru_ba[l],
                         lru_wx[l], lru_bx[l], lru_lambda[l], sg_ln_g[l], sg_ln_b[l],
                         sg_ws[l], sg_bs[l], w_branch[l], w_out[l], ln1_g[l], ln1_b[l],
                         w1[l], b1[l], w2[l], b2[l], ln2_g[l], ln2_b[l])
    return x
```

```python
import math
from contextlib import ExitStack

import numpy as np
import concourse.bass as bass
import concourse.mybir as mybir
from concourse.bass_utils import run_bass_kernel_spmd

F32 = mybir.dt.float32
BF16 = mybir.dt.bfloat16
AF = mybir.ActivationFunctionType
ALU = mybir.AluOpType

D_MODEL = 1024
BW = 512
DFF = 4096
DEPTH = 2
ALPHA = (2 * DEPTH) ** 0.25
LN_EPS = 1e-5
T = 512
NUNIT = 43
U_TM, U_FM, U_GT, U_BR, U_WO, U_W1, U_W2 = 0, 5, 13, 21, 25, 27, 35
NSM = 132
SM_BC, SM_CW, SM_CB, SM_BA, SM_BX, SM_LAM, SM_BS, SM_B1 = 0, 64, 80, 84, 88, 92, 96, 100
NCONST = 4096
C_ID, C_TRI, C_NONE, C_NEGM, C_DT, C_QD, C_KDT, C_TRIL = 0, 128, 256, 384, 2432, 2944, 3456, 3968
NEG = -30000.0


class View:
    __slots__ = ("ap", "key", "lo", "hi")

    def __init__(self, ap, key, lo, hi):
        self.ap, self.key, self.lo, self.hi = ap, key, lo, hi


class Buf:
    def __init__(self, ap, key, shape, part=True, base=0, esz=1, whole=False):
        self.whole = whole
        self.ap = ap
        self.key = key
        self.shape = list(shape)
        self.part = part
        self.base = base
        self.esz = esz
        free = self.shape[1:] if part else self.shape
        st = []
        s = 1
        for d in reversed(free):
            st.append(s)
            s *= d
        self.strides = list(reversed(st))

    def __getitem__(self, idx):
        if not isinstance(idx, tuple):
            idx = (idx,)
        idx = list(idx) + [slice(None)] * (len(self.shape) - len(idx))
        ap = self.ap[tuple(idx)]
        fi = idx[1:] if self.part else idx
        fs = self.shape[1:] if self.part else self.shape
        lo = 0
        hi = 0
        for ix, st, d in zip(fi, self.strides, fs):
            if isinstance(ix, int):
                a, b = ix, ix + 1
            else:
                a = 0 if ix.start is None else ix.start
                b = d if ix.stop is None else ix.stop
            lo += a * st
            hi += (b - 1) * st
        if self.whole:
            return View(ap, self.key, 0, 1)
        return View(ap, self.key, self.base + lo * self.esz, self.base + (hi + 1) * self.esz)

    def v(self):
        return self[tuple(slice(None) for _ in self.shape)]


class _Op:
    __slots__ = ("eng", "idx", "fn", "sig", "dma", "deps", "seq", "dman")


DMA_RING = 8
ENGS = ("pe", "act", "dve", "pool", "sp")


class Sched:
    def __init__(self):
        self.ops = {e: [] for e in ENGS}
        self.reg = {}
        self.seq = 0
        self.ndma = {e: 0 for e in ENGS}

    def op(self, eng, fn, reads=(), writes=(), sig=True, dma=False):
        o = _Op()
        o.eng, o.fn, o.sig, o.dma = eng, fn, sig, dma
        o.idx = len(self.ops[eng])
        o.seq = self.seq
        self.seq += 1
        o.dman = -1
        deps = {}
        if dma:
            o.dman = self.ndma[eng]
            self.ndma[eng] += 1
            o.sig = True
        for v in reads:
            self._read(v, o, deps)
        for v in writes:
            self._write(v, o, deps)
        out = []
        for p, raw in deps.items():
            if p is o:
                continue
            if p.eng == eng and not p.dma and not dma:
                if eng == "pe":
                    continue
            out.append(p)
        o.deps = out
        self.ops[eng].append(o)
        return o

    def _read(self, v, o, deps):
        recs = self.reg.setdefault(v.key, {})
        for (lo, hi), r in recs.items():
            if lo < v.hi and v.lo < hi and r[0] is not None:
                deps[r[0]] = True
        r = recs.get((v.lo, v.hi))
        if r is None:
            r = [None, {}, []]
            recs[(v.lo, v.hi)] = r
        if o.dma:
            r[2].append(o)
        else:
            r[1][o.eng] = o

    def _write(self, v, o, deps):
        recs = self.reg.setdefault(v.key, {})
        dead = []
        for (lo, hi), r in recs.items():
            if lo < v.hi and v.lo < hi:
                if r[0] is not None:
                    deps.setdefault(r[0], False)
                for p in r[1].values():
                    deps.setdefault(p, False)
                for p in r[2]:
                    deps.setdefault(p, False)
                if v.lo <= lo and hi <= v.hi:
                    dead.append((lo, hi))
        for k in dead:
            del recs[k]
        recs[(v.lo, v.hi)] = [o, {}, []]

    def emit(self, block_fns, csem, dsem):
        allops = sorted((o for e in ENGS for o in self.ops[e]), key=lambda o: o.seq)
        for o in allops:
            for p in o.deps:
                if p.dma or p.sig:
                    continue
                lst = self.ops[p.eng]
                k = p.idx
                while k < len(lst) and (lst[k].dma or not lst[k].sig):
                    k += 1
                if k >= len(lst) or lst[k].seq >= o.seq:
                    p.sig = True
        sigcount = {}
        nextsig = {}
        for e in ENGS:
            ops = self.ops[e]
            comp = [o for o in ops if not o.dma]
            if comp and not comp[-1].sig:
                comp[-1].sig = True
            c = 0
            sc = {}
            for o in ops:
                if not o.dma and o.sig:
                    c += 1
                sc[o.idx] = c
            sigcount[e] = sc
            ns = {}
            nxt = None
            for o in reversed(ops):
                if not o.dma and o.sig:
                    nxt = o
                ns[o.idx] = nxt
            nextsig[e] = ns

        def event(p, consumer):
            if p.dma:
                n = p.dman
                return dsem[p.eng][n % DMA_RING], 16 * (n // DMA_RING + 1)
            j = nextsig[p.eng][p.idx]
            assert j is not None
            assert j is p or j.seq < consumer.seq, "signal recorded after its consumer"
            return csem[p.eng], sigcount[p.eng][j.idx]

        def build(e):
            def body(h):
                waited = {}
                for o in self.ops[e]:
                    need = {}
                    for p in o.deps:
                        s, val = event(p, o)
                        k = id(s)
                        if need.get(k, (None, 0))[1] < val:
                            need[k] = (s, val)
                    if o.dma and o.dman >= DMA_RING:
                        n = o.dman - DMA_RING
                        s = dsem[e][n % DMA_RING]
                        val = 16 * (n // DMA_RING + 1)
                        k = id(s)
                        if need.get(k, (None, 0))[1] < val:
                            need[k] = (s, val)
                    for k, (s, val) in need.items():
                        if waited.get(k, 0) < val:
                            h.wait_ge(s, val)
                            waited[k] = val
                    inst = o.fn(h)
                    if o.dma:
                        inst.then_inc(dsem[e][o.dman % DMA_RING], 16)
                    elif o.sig:
                        inst.then_inc(csem[e], 1)
                nd = self.ndma[e]
                for n in range(max(0, nd - DMA_RING), nd):
                    s = dsem[e][n % DMA_RING]
                    val = 16 * (n // DMA_RING + 1)
                    if waited.get(id(s), 0) < val:
                        h.wait_ge(s, val)
                        waited[id(s)] = val
            return body

        for e in ENGS:
            if self.ops[e]:
                block_fns[e](build(e))


ARENA_BYTES = 192 * 1024


def build_program(S_LEN, NL):
    NT = S_LEN // T
    NBT = S_LEN // 128
    nc = bass.Bass("TRN2", target_bir_lowering=False)
    x_in = nc.dram_tensor("x", [S_LEN, D_MODEL], F32, kind="ExternalInput").ap()
    NWROWS = NL * NUNIT * 256
    wpack = nc.dram_tensor("wpack", [NWROWS, 2048], F32, kind="ExternalInput").ap()
    smalls_in = nc.dram_tensor("smalls", [NL, 128, NSM], F32, kind="ExternalInput").ap()
    rowtab_in = nc.dram_tensor("rowtab", [NL, 128, 5120], F32, kind="ExternalInput").ap()
    wabd_in = nc.dram_tensor("wabd", [NL, 128, 1024], F32, kind="ExternalInput").ap()
    sgws_in = nc.dram_tensor("sgws", [NL, 128, 512], F32, kind="ExternalInput").ap()
    consts_in = nc.dram_tensor("consts", [128, NCONST], F32, kind="ExternalInput").ap()
    rot_in = nc.dram_tensor("rot", [4, 128, S_LEN], F32, kind="ExternalInput").ap()
    brow_in = nc.dram_tensor("brow", [NL, 1, 3584], F32, kind="ExternalInput").ap()
    y_out = nc.dram_tensor("y", [S_LEN, D_MODEL], F32, kind="ExternalOutput").ap()
    ws_d = nc.dram_tensor("ws", [NWROWS, 2048], BF16, kind="Internal").ap()
    kh_d = nc.dram_tensor("kh", [NL * 4 * NT, 128, 512], BF16, kind="Internal").ap()
    vh_d = nc.dram_tensor("vh", [NL * NBT, 128, 512], BF16, kind="Internal").ap()
    browd_d = nc.dram_tensor("browd", [NL, 1, 3584], BF16, kind="Internal").ap()

    S = Sched()
    es = ExitStack()
    with es:
        art = es.enter_context(nc.sbuf_tensor("arena", [128, ARENA_BYTES // 2], BF16))
        banks = []
        for i in range(6):
            pt_ = es.enter_context(nc.psum_tensor(f"pb{i}", [128, 512], F32))
            banks.append(Buf(pt_, f"pb{i}", [128, 512], whole=True))
        PT = []
        for i in range(2):
            ptt_ = es.enter_context(nc.psum_tensor(f"ptr{i}", [128, 1024], BF16))
            PT.append(Buf(ptt_, f"ptr{i}", [128, 1024], whole=True))
        csem = {e: es.enter_context(nc.semaphore("c_" + e)) for e in ENGS}
        dsem = {e: [es.enter_context(nc.semaphore(f"d_{e}{i}")) for i in range(DMA_RING)] for e in ENGS}
        block = es.enter_context(nc.Block())

        WS = Buf(ws_d, "ws", [NWROWS, 2048], part=False)
        KH = Buf(kh_d, "kh", [NL * 4 * NT, 128, 512], part=False)
        VH = Buf(vh_d, "vh", [NL * NBT, 128, 512], part=False)
        BROWD = Buf(browd_d, "browd", [NL, 1, 3584], part=False)

        def abuf(off, shape, dt, parts=128):
            esz = 4 if dt == F32 else 2
            n = 1
            for d in shape:
                n *= d
            assert off % 4 == 0 and off + n * esz <= ARENA_BYTES, (off, shape)
            ap = art[0:parts, off // 2: off // 2 + n * esz // 2]
            if dt == F32:
                ap = ap.bitcast(F32)
            if len(shape) > 1:
                names = "abcdefg"[:len(shape)]
                pat = "p (" + " ".join(names) + ") -> p " + " ".join(names)
                ap = ap.rearrange(pat, **{n_: d_ for n_, d_ in zip(names[:-1], shape[:-1])})
            return Buf(ap, "AR", [parts] + list(shape), base=off, esz=esz)

        cur = [0]

        def palloc(shape, dt, parts=128):
            esz = 4 if dt == F32 else 2
            n = 1
            for d in shape:
                n *= d
            b = abuf(cur[0], shape, dt, parts)
            cur[0] += (n * esz + 31) // 32 * 32
            return b

        XA = palloc([4, 1024], F32)
        XB = palloc([4, 1024], F32)
        WR = [palloc([4096], BF16) for _ in range(3)]
        identb = palloc([128], BF16)
        trineg = palloc([128], BF16)
        negones = palloc([128], BF16)
        negm = palloc([4, 512], BF16)
        DTt = palloc([512], F32)
        QDt = palloc([4, 128], F32)
        KDTt = palloc([512], F32)
        tril = palloc([128], F32)
        smalls = palloc([NL, NSM], F32)
        lrc = palloc([NL, 4, 4], F32)
        wabd = palloc([NL, 2, 4, 128], BF16)
        wsT = palloc([NL, 4, 128], BF16)
        ones_row = palloc([128], BF16, parts=1)
        st = palloc([NL, 4, 128], F32)
        stbf = palloc([NL, 4, 128], BF16)
        hstate = palloc([NL, 4], F32)
        lxtail = palloc([NL, 4, 3], F32)
        SCR = cur[0]

        def sbuf(off, shape, dt, parts=128):
            return abuf(SCR + off, shape, dt, parts)

        xT = sbuf(0, [8, 512], BF16)
        xb = sbuf(8192, [4, 1024], BF16)
        ybT = [sbuf(16384 + 4096 * b, [4, 512], BF16) for b in range(4)]
        su = sbuf(32768, [4, 512], F32)
        sgv = sbuf(40960, [4, 512], F32)
        sq = sbuf(49152, [4, 512], BF16)
        lx_ext = sbuf(53248, [4, 516], F32)
        gg = sbuf(61504, [4, 512], F32)
        rv = sbuf(69696, [4, 512], BF16)
        rgs = sbuf(73792, [4, 512], F32)
        rq = sbuf(81984, [4, 512], BF16)
        rk = sbuf(86080, [4, 512], BF16)
        LOC_D = 90176
        rot_t = [sbuf(8192 + 2048 * i, [512], F32) for i in range(4)]
        rtmp = [sbuf(16384 + 2048 * i, [512], F32) for i in range(4)]
        sv = sbuf(24576, [4, 512], BF16)
        sk = sbuf(28672, [4, 512], BF16)
        browb = sbuf(8192, [3584], BF16, parts=1)
        qd = sbuf(LOC_D, [4, 512], BF16)
        rkt = sbuf(LOC_D + 4096, [4, 512], BF16)
        vdec = sbuf(LOC_D + 8192, [4, 512], BF16)
        sd_ = [sbuf(LOC_D + 12288 + 1024 * i, [4, 128], BF16) for i in range(2)]
        yn = sbuf(LOC_D + 14336, [512], F32)
        ya = sbuf(LOC_D + 16384, [512], BF16)
        stats = sbuf(LOC_D + 17408, [4, 6], F32)
        mv = sbuf(LOC_D + 17408 + 96, [4, 2], F32)
        rs4 = sbuf(LOC_D + 17408 + 128, [4], F32)
        END_D = LOC_D + 17408 + 160
        LOC_E = 69696
        e_xc = sbuf(LOC_E, [512], F32)
        e_xcb = sbuf(LOC_E + 2048, [512], BF16)
        e_tr = sbuf(LOC_E + 3072, [512], F32)
        e_ti = sbuf(LOC_E + 5120, [512], F32)
        e_a = sbuf(LOC_E + 7168, [512], F32)
        e_s = sbuf(LOC_E + 9216, [512], F32)
        e_u = sbuf(LOC_E + 11264, [512], F32)
        e_h = [sbuf(LOC_E + 13312 + 2048 * i, [512], F32) for i in range(2)]
        LOC_F = 53248
        f_k = [sbuf(LOC_F + 4096 * i, [2048], BF16) for i in range(2)]
        f_v = [sbuf(LOC_F + 8192 + 4096 * i, [16, 128], BF16) for i in range(2)]
        f_e = [sbuf(LOC_F + 16384 + 2048 * i, [512], F32) for i in range(2)]
        f_lp = [sbuf(LOC_F + 20480 + 1024 * i, [512], BF16) for i in range(2)]
        f_w = [sbuf(LOC_F + 22528 + 1024 * i, [512], BF16) for i in range(2)]
        f_r = sbuf(LOC_F + 24576, [512], F32)
        f_rb = [sbuf(LOC_F + 26624 + 1024 * i, [512], BF16) for i in range(3)]
        LOC_G = 53248
        g_tab = sbuf(LOC_G, [2, 512], F32)
        g_vn = sbuf(LOC_G + 4096, [512], F32)
        g_vl = sbuf(LOC_G + 6144, [512], BF16)
        g_yd = sbuf(LOC_G + 7168, [512], BF16)
        g_st = sbuf(LOC_G + 8192, [6], F32)
        g_mv = sbuf(LOC_G + 8192 + 32, [2], F32)
        g_rs = sbuf(LOC_G + 8192 + 64, [1], F32)
        mergedT = sbuf(32768, [8, 512], BF16)
        h_g = [sbuf(40960 + 2048 * i, [512], F32) for i in range(2)]
        h_acc = sbuf(40960 + 4096, [512], F32)
        h_tmp = [sbuf(40960 + 6144 + 2048 * i, [512], F32) for i in range(2)]
        LOC_I = 53248
        i_tab = sbuf(LOC_I, [2, 1024], F32)
        i_st = sbuf(LOC_I + 8192, [4, 2, 6], F32)
        i_mv = sbuf(LOC_I + 8192 + 192, [4, 2], F32)
        i_rs = sbuf(LOC_I + 8192 + 224, [4], F32)
        hT = sbuf(16384, [32, 512], BF16)
        j_r = [sbuf(49152 + 2048 * i, [512], F32) for i in range(2)]
        assert END_D <= ARENA_BYTES - SCR, (END_D, ARENA_BYTES - SCR)

        def vw(x):
            return x.v() if isinstance(x, Buf) else x

        def mm(out, lhsT, rhs, start, stop, sig=None):
            S.op("pe", lambda h: h.matmul(out.ap, lhsT=lhsT.ap, rhs=rhs.ap, start=start, stop=stop),
                 reads=[lhsT, rhs], writes=[out], sig=(stop if sig is None else sig))

        def tr(out, in_):
            idv = identb.v()
            S.op("pe", lambda h: h.transpose(out=out.ap, in_=in_.ap, identity=idv.ap), reads=[in_, idv], writes=[out])

        def act(out, in_, func, bias=None, scale=None):
            reads = [in_]
            kw = {}
            if bias is not None:
                if isinstance(bias, View):
                    reads.append(bias)
                    kw["bias"] = bias.ap
                else:
                    kw["bias"] = float(bias)
            if scale is not None:
                if isinstance(scale, View):
                    reads.append(scale)
                    kw["scale"] = scale.ap
                else:
                    kw["scale"] = float(scale)
            S.op("act", lambda h: h.activation(out=out.ap, in_=in_.ap, func=func, **kw), reads=reads, writes=[out])

        def tt(eng, out, a, b, op):
            S.op(eng, lambda h: h.tensor_tensor(out=out.ap, in0=a.ap, in1=b.ap, op=op), reads=[a, b], writes=[out])

        def ts(eng, out, a, s1, s2, op0, op1=None):
            reads = [a]
            k1 = s1.ap if isinstance(s1, View) else float(s1)
            if isinstance(s1, View):
                reads.append(s1)
            if op1 is None:
                S.op(eng, lambda h: h.tensor_scalar(out=out.ap, in0=a.ap, scalar1=k1, scalar2=None, op0=op0),
                     reads=reads, writes=[out])
                return
            k2 = s2.ap if isinstance(s2, View) else float(s2)
            if isinstance(s2, View):
                reads.append(s2)
            S.op(eng, lambda h: h.tensor_scalar(out=out.ap, in0=a.ap, scalar1=k1, scalar2=k2, op0=op0, op1=op1),
                 reads=reads, writes=[out])

        def stt(eng, out, in0, sc, in1, op0, op1):
            reads = [in0, in1]
            k = sc.ap if isinstance(sc, View) else float(sc)
            if isinstance(sc, View):
                reads.append(sc)
            S.op(eng, lambda h: h.scalar_tensor_tensor(out=out.ap, in0=in0.ap, scalar=k, in1=in1.ap, op0=op0, op1=op1),
                 reads=reads, writes=[out])

        def cp(eng, out, in_):
            if eng == "act":
                S.op("act", lambda h: h.copy(out=out.ap, in_=in_.ap), reads=[in_], writes=[out])
            else:
                S.op(eng, lambda h: h.tensor_copy(out=out.ap, in_=in_.ap), reads=[in_], writes=[out])

        def mset(eng, out, val):
            S.op(eng, lambda h: h.memset(out.ap, val), writes=[out])

        def dma(eng, out_ap, in_ap, reads=(), writes=(), **kw):
            S.op(eng, lambda h: h.dma_start(out=out_ap, in_=in_ap, **kw), reads=reads, writes=writes, dma=True)

        bank_rr = [0]

        def nbank():
            b = banks[bank_rr[0] % 6]
            bank_rr[0] += 1
            return b

        wr_rr = [0]

        def wload(l, u):
            w = WR[wr_rr[0] % 3]
            wr_rr[0] += 1
            r0 = (l * NUNIT + u) * 256
            src = WS[r0:r0 + 256, :]
            dma("sp", w.v().ap, ws_d[r0:r0 + 256, :].rearrange("(p a) c -> p (a c)", a=2), reads=[src], writes=[w.v()])
            return w

        tr_rr = [0]

        def trhalf():
            hf = tr_rr[0] % 2
            tr_rr[0] += 1
            return PT[hf]

        csl = lambda a, n: consts_in[:, a:a + n]
        dma("pool", identb.v().ap, csl(C_ID, 128), writes=[identb.v()])
        dma("pool", trineg.v().ap, csl(C_TRI, 128), writes=[trineg.v()])
        dma("pool", negones.v().ap, csl(C_NONE, 128), writes=[negones.v()])
        dma("pool", negm.v().ap, csl(C_NEGM, 2048).rearrange("p (a b) -> p a b", a=4), writes=[negm.v()])
        dma("sp", DTt.v().ap, csl(C_DT, 512), writes=[DTt.v()])
        dma("sp", QDt.v().ap, csl(C_QD, 512).rearrange("p (a b) -> p a b", a=4), writes=[QDt.v()])
        dma("sp", KDTt.v().ap, csl(C_KDT, 512), writes=[KDTt.v()])
        dma("sp", tril.v().ap, csl(C_TRIL, 128), writes=[tril.v()])
        dma("sp", smalls.v().ap, smalls_in.rearrange("l p c -> p l c"), writes=[smalls.v()])
        dma("pool", wabd.v().ap, wabd_in.rearrange("l p (a b c) -> p l a b c", a=2, b=4), writes=[wabd.v()])
        for l in range(NL):
            dma("pool", browd_d[l], brow_in[l], writes=[BROWD[l:l + 1, :, :]])
        CH = 512
        for r0 in range(0, NWROWS, CH):
            r1 = min(NWROWS, r0 + CH)
            dst = WS[r0:r1, :]
            dma("pool", dst.ap, wpack[r0:r1, :], writes=[dst])
        mset("dve", ones_row.v(), 1.0)
        mset("dve", st.v(), 0.0)
        mset("dve", stbf.v(), 0.0)
        mset("dve", hstate.v(), 0.0)
        mset("dve", lxtail.v(), 0.0)
        for l in range(NL):
            lam = smalls[:, l, SM_LAM:SM_LAM + 4]
            act(lrc[:, l, :, 0], lam, AF.Exp, scale=-1.0)
            act(lrc[:, l, :, 0], lrc[:, l, :, 0], AF.Ln, bias=1.0)
            ts("dve", lrc[:, l, :, 1], lrc[:, l, :, 0], -8.0, None, ALU.mult)
            ts("dve", lrc[:, l, :, 0], lrc[:, l, :, 0], -4.0, None, ALU.mult)
            ts("dve", lrc[:, l, :, 2], smalls[:, l, SM_BA:SM_BA + 4], 0.5, None, ALU.mult)
            ts("dve", lrc[:, l, :, 3], smalls[:, l, SM_BX:SM_BX + 4], 0.5, None, ALU.mult)
            wtmp = sbuf(0, [4, 128], F32)
            wtb = sbuf(2048, [4, 128], BF16)
            dma("sp", wtmp.v().ap, sgws_in[l].rearrange("p (a b) -> p a b", a=4), writes=[wtmp.v()])
            for g in range(4):
                tt("dve", wtb[:, g, :], wtmp[:, g, :], tril.v(), ALU.mult)
            for g in range(4):
                tr(PT[0][:, g * 128:(g + 1) * 128], wtb[:, g, :])
            cp("dve", wsT[:, l, :, :], Buf(PT[0].ap[:, 0:512].rearrange("p (a b) -> p a b", a=4), PT[0].key, [128, 4, 128], whole=True).v())

        def ln_tile(xbuf, gtab, btab):
            for blk in range(4):
                for c in range(2):
                    sc = xbuf[:, blk, c * 512:(c + 1) * 512]
                    S.op("dve", lambda h, c=c, sc=sc, blk=blk: h.bn_stats(out=i_st[:, blk, c, :].ap, in_=sc.ap),
                         reads=[sc], writes=[i_st[:, blk, c, :]])
            for blk in range(4):
                S.op("dve", lambda h, blk=blk: h.bn_aggr(out=i_mv[:, blk, :].ap, in_=i_st[:, blk, :, :].ap),
                     reads=[i_st[:, blk, :, :]], writes=[i_mv[:, blk, :]])
            ts("dve", i_rs.v(), i_mv[:, :, 1], LN_EPS, None, ALU.add)
            act(i_rs.v(), i_rs.v(), AF.Sqrt)
            S.op("dve", lambda h: h.reciprocal(out=i_rs.v().ap, in_=i_rs.v().ap), reads=[i_rs.v()], writes=[i_rs.v()])
            for blk in range(4):
                src = xbuf[:, blk, :]
                stt("dve", src, src, i_mv[:, blk, 0:1], gtab, ALU.subtract, ALU.mult)
                stt("dve", src, src, i_rs[:, blk:blk + 1], btab, ALU.mult, ALU.add)

        def ln_blk(xbuf, blk, gtab, btab):
            for c in range(2):
                sc = xbuf[:, blk, c * 512:(c + 1) * 512]
                S.op("dve", lambda h, c=c, sc=sc: h.bn_stats(out=i_st[:, blk, c, :].ap, in_=sc.ap),
                     reads=[sc], writes=[i_st[:, blk, c, :]])
            S.op("dve", lambda h: h.bn_aggr(out=i_mv[:, blk, :].ap, in_=i_st[:, blk, :, :].ap),
                 reads=[i_st[:, blk, :, :]], writes=[i_mv[:, blk, :]])
            rsv = i_rs[:, blk:blk + 1]
            ts("dve", rsv, i_mv[:, blk, 1:2], LN_EPS, None, ALU.add)
            act(rsv, rsv, AF.Sqrt)
            S.op("dve", lambda h: h.reciprocal(out=rsv.ap, in_=rsv.ap), reads=[rsv], writes=[rsv])
            src = xbuf[:, blk, :]
            stt("dve", src, src, i_mv[:, blk, 0:1], gtab, ALU.subtract, ALU.mult)
            stt("dve", src, src, rsv, btab, ALU.mult, ALU.add)

        def transpose_blk(src_f32, blk, dst):
            cp("pool", xb[:, blk, :], src_f32[:, blk, :])
            hf = trhalf()
            for dc in range(8):
                tr(hf[:, dc * 128:(dc + 1) * 128], xb[:, blk, dc * 128:(dc + 1) * 128])
            cp("act" if blk % 2 == 0 else "dve", dst[:, :, blk * 128:(blk + 1) * 128],
               Buf(hf.ap.rearrange("p (a b) -> p a b", a=8), hf.key, [128, 8, 128], whole=True).v())

        def transpose_tile(src_f32, dst):
            for blk in range(4):
                cp("pool", xb[:, blk, :], src_f32[:, blk, :])
                hf = trhalf()
                for dc in range(8):
                    tr(hf[:, dc * 128:(dc + 1) * 128], xb[:, blk, dc * 128:(dc + 1) * 128])
                cp("act" if blk % 2 == 0 else "dve", dst[:, :, blk * 128:(blk + 1) * 128],
                   Buf(hf.ap.rearrange("p (a b) -> p a b", a=8), hf.key, [128, 8, 128], whole=True).v())

        def tile_layer(t, l, last):
            xt_, x1_ = XA, XB
            bc = lambda j: smalls[:, l, SM_BC + j:SM_BC + j + 1]
            transpose_tile(xt_, xT)
            dma("sp", browb.v().ap, browd_d[l], reads=[BROWD[l:l + 1, :, :]], writes=[browb.v()])
            for g in range(5):
                w = wload(l, U_TM + g)
                for blk in range(4):
                    pb = nbank()
                    for kc in range(8):
                        mm(pb.v(), xT[:, kc, blk * 128:(blk + 1) * 128], w[:, kc * 512:(kc + 1) * 512], kc == 0, False)
                    mm(pb.v(), ones_row.v(), browb[:, g * 512:(g + 1) * 512], False, True)
                    if g == 0:
                        cp("act", rv[:, blk, :], pb.v())
                    elif g == 1:
                        act(rgs[:, blk, :], pb.v(), AF.Silu)
                    elif g == 2:
                        cp("act", sv[:, blk, :], pb.v())
                    elif g == 3:
                        act(su[:, blk, :], pb.v(), AF.Gelu_apprx_tanh)
                    else:
                        act(sgv[:, blk, :], pb.v(), AF.Gelu_apprx_tanh)
                if g == 2:
                    r0 = l * NBT + t * 4
                    dst = VH[r0:r0 + 4, :, :]
                    dma("sp", vh_d[r0:r0 + 4].rearrange("b p c -> p b c"), sv.v().ap, reads=[sv.v()], writes=[dst])
            for i in range(4):
                dma("sp", rot_t[i].v().ap, rot_in[i, :, t * T:(t + 1) * T], writes=[rot_t[i].v()])
            for qk in range(2):
                w0 = wload(l, U_FM + 2 * qk)
                w1 = wload(l, U_FM + 2 * qk + 1)
                dstb = rq if qk == 0 else rk
                for h in range(4):
                    pa = nbank()
                    pbk = nbank()
                    for kc in range(8):
                        mm(pa.v(), w0[:, kc * 512 + h * 128: kc * 512 + (h + 1) * 128], xT[:, kc, :], kc == 0, kc == 7)
                    for kc in range(8):
                        mm(pbk.v(), w1[:, kc * 512 + h * 128: kc * 512 + (h + 1) * 128], xT[:, kc, :], kc == 0, kc == 7)
                    j0 = (2 * qk) * 4 + h
                    j1 = (2 * qk + 1) * 4 + h
                    ta = rtmp[(h % 2) * 2]
                    tb = rtmp[(h % 2) * 2 + 1]
                    stt("dve", ta.v(), pa.v(), bc(j0), rot_t[2 * qk].v(), ALU.add, ALU.mult)
                    stt("dve", tb.v(), pbk.v(), bc(j1), rot_t[2 * qk + 1].v(), ALU.add, ALU.mult)
                    tt("pool", dstb[:, h, :], ta.v(), tb.v(), ALU.add)
            w = wload(l, U_FM + 4)
            for cc in range(4):
                pb = nbank()
                for kc in range(8):
                    mm(pb.v(), w[:, kc * 512 + cc * 128: kc * 512 + (cc + 1) * 128], xT[:, kc, :], kc == 0, kc == 7)
                act(lx_ext[:, cc, 3:515], pb.v(), AF.Identity, bias=bc(16 + cc))
            w = wload(l, U_FM + 5)
            for cc in range(4):
                pb = nbank()
                for kc in range(8):
                    mm(pb.v(), w[:, kc * 512 + cc * 128: kc * 512 + (cc + 1) * 128], xT[:, kc, :], kc == 0, kc == 7)
                act(gg[:, cc, :], pb.v(), AF.Gelu_apprx_tanh, bias=bc(20 + cc))
            w = wload(l, U_FM + 6)
            for h in range(4):
                pb = nbank()
                for kc in range(8):
                    mm(pb.v(), w[:, kc * 512 + h * 128: kc * 512 + (h + 1) * 128], xT[:, kc, :], kc == 0, kc == 7)
                ts("dve", sq[:, h, :], pb.v(), bc(24 + h), 128.0 ** -0.5, ALU.add, ALU.mult)
            w = wload(l, U_FM + 7)
            for h in range(4):
                pb = nbank()
                for kc in range(8):
                    mm(pb.v(), w[:, kc * 512 + h * 128: kc * 512 + (h + 1) * 128], xT[:, kc, :], kc == 0, kc == 7)
                act(sk[:, h, :], pb.v(), AF.Identity, bias=bc(28 + h))
            for h in range(4):
                r = (l * 4 + h) * NT + t
                dma("sp", kh_d[r], sk[:, h, :].ap, reads=[sk[:, h, :]], writes=[KH[r:r + 1, :, :]])

            for c in range(4):
                cs = slice(c * 128, (c + 1) * 128)
                tt("pool", qd[:, :, cs], rq[:, :, cs], QDt.v(), ALU.mult)
                tt("pool", vdec[:, c, :], rv[:, c, :], KDTt.v(), ALU.mult)
            for c in range(4):
                cs = slice(c * 128, (c + 1) * 128)
                hf = trhalf()
                for h in range(4):
                    tr(hf[:, h * 128:(h + 1) * 128], rk[:, h, cs])
                cp("dve", rkt[:, c, :], hf[:, 0:512])
                sb_ = banks[3]
                for h in range(4):
                    mm(sb_[:, h * 128:(h + 1) * 128], rk[:, h, cs], rq[:, h, cs], True, True, sig=(h == 3))
                sdc = sd_[c % 2]
                tt("dve", sdc.v(), Buf(sb_.ap.rearrange("p (a b) -> p a b", a=4), sb_.key, [128, 4, 128], whole=True).v(),
                   Buf(DTt.ap.rearrange("p (a b) -> p a b", a=4), "AR", [128, 4, 128], base=DTt.base, esz=4).v(), ALU.mult)
                yb_ = banks[4]
                for h in range(4):
                    hs = slice(h * 128, (h + 1) * 128)
                    mm(yb_[:, hs], sdc[:, h, :], rv[:, c, hs], True, False, sig=False)
                    mm(yb_[:, hs], qd[:, h, cs], stbf[:, l, h, :], False, True, sig=(h == 3))
                kvb = banks[5]
                for h in range(4):
                    hs = slice(h * 128, (h + 1) * 128)
                    mm(kvb[:, hs], rkt[:, c, hs], vdec[:, c, hs], True, True, sig=(h == 3))
                for h in range(4):
                    hs = slice(h * 128, (h + 1) * 128)
                    cdk = float(np.exp(np.float64(128.0) * np.log1p(-(2.0 ** (-5.0 - h)))))
                    stt("dve", st[:, l, h, :], st[:, l, h, :], cdk, kvb[:, hs], ALU.mult, ALU.add)
                cp("act", stbf[:, l, :, :], st[:, l, :, :])
                for h in range(4):
                    hs = slice(h * 128, (h + 1) * 128)
                    S.op("dve", lambda hh, h=h, hs=hs: hh.bn_stats(out=stats[:, h, :].ap, in_=yb_[:, hs].ap),
                         reads=[yb_[:, hs]], writes=[stats[:, h, :]])
                    S.op("dve", lambda hh, h=h: hh.bn_aggr(out=mv[:, h, :].ap, in_=stats[:, h, :].ap),
                         reads=[stats[:, h, :]], writes=[mv[:, h, :]])
                ts("dve", rs4.v(), mv[:, :, 1], LN_EPS, None, ALU.add)
                act(rs4.v(), rs4.v(), AF.Sqrt)
                S.op("dve", lambda hh: hh.reciprocal(out=rs4.v().ap, in_=rs4.v().ap), reads=[rs4.v()], writes=[rs4.v()])
                for h in range(4):
                    hs = slice(h * 128, (h + 1) * 128)
                    ts("dve", yn[:, hs], yb_[:, hs], mv[:, h, 0:1], rs4[:, h:h + 1], ALU.subtract, ALU.mult)
                tt("pool", ya.v(), yn.v(), rgs[:, c, :], ALU.mult)
                hf = trhalf()
                for wc in range(4):
                    tr(hf[:, wc * 128:(wc + 1) * 128], ya[:, wc * 128:(wc + 1) * 128])
                cp("act", ybT[0][:, :, cs], Buf(hf.ap[:, 0:512].rearrange("p (a b) -> p a b", a=4), hf.key, [128, 4, 128], whole=True).v())

            for cc in range(4):
                cp("pool", lx_ext[:, cc, 0:3], lxtail[:, l, cc, :])
            for cc in range(4):
                cw = lambda j: smalls[:, l, SM_CW + cc * 4 + j: SM_CW + cc * 4 + j + 1]
                ts("dve", e_xc.v(), lx_ext[:, cc, 0:512], cw(0), smalls[:, l, SM_CB + cc:SM_CB + cc + 1], ALU.mult, ALU.add)
                for j in range(1, 4):
                    stt("dve", e_xc.v(), lx_ext[:, cc, j:j + 512], cw(j), e_xc.v(), ALU.mult, ALU.add)
                cp("pool", lxtail[:, l, cc, :], lx_ext[:, cc, 512:515])
                cp("pool", e_xcb.v(), e_xc.v())
                pr = nbank()
                pi = nbank()
                mm(pr.v(), wabd[:, l, 0, cc, :], e_xcb.v(), True, True)
                mm(pi.v(), wabd[:, l, 1, cc, :], e_xcb.v(), True, True)
                act(e_tr.v(), pr.v(), AF.Tanh, bias=lrc[:, l, cc, 2:3], scale=0.5)
                act(e_ti.v(), pi.v(), AF.Tanh, bias=lrc[:, l, cc, 3:4], scale=0.5)
                act(e_a.v(), e_tr.v(), AF.Exp, bias=lrc[:, l, cc, 0:1], scale=lrc[:, l, cc, 0:1])
                act(e_s.v(), e_tr.v(), AF.Exp, bias=lrc[:, l, cc, 1:2], scale=lrc[:, l, cc, 1:2])
                act(e_s.v(), e_s.v(), AF.Sqrt, bias=0.25, scale=-0.25)
                tt("dve", e_u.v(), e_s.v(), e_xc.v(), ALU.mult)
                stt("dve", e_u.v(), e_ti.v(), 1.0, e_u.v(), ALU.add, ALU.mult)
                eh = e_h[cc % 2]
                S.op("dve", lambda hh, eh=eh, cc=cc: hh.tensor_tensor_scan(out=eh.v().ap, data0=e_a.v().ap, data1=e_u.v().ap,
                                                                          initial=hstate[:, l, cc:cc + 1].ap,
                                                                          op0=ALU.mult, op1=ALU.add),
                     reads=[e_a.v(), e_u.v(), hstate[:, l, cc:cc + 1]], writes=[eh.v()])
                cp("dve", hstate[:, l, cc:cc + 1], eh[:, 511:512])
                tt("pool", ybT[1][:, cc, :], eh.v(), gg[:, cc, :], ALU.mult)

            nkb = 4 * (t + 1)
            nch = (t + 4) // 4
            steps = []
            for h in range(4):
                kbi = 0
                for ch in range(nch - 1, -1, -1):
                    t0 = ch * 4
                    ntl = min(t + 1, t0 + 4) - t0
                    for kl in range(ntl * 4 - 1, -1, -1):
                        steps.append((h, ch, t0, ntl, kl, kbi, kl == ntl * 4 - 1))
                        kbi += 1

            def kv_bufs(h, ch):
                return f_k[(h * nch + ch) % 2], f_v[(h * nch + ch) % 2]

            def s1(i):
                h, ch, t0, ntl, kl, kbi, first = steps[i]
                fk, fv = kv_bufs(h, ch)
                if first:
                    r = (l * 4 + h) * NT + t0
                    dma("sp", fk[:, 0:ntl * 512].ap.rearrange("p (a b) -> p a b", a=ntl),
                        kh_d[r:r + ntl].rearrange("a p c -> p a c"), reads=[KH[r:r + ntl, :, :]], writes=[fk[:, 0:ntl * 512]])
                    rb = l * NBT + t0 * 4
                    dma("sp", fv[:, 0:ntl * 4, :].ap, vh_d[rb:rb + ntl * 4, :, h * 128:(h + 1) * 128].rearrange("b p c -> p b c"),
                        reads=[VH[rb:rb + ntl * 4, :, :]], writes=[fv[:, 0:ntl * 4, :]])
                kb = t0 * 4 + kl
                diag = kb >= 4 * t
                a_ = kb - 4 * t
                par = i % 2
                kT = fk[:, kl * 128:(kl + 1) * 128]
                pa = banks[par]
                mm(pa.v(), kT, sq[:, h, :], True, not diag)
                if diag:
                    mm(pa.v(), identb.v(), negm[:, a_, :], False, True)
                act(f_e[par].v(), pa.v(), AF.Exp)
                act(f_lp[par].v(), f_e[par].v(), AF.Ln, bias=1.0)
                if kbi < nkb - 1:
                    if kbi == 0:
                        cp("dve", f_r.v(), f_lp[par].v())
                    else:
                        tt("dve", f_r.v(), f_r.v(), f_lp[par].v(), ALU.add)
                    cp("dve", f_rb[(i + 1) % 3].v(), f_r.v())

            def s2(i):
                h, ch, t0, ntl, kl, kbi, first = steps[i]
                fk, fv = kv_bufs(h, ch)
                kb = t0 * 4 + kl
                diag = kb >= 4 * t
                a_ = kb - 4 * t
                par = i % 2
                kT = fk[:, kl * 128:(kl + 1) * 128]
                pbk = banks[2 + par]
                ob = banks[4 + h % 2]
                mm(pbk.v(), kT, sq[:, h, :], True, False, sig=False)
                lastmm = "tri"
                if kbi > 0:
                    lastmm = "ones"
                if diag:
                    lastmm = "neg"
                mm(pbk.v(), trineg.v(), f_lp[par].v(), False, lastmm == "tri", sig=(lastmm == "tri"))
                if kbi > 0:
                    mm(pbk.v(), negones.v(), f_rb[i % 3].v(), False, lastmm == "ones", sig=(lastmm == "ones"))
                if diag:
                    mm(pbk.v(), identb.v(), negm[:, a_, :], False, True)
                act(f_w[par].v(), pbk.v(), AF.Exp)

            def s3(i):
                h, ch, t0, ntl, kl, kbi, first = steps[i]
                fk, fv = kv_bufs(h, ch)
                par = i % 2
                ob = banks[4 + h % 2]
                mm(ob.v(), fv[:, kl, :], f_w[par].v(), kbi == 0, kbi == nkb - 1)
                if kbi == nkb - 1:
                    cp("dve", ybT[2][:, h, :], ob.v())

            s1(0)
            for i in range(len(steps)):
                if i + 1 < len(steps):
                    s1(i + 1)
                s2(i)
                if i >= 1:
                    s3(i - 1)
            s3(len(steps) - 1)

            dma("sp", g_tab.v().ap, rowtab_in[l, :, 0:1024].rearrange("p (a b) -> p a b", a=2), writes=[g_tab.v()])
            for blk in range(4):
                bs_ = slice(blk * 128, (blk + 1) * 128)
                src = sgv[:, blk, :]
                S.op("dve", lambda hh, src=src: hh.bn_stats(out=g_st.v().ap, in_=src.ap), reads=[src], writes=[g_st.v()])
                S.op("dve", lambda hh: hh.bn_aggr(out=g_mv.v().ap, in_=g_st.v().ap), reads=[g_st.v()], writes=[g_mv.v()])
                ts("dve", g_rs.v(), g_mv[:, 1:2], LN_EPS, None, ALU.add)
                act(g_rs.v(), g_rs.v(), AF.Sqrt)
                S.op("dve", lambda hh: hh.reciprocal(out=g_rs.v().ap, in_=g_rs.v().ap), reads=[g_rs.v()], writes=[g_rs.v()])
                stt("dve", g_vn.v(), src, g_mv[:, 0:1], g_tab[:, 0, :], ALU.subtract, ALU.mult)
                stt("dve", g_vl.v(), g_vn.v(), g_rs.v(), g_tab[:, 1, :], ALU.mult, ALU.add)
                pb = nbank()
                for g in range(4):
                    gs = slice(g * 128, (g + 1) * 128)
                    mm(pb[:, gs], wsT[:, l, g, :], g_vl[:, gs], True, True, sig=(g == 3))
                for g in range(4):
                    gs = slice(g * 128, (g + 1) * 128)
                    stt("dve", g_yd[:, gs], pb[:, gs], smalls[:, l, SM_BS + g:SM_BS + g + 1], su[:, blk, gs], ALU.add, ALU.mult)
                hf = trhalf()
                for wc in range(4):
                    tr(hf[:, wc * 128:(wc + 1) * 128], g_yd[:, wc * 128:(wc + 1) * 128])
                cp("act", ybT[3][:, :, bs_], Buf(hf.ap[:, 0:512].rearrange("p (a b) -> p a b", a=4), hf.key, [128, 4, 128], whole=True).v())

            for dc in range(8):
                wg = wload(l, U_GT + dc)
                if dc % 2 == 0:
                    wbr = wload(l, U_BR + dc // 2)
                dcl = dc % 2
                for b in range(4):
                    pp = nbank()
                    pg = nbank()
                    for wc in range(4):
                        o_ = dcl * 2048 + (b * 4 + wc) * 128
                        mm(pp.v(), wbr[:, o_:o_ + 128], ybT[b][:, wc, :], wc == 0, wc == 3)
                    for kc in range(8):
                        mm(pg.v(), wg[:, kc * 512 + b * 128: kc * 512 + (b + 1) * 128], xT[:, kc, :], kc == 0, kc == 7)
                    gb = h_g[b % 2]
                    act(gb.v(), pg.v(), AF.Sigmoid, bias=bc(32 + dc * 4 + b))
                    if b == 0:
                        tt("dve", h_acc.v(), gb.v(), pp.v(), ALU.mult)
                    else:
                        tmp = h_tmp[b % 2]
                        tt("dve", tmp.v(), gb.v(), pp.v(), ALU.mult)
                        if b < 3:
                            tt("pool", h_acc.v(), h_acc.v(), tmp.v(), ALU.add)
                        else:
                            tt("pool", mergedT[:, dc, :], h_acc.v(), tmp.v(), ALU.add)

            dma("sp", i_tab.v().ap, rowtab_in[l, :, 1024:3072].rearrange("p (a b) -> p a b", a=2), writes=[i_tab.v()])
            wos = [wload(l, U_WO), wload(l, U_WO + 1)]
            for blk in range(4):
                for half in range(2):
                    bk = banks[(blk % 2) * 2 + half]
                    w = wos[half]
                    hsl = slice(half * 512, (half + 1) * 512)
                    for dc in range(8):
                        mm(bk.v(), mergedT[:, dc, blk * 128:(blk + 1) * 128], w[:, dc * 512:(dc + 1) * 512], dc == 0, dc == 7)
                    stt("dve", x1_[:, blk, hsl], xt_[:, blk, hsl], ALPHA, bk.v(), ALU.mult, ALU.add)
                ln_blk(x1_, blk, i_tab[:, 0, :], i_tab[:, 1, :])
                transpose_blk(x1_, blk, xT)

            dma("sp", i_tab.v().ap, rowtab_in[l, :, 3072:5120].rearrange("p (a b) -> p a b", a=2), writes=[i_tab.v()])
            dma("sp", browb.v().ap, browd_d[l], reads=[BROWD[l:l + 1, :, :]], writes=[browb.v()])
            for u in range(8):
                w = wload(l, U_W1 + u)
                for f in range(4):
                    fc = u * 4 + f
                    pb = nbank()
                    for kc in range(8):
                        mm(pb.v(), w[:, kc * 512 + f * 128: kc * 512 + (f + 1) * 128], xT[:, kc, :], kc == 0, kc == 7)
                    jr = j_r[fc % 2]
                    act(jr.v(), pb.v(), AF.Relu, bias=smalls[:, l, SM_B1 + fc:SM_B1 + fc + 1])
                    tt("pool", hT[:, fc, :], jr.v(), jr.v(), ALU.mult)
            for half in range(2):
                for u4 in range(4):
                    w = wload(l, U_W2 + half * 4 + u4)
                    for j in range(8):
                        fc = u4 * 8 + j
                        for blk in range(4):
                            mm(banks[blk].v(), hT[:, fc, blk * 128:(blk + 1) * 128], w[:, j * 512:(j + 1) * 512], fc == 0, False, sig=False)
                for blk in range(4):
                    mm(banks[blk].v(), ones_row.v(), browb[:, 2560 + half * 512: 2560 + (half + 1) * 512], False, True)
                for blk in range(4):
                    hsl = slice(half * 512, (half + 1) * 512)
                    stt("dve", xt_[:, blk, hsl], x1_[:, blk, hsl], ALPHA, banks[blk].v(), ALU.mult, ALU.add)
            ln_tile(xt_, i_tab[:, 0, :], i_tab[:, 1, :])
            if last:
                dma("sp", y_out[t * T:(t + 1) * T, :].rearrange("(b p) c -> p b c", p=128), xt_.v().ap, reads=[xt_.v()])

        for t in range(NT):
            dma("sp", XA.v().ap, x_in[t * T:(t + 1) * T, :].rearrange("(b p) c -> p b c", p=128), writes=[XA.v()])
            for l in range(NL):
                tile_layer(t, l, l == NL - 1)

        S.emit({"pe": block.tensor, "act": block.scalar, "dve": block.vector, "pool": block.gpsimd, "sp": block.sync},
               csem, dsem)
    return nc, S


def _unit_cols(Wc):
    return np.ascontiguousarray(Wc.reshape(8, 128, 512).transpose(1, 0, 2)).reshape(128, 4096)


def _swap_half(a):
    s = a.reshape(a.shape[:-1] + (4, 2, 64))
    return np.ascontiguousarray(s[..., ::-1, :]).reshape(a.shape)


def pack_layer(inp, l):
    w_in = inp["w_in"][l]
    b_in = inp["b_in"][l]
    sec = lambda i, n=512: slice(i, i + n)
    RQ, RK, RV, RG, LX, LG, SQ, SK, SV, SU, SGV, GT = 0, 512, 1024, 1536, 2048, 2560, 3072, 3584, 4096, 4608, 5120, 5632
    units = []
    for c0 in (RV, RG, SV, SU, SGV):
        units.append(_unit_cols(w_in[:, sec(c0)]))
    units.append(_unit_cols(w_in[:, sec(RQ)]))
    units.append(_unit_cols(_swap_half(w_in[:, sec(RQ)])))
    units.append(_unit_cols(w_in[:, sec(RK)]))
    units.append(_unit_cols(_swap_half(w_in[:, sec(RK)])))
    for c0 in (LX, LG, SQ, SK):
        units.append(_unit_cols(w_in[:, sec(c0)]))
    for dc in range(8):
        cols = np.concatenate([w_in[:, GT + b * 1024 + dc * 128: GT + b * 1024 + (dc + 1) * 128] for b in range(4)], axis=1)
        units.append(_unit_cols(cols))
    wb = inp["w_branch"][l].reshape(4, 4, 128, 8, 128).transpose(2, 3, 0, 1, 4).reshape(128, 8, 16, 128)
    for j in range(4):
        units.append(np.ascontiguousarray(wb[:, 2 * j:2 * j + 2]).reshape(128, 4096))
    for half in range(2):
        units.append(_unit_cols(inp["w_out"][l][:, half * 512:(half + 1) * 512]))
    for u in range(8):
        units.append(_unit_cols(inp["w1"][l][:, u * 512:(u + 1) * 512]))
    w2 = inp["w2"][l]
    for half in range(2):
        wh = w2[:, half * 512:(half + 1) * 512].reshape(32, 128, 512)
        for u4 in range(4):
            units.append(np.ascontiguousarray(wh[8 * u4:8 * u4 + 8].transpose(1, 0, 2)).reshape(128, 4096))
    assert len(units) == NUNIT
    wp = np.stack(units, 0).astype(np.float32)
    sm = np.zeros((128, NSM), np.float32)
    colv = lambda v: np.ascontiguousarray(v.reshape(-1, 128).T)
    fm_b = [b_in[sec(RQ)], _swap_half(b_in[sec(RQ)]), b_in[sec(RK)], _swap_half(b_in[sec(RK)]),
            b_in[sec(LX)], b_in[sec(LG)], b_in[sec(SQ)], b_in[sec(SK)]]
    sm[:, SM_BC:SM_BC + 32] = np.concatenate([colv(v) for v in fm_b], axis=1)
    gcols = []
    for dc in range(8):
        for b in range(4):
            gcols.append(b_in[GT + b * 1024 + dc * 128: GT + b * 1024 + (dc + 1) * 128][:, None])
    sm[:, SM_BC + 32:SM_BC + 64] = np.concatenate(gcols, axis=1)
    cw = inp["conv_w"][l]
    sm[:, SM_CW:SM_CW + 16] = cw.reshape(4, 4, 128).transpose(2, 1, 0).reshape(128, 16)
    sm[:, SM_CB:SM_CB + 4] = colv(inp["conv_b"][l])
    sm[:, SM_BA:SM_BA + 4] = colv(inp["lru_ba"][l])
    sm[:, SM_BX:SM_BX + 4] = colv(inp["lru_bx"][l])
    sm[:, SM_LAM:SM_LAM + 4] = colv(inp["lru_lambda"][l])
    sm[:, SM_BS:SM_BS + 4] = inp["sg_bs"][l].T
    sm[:, SM_B1:SM_B1 + 32] = colv(inp["b1"][l])
    rep = lambda v: np.broadcast_to(v[None, :], (128, v.shape[0]))
    rowtab = np.concatenate([rep(inp["sg_ln_g"][l]), rep(inp["sg_ln_b"][l]), rep(inp["ln1_g"][l]), rep(inp["ln1_b"][l]),
                             rep(inp["ln2_g"][l]), rep(inp["ln2_b"][l])], axis=1).astype(np.float32)
    wabd = np.zeros((128, 2, 4, 128), np.float32)
    for k, nm in enumerate(("lru_wa", "lru_wx")):
        wm = inp[nm][l]
        for cc in range(4):
            wabd[0:64, k, cc, 0:64] = wm[2 * cc]
            wabd[64:128, k, cc, 64:128] = wm[2 * cc + 1]
    sgws = np.ascontiguousarray(inp["sg_ws"][l].transpose(1, 0, 2)).reshape(128, 512)
    brow = np.concatenate([b_in[sec(RV)], b_in[sec(RG)], b_in[sec(SV)], b_in[sec(SU)], b_in[sec(SGV)], inp["b2"][l]])[None, :]
    return wp, sm, rowtab, wabd.reshape(128, 1024), sgws, brow.astype(np.float32)


def make_consts(S_LEN):
    c = np.zeros((128, NCONST), np.float32)
    idx = np.arange(128)
    c[:, C_ID:C_ID + 128] = np.eye(128)
    c[:, C_TRI:C_TRI + 128] = -(idx[:, None] >= idx[None, :]).astype(np.float32)
    c[:, C_NONE:C_NONE + 128] = -1.0
    for a in range(4):
        key = a * 128 + idx[:, None]
        qry = np.arange(512)[None, :]
        c[:, C_NEGM + a * 512:C_NEGM + (a + 1) * 512] = np.where(key >= qry, NEG, 0.0)
    log_g = np.log1p(-(2.0 ** (-5.0 - np.arange(4, dtype=np.float64))))
    for h in range(4):
        diff = idx[None, :] - idx[:, None]
        c[:, C_DT + h * 128:C_DT + (h + 1) * 128] = np.where(diff >= 0, np.exp(log_g[h] * np.maximum(diff, 0)), 0.0)
        c[:, C_QD + h * 128:C_QD + (h + 1) * 128] = np.exp(log_g[h] * (idx + 1.0))[None, :]
        c[:, C_KDT + h * 128:C_KDT + (h + 1) * 128] = np.exp(log_g[h] * (127.0 - idx))[:, None]
    c[:, C_TRIL:C_TRIL + 128] = (idx[None, :] <= idx[:, None]).astype(np.float32)
    inv_freq = (np.float32(10000.0) ** (-np.arange(64, dtype=np.float32) / np.float32(64))).astype(np.float32)
    pos = np.arange(S_LEN, dtype=np.float32)
    ang = (pos[None, :] * inv_freq[:, None]).astype(np.float32).astype(np.float64)
    cos = np.cos(ang)
    sin = np.sin(ang)
    C = np.concatenate([cos, cos], 0)
    Sg = np.concatenate([-sin, sin], 0)
    ks = 128.0 ** -0.5
    rot = np.stack([C, Sg, C * ks, Sg * ks], 0).astype(np.float32)
    return c, rot


_CACHE = {}


def run_model(inputs, S_LEN, NL, n_cores, batch_of_core):
    key = (S_LEN, NL)
    if key not in _CACHE:
        _CACHE[key] = build_program(S_LEN, NL)
    nc, _ = _CACHE[key]
    inp = {k: np.asarray(v) for k, v in inputs.items()}
    packs = [pack_layer(inp, l) for l in range(NL)]
    wpack = np.concatenate([p[0].reshape(NUNIT * 256, 2048) for p in packs], 0)
    smalls = np.stack([p[1] for p in packs], 0)
    rowtab = np.stack([p[2] for p in packs], 0)
    wabd = np.stack([p[3] for p in packs], 0)
    sgws = np.stack([p[4] for p in packs], 0)
    brow = np.stack([p[5] for p in packs], 0)
    consts, rot = make_consts(S_LEN)
    shared = {"wpack": wpack, "smalls": smalls, "rowtab": rowtab, "wabd": wabd, "sgws": sgws,
              "consts": consts, "rot": rot, "brow": brow}
    in_maps = []
    for c in range(n_cores):
        m = dict(shared)
        m["x"] = np.ascontiguousarray(inp["x"][batch_of_core[c]], dtype=np.float32)
        in_maps.append(m)
    res = run_bass_kernel_spmd(nc, in_maps, core_ids=list(range(n_cores)))
    return [r["y"] for r in res.results]


def kernel(**inputs):
    x = np.asarray(inputs["x"])
    B, S_LEN, _ = x.shape
    boc = [c % B for c in range(8)]
    outs = run_model(inputs, S_LEN, DEPTH, 8, boc)
    return np.stack([outs[b] for b in range(B)], 0).astype(np.float32)
```

```python
import math
from contextlib import ExitStack

import numpy as np
import concourse.bass as bass
import concourse.mybir as mybir
from concourse.bass_utils import run_bass_kernel_spmd

F32 = mybir.dt.float32
BF16 = mybir.dt.bfloat16
AF = mybir.ActivationFunctionType
ALU = mybir.AluOpType

D_MODEL = 1024
BW = 512
DFF = 4096
DEPTH = 2
ALPHA = (2 * DEPTH) ** 0.25
LN_EPS = 1e-5
T = 512
NUNIT = 43
U_TM, U_FM, U_GT, U_BR, U_WO, U_W1, U_W2 = 0, 5, 13, 21, 25, 27, 35
NSM = 132
SM_BC, SM_CW, SM_CB, SM_BA, SM_BX, SM_LAM, SM_BS, SM_B1 = 0, 64, 80, 84, 88, 92, 96, 100
NCONST = 4096
C_ID, C_TRI, C_NONE, C_NEGM, C_DT, C_QD, C_KDT, C_TRIL = 0, 128, 256, 384, 2432, 2944, 3456, 3968
NEG = -30000.0


class View:
    __slots__ = ("ap", "key", "lo", "hi")

    def __init__(self, ap, key, lo, hi):
        self.ap, self.key, self.lo, self.hi = ap, key, lo, hi


class Buf:
    def __init__(self, ap, key, shape, part=True, base=0, esz=1, whole=False):
        self.whole = whole
        self.ap = ap
        self.key = key
        self.shape = list(shape)
        self.part = part
        self.base = base
        self.esz = esz
        free = self.shape[1:] if part else self.shape
        st = []
        s = 1
        for d in reversed(free):
            st.append(s)
            s *= d
        self.strides = list(reversed(st))

    def __getitem__(self, idx):
        if not isinstance(idx, tuple):
            idx = (idx,)
        idx = list(idx) + [slice(None)] * (len(self.shape) - len(idx))
        ap = self.ap[tuple(idx)]
        fi = idx[1:] if self.part else idx
        fs = self.shape[1:] if self.part else self.shape
        lo = 0
        hi = 0
        for ix, st, d in zip(fi, self.strides, fs):
            if isinstance(ix, int):
                a, b = ix, ix + 1
            else:
                a = 0 if ix.start is None else ix.start
                b = d if ix.stop is None else ix.stop
            lo += a * st
            hi += (b - 1) * st
        if self.whole:
            return View(ap, self.key, 0, 1)
        return View(ap, self.key, self.base + lo * self.esz, self.base + (hi + 1) * self.esz)

    def v(self):
        return self[tuple(slice(None) for _ in self.shape)]


class _Op:
    __slots__ = ("eng", "idx", "fn", "sig", "dma", "deps", "seq", "dman")


DMA_RING = 8
ENGS = ("pe", "act", "dve", "pool", "sp")


class Sched:
    def __init__(self):
        self.ops = {e: [] for e in ENGS}
        self.reg = {}
        self.seq = 0
        self.ndma = {e: 0 for e in ENGS}

    def op(self, eng, fn, reads=(), writes=(), sig=True, dma=False):
        o = _Op()
        o.eng, o.fn, o.sig, o.dma = eng, fn, sig, dma
        o.idx = len(self.ops[eng])
        o.seq = self.seq
        self.seq += 1
        o.dman = -1
        deps = {}
        if dma:
            o.dman = self.ndma[eng]
            self.ndma[eng] += 1
            o.sig = True
        for v in reads:
            self._read(v, o, deps)
        for v in writes:
            self._write(v, o, deps)
        out = []
        for p, raw in deps.items():
            if p is o:
                continue
            if p.eng == eng and not p.dma and not dma:
                if eng == "pe":
                    continue
            out.append(p)
        o.deps = out
        self.ops[eng].append(o)
        return o

    def _read(self, v, o, deps):
        recs = self.reg.setdefault(v.key, {})
        for (lo, hi), r in recs.items():
            if lo < v.hi and v.lo < hi and r[0] is not None:
                deps[r[0]] = True
        r = recs.get((v.lo, v.hi))
        if r is None:
            r = [None, {}, []]
            recs[(v.lo, v.hi)] = r
        if o.dma:
            r[2].append(o)
        else:
            r[1][o.eng] = o

    def _write(self, v, o, deps):
        recs = self.reg.setdefault(v.key, {})
        dead = []
        for (lo, hi), r in recs.items():
            if lo < v.hi and v.lo < hi:
                if r[0] is not None:
                    deps.setdefault(r[0], False)
                for p in r[1].values():
                    deps.setdefault(p, False)
                for p in r[2]:
                    deps.setdefault(p, False)
                if v.lo <= lo and hi <= v.hi:
                    dead.append((lo, hi))
        for k in dead:
            del recs[k]
        recs[(v.lo, v.hi)] = [o, {}, []]

    def emit(self, block_fns, csem, dsem):
        allops = sorted((o for e in ENGS for o in self.ops[e]), key=lambda o: o.seq)
        for o in allops:
            for p in o.deps:
                if p.dma or p.sig:
                    continue
                lst = self.ops[p.eng]
                k = p.idx
                while k < len(lst) and (lst[k].dma or not lst[k].sig):
                    k += 1
                if k >= len(lst) or lst[k].seq >= o.seq:
                    p.sig = True
        sigcount = {}
        nextsig = {}
        for e in ENGS:
            ops = self.ops[e]
            comp = [o for o in ops if not o.dma]
            if comp and not comp[-1].sig:
                comp[-1].sig = True
            c = 0
            sc = {}
            for o in ops:
                if not o.dma and o.sig:
                    c += 1
                sc[o.idx] = c
            sigcount[e] = sc
            ns = {}
            nxt = None
            for o in reversed(ops):
                if not o.dma and o.sig:
                    nxt = o
                ns[o.idx] = nxt
            nextsig[e] = ns

        def event(p, consumer):
            if p.dma:
                n = p.dman
                return dsem[p.eng][n % DMA_RING], 16 * (n // DMA_RING + 1)
            j = nextsig[p.eng][p.idx]
            assert j is not None
            assert j is p or j.seq < consumer.seq, "signal recorded after its consumer"
            return csem[p.eng], sigcount[p.eng][j.idx]

        def build(e):
            def body(h):
                waited = {}
                for o in self.ops[e]:
                    need = {}
                    for p in o.deps:
                        s, val = event(p, o)
                        k = id(s)
                        if need.get(k, (None, 0))[1] < val:
                            need[k] = (s, val)
                    if o.dma and o.dman >= DMA_RING:
                        n = o.dman - DMA_RING
                        s = dsem[e][n % DMA_RING]
                        val = 16 * (n // DMA_RING + 1)
                        k = id(s)
                        if need.get(k, (None, 0))[1] < val:
                            need[k] = (s, val)
                    for k, (s, val) in need.items():
                        if waited.get(k, 0) < val:
                            h.wait_ge(s, val)
                            waited[k] = val
                    inst = o.fn(h)
                    if o.dma:
                        inst.then_inc(dsem[e][o.dman % DMA_RING], 16)
                    elif o.sig:
                        inst.then_inc(csem[e], 1)
                nd = self.ndma[e]
                for n in range(max(0, nd - DMA_RING), nd):
                    s = dsem[e][n % DMA_RING]
                    val = 16 * (n // DMA_RING + 1)
                    if waited.get(id(s), 0) < val:
                        h.wait_ge(s, val)
                        waited[id(s)] = val
            return body

        for e in ENGS:
            if self.ops[e]:
                block_fns[e](build(e))


ARENA_BYTES = 192 * 1024


def build_program(S_LEN, NL):
    NT = S_LEN // T
    NBT = S_LEN // 128
    nc = bass.Bass("TRN2", target_bir_lowering=False)
    x_in = nc.dram_tensor("x", [S_LEN, D_MODEL], F32, kind="ExternalInput").ap()
    NWROWS = NL * NUNIT * 256
    wpack = nc.dram_tensor("wpack", [NWROWS, 2048], F32, kind="ExternalInput").ap()
    smalls_in = nc.dram_tensor("smalls", [NL, 128, NSM], F32, kind="ExternalInput").ap()
    rowtab_in = nc.dram_tensor("rowtab", [NL, 128, 5120], F32, kind="ExternalInput").ap()
    wabd_in = nc.dram_tensor("wabd", [NL, 128, 1024], F32, kind="ExternalInput").ap()
    sgws_in = nc.dram_tensor("sgws", [NL, 128, 512], F32, kind="ExternalInput").ap()
    consts_in = nc.dram_tensor("consts", [128, NCONST], F32, kind="ExternalInput").ap()
    rot_in = nc.dram_tensor("rot", [4, 128, S_LEN], F32, kind="ExternalInput").ap()
    brow_in = nc.dram_tensor("brow", [NL, 1, 3584], F32, kind="ExternalInput").ap()
    y_out = nc.dram_tensor("y", [S_LEN, D_MODEL], F32, kind="ExternalOutput").ap()
    ws_d = nc.dram_tensor("ws", [NWROWS, 2048], BF16, kind="Internal").ap()
    kh_d = nc.dram_tensor("kh", [NL * 4 * NT, 128, 512], BF16, kind="Internal").ap()
    vh_d = nc.dram_tensor("vh", [NL * NBT, 128, 512], BF16, kind="Internal").ap()
    browd_d = nc.dram_tensor("browd", [NL, 1, 3584], BF16, kind="Internal").ap()

    S = Sched()
    es = ExitStack()
    with es:
        art = es.enter_context(nc.sbuf_tensor("arena", [128, ARENA_BYTES // 2], BF16))
        banks = []
        for i in range(6):
            pt_ = es.enter_context(nc.psum_tensor(f"pb{i}", [128, 512], F32))
            banks.append(Buf(pt_, f"pb{i}", [128, 512], whole=True))
        PT = []
        for i in range(2):
            ptt_ = es.enter_context(nc.psum_tensor(f"ptr{i}", [128, 1024], BF16))
            PT.append(Buf(ptt_, f"ptr{i}", [128, 1024], whole=True))
        csem = {e: es.enter_context(nc.semaphore("c_" + e)) for e in ENGS}
        dsem = {e: [es.enter_context(nc.semaphore(f"d_{e}{i}")) for i in range(DMA_RING)] for e in ENGS}
        block = es.enter_context(nc.Block())

        WS = Buf(ws_d, "ws", [NWROWS, 2048], part=False)
        KH = Buf(kh_d, "kh", [NL * 4 * NT, 128, 512], part=False)
        VH = Buf(vh_d, "vh", [NL * NBT, 128, 512], part=False)
        BROWD = Buf(browd_d, "browd", [NL, 1, 3584], part=False)

        def abuf(off, shape, dt, parts=128):
            esz = 4 if dt == F32 else 2
            n = 1
            for d in shape:
                n *= d
            assert off % 4 == 0 and off + n * esz <= ARENA_BYTES, (off, shape)
            ap = art[0:parts, off // 2: off // 2 + n * esz // 2]
            if dt == F32:
                ap = ap.bitcast(F32)
            if len(shape) > 1:
                names = "abcdefg"[:len(shape)]
                pat = "p (" + " ".join(names) + ") -> p " + " ".join(names)
                ap = ap.rearrange(pat, **{n_: d_ for n_, d_ in zip(names[:-1], shape[:-1])})
            return Buf(ap, "AR", [parts] + list(shape), base=off, esz=esz)

        cur = [0]

        def palloc(shape, dt, parts=128):
            esz = 4 if dt == F32 else 2
            n = 1
            for d in shape:
                n *= d
            b = abuf(cur[0], shape, dt, parts)
            cur[0] += (n * esz + 31) // 32 * 32
            return b

        XA = palloc([4, 1024], F32)
        XB = palloc([4, 1024], F32)
        WR = [palloc([4096], BF16) for _ in range(3)]
        identb = palloc([128], BF16)
        trineg = palloc([128], BF16)
        negones = palloc([128], BF16)
        negm = palloc([4, 512], BF16)
        DTt = palloc([512], F32)
        QDt = palloc([4, 128], F32)
        KDTt = palloc([512], F32)
        tril = palloc([128], F32)
        smalls = palloc([NL, NSM], F32)
        lrc = palloc([NL, 4, 4], F32)
        wabd = palloc([NL, 2, 4, 128], BF16)
        wsT = palloc([NL, 4, 128], BF16)
        ones_row = palloc([128], BF16, parts=1)
        st = palloc([NL, 4, 128], F32)
        stbf = palloc([NL, 4, 128], BF16)
        hstate = palloc([NL, 4], F32)
        lxtail = palloc([NL, 4, 3], F32)
        SCR = cur[0]

        def sbuf(off, shape, dt, parts=128):
            return abuf(SCR + off, shape, dt, parts)

        xT = sbuf(0, [8, 512], BF16)
        xb = sbuf(8192, [4, 1024], BF16)
        ybT = [sbuf(16384 + 4096 * b, [4, 512], BF16) for b in range(4)]
        su = sbuf(32768, [4, 512], F32)
        sgv = sbuf(40960, [4, 512], F32)
        sq = sbuf(49152, [4, 512], BF16)
        lx_ext = sbuf(53248, [4, 516], F32)
        gg = sbuf(61504, [4, 512], F32)
        rv = sbuf(69696, [4, 512], BF16)
        rgs = sbuf(73792, [4, 512], F32)
        rq = sbuf(81984, [4, 512], BF16)
        rk = sbuf(86080, [4, 512], BF16)
        LOC_D = 90176
        rot_t = [sbuf(8192 + 2048 * i, [512], F32) for i in range(4)]
        rtmp = [sbuf(16384 + 2048 * i, [512], F32) for i in range(4)]
        sv = sbuf(24576, [4, 512], BF16)
        sk = sbuf(28672, [4, 512], BF16)
        browb = sbuf(8192, [3584], BF16, parts=1)
        qd = sbuf(LOC_D, [4, 512], BF16)
        rkt = sbuf(LOC_D + 4096, [4, 512], BF16)
        vdec = sbuf(LOC_D + 8192, [4, 512], BF16)
        sd_ = [sbuf(LOC_D + 12288 + 1024 * i, [4, 128], BF16) for i in range(2)]
        yn = sbuf(LOC_D + 14336, [512], F32)
        ya = sbuf(LOC_D + 16384, [512], BF16)
        stats = sbuf(LOC_D + 17408, [4, 6], F32)
        mv = sbuf(LOC_D + 17408 + 96, [4, 2], F32)
        rs4 = sbuf(LOC_D + 17408 + 128, [4], F32)
        END_D = LOC_D + 17408 + 160
        LOC_E = 69696
        e_xc = sbuf(LOC_E, [512], F32)
        e_xcb = sbuf(LOC_E + 2048, [512], BF16)
        e_tr = sbuf(LOC_E + 3072, [512], F32)
        e_ti = sbuf(LOC_E + 5120, [512], F32)
        e_a = sbuf(LOC_E + 7168, [512], F32)
        e_s = sbuf(LOC_E + 9216, [512], F32)
        e_u = sbuf(LOC_E + 11264, [512], F32)
        e_h = [sbuf(LOC_E + 13312 + 2048 * i, [512], F32) for i in range(2)]
        LOC_F = 53248
        f_k = [sbuf(LOC_F + 4096 * i, [2048], BF16) for i in range(2)]
        f_v = [sbuf(LOC_F + 8192 + 4096 * i, [16, 128], BF16) for i in range(2)]
        f_e = [sbuf(LOC_F + 16384 + 2048 * i, [512], F32) for i in range(2)]
        f_lp = [sbuf(LOC_F + 20480 + 1024 * i, [512], BF16) for i in range(2)]
        f_w = [sbuf(LOC_F + 22528 + 1024 * i, [512], BF16) for i in range(2)]
        f_r = sbuf(LOC_F + 24576, [512], F32)
        f_rb = [sbuf(LOC_F + 26624 + 1024 * i, [512], BF16) for i in range(3)]
        LOC_G = 53248
        g_tab = sbuf(LOC_G, [2, 512], F32)
        g_vn = sbuf(LOC_G + 4096, [512], F32)
        g_vl = sbuf(LOC_G + 6144, [512], BF16)
        g_yd = sbuf(LOC_G + 7168, [512], BF16)
        g_st = sbuf(LOC_G + 8192, [6], F32)
        g_mv = sbuf(LOC_G + 8192 + 32, [2], F32)
        g_rs = sbuf(LOC_G + 8192 + 64, [1], F32)
        mergedT = sbuf(32768, [8, 512], BF16)
        h_g = [sbuf(40960 + 2048 * i, [512], F32) for i in range(2)]
        h_acc = sbuf(40960 + 4096, [512], F32)
        h_tmp = [sbuf(40960 + 6144 + 2048 * i, [512], F32) for i in range(2)]
        LOC_I = 53248
        i_tab = sbuf(LOC_I, [2, 1024], F32)
        i_st = sbuf(LOC_I + 8192, [4, 2, 6], F32)
        i_mv = sbuf(LOC_I + 8192 + 192, [4, 2], F32)
        i_rs = sbuf(LOC_I + 8192 + 224, [4], F32)
        hT = sbuf(16384, [32, 512], BF16)
        j_r = [sbuf(49152 + 2048 * i, [512], F32) for i in range(2)]
        assert END_D <= ARENA_BYTES - SCR, (END_D, ARENA_BYTES - SCR)

        def vw(x):
            return x.v() if isinstance(x, Buf) else x

        def mm(out, lhsT, rhs, start, stop, sig=None):
            S.op("pe", lambda h: h.matmul(out.ap, lhsT=lhsT.ap, rhs=rhs.ap, start=start, stop=stop),
                 reads=[lhsT, rhs], writes=[out], sig=(stop if sig is None else sig))

        def tr(out, in_):
            idv = identb.v()
            S.op("pe", lambda h: h.transpose(out=out.ap, in_=in_.ap, identity=idv.ap), reads=[in_, idv], writes=[out])

        def act(out, in_, func, bias=None, scale=None):
            reads = [in_]
            kw = {}
            if bias is not None:
                if isinstance(bias, View):
                    reads.append(bias)
                    kw["bias"] = bias.ap
                else:
                    kw["bias"] = float(bias)
            if scale is not None:
                if isinstance(scale, View):
                    reads.append(scale)
                    kw["scale"] = scale.ap
                else:
                    kw["scale"] = float(scale)
            S.op("act", lambda h: h.activation(out=out.ap, in_=in_.ap, func=func, **kw), reads=reads, writes=[out])

        def tt(eng, out, a, b, op):
            S.op(eng, lambda h: h.tensor_tensor(out=out.ap, in0=a.ap, in1=b.ap, op=op), reads=[a, b], writes=[out])

        def ts(eng, out, a, s1, s2, op0, op1=None):
            reads = [a]
            k1 = s1.ap if isinstance(s1, View) else float(s1)
            if isinstance(s1, View):
                reads.append(s1)
            if op1 is None:
                S.op(eng, lambda h: h.tensor_scalar(out=out.ap, in0=a.ap, scalar1=k1, scalar2=None, op0=op0),
                     reads=reads, writes=[out])
                return
            k2 = s2.ap if isinstance(s2, View) else float(s2)
            if isinstance(s2, View):
                reads.append(s2)
            S.op(eng, lambda h: h.tensor_scalar(out=out.ap, in0=a.ap, scalar1=k1, scalar2=k2, op0=op0, op1=op1),
                 reads=reads, writes=[out])

        def stt(eng, out, in0, sc, in1, op0, op1):
            reads = [in0, in1]
            k = sc.ap if isinstance(sc, View) else float(sc)
            if isinstance(sc, View):
                reads.append(sc)
            S.op(eng, lambda h: h.scalar_tensor_tensor(out=out.ap, in0=in0.ap, scalar=k, in1=in1.ap, op0=op0, op1=op1),
                 reads=reads, writes=[out])

        def cp(eng, out, in_):
            if eng == "act":
                S.op("act", lambda h: h.copy(out=out.ap, in_=in_.ap), reads=[in_], writes=[out])
            else:
                S.op(eng, lambda h: h.tensor_copy(out=out.ap, in_=in_.ap), reads=[in_], writes=[out])

        _tt0, _cp0 = tt, cp

        def tt(eng, out, a, b, op):
            _tt0(eng, out, a, b, op)
            if eng == "pool":
                issue_cast()

        def cp(eng, out, in_):
            _cp0(eng, out, in_)
            if eng == "pool":
                issue_cast()

        def mset(eng, out, val):
            S.op(eng, lambda h: h.memset(out.ap, val), writes=[out])

        def dma(eng, out_ap, in_ap, reads=(), writes=(), **kw):
            S.op(eng, lambda h: h.dma_start(out=out_ap, in_=in_ap, **kw), reads=reads, writes=writes, dma=True)

        bank_rr = [0]

        def nbank():
            b = banks[bank_rr[0] % 6]
            bank_rr[0] += 1
            return b

        wr_rr = [0]
        cast_upto = [0]
        pending_casts = []

        def wload(l, u):
            w = WR[wr_rr[0] % 3]
            wr_rr[0] += 1
            r0 = (l * NUNIT + u) * 256
            while cast_upto[0] < r0 + 256:
                issue_cast()
            src = WS[r0:r0 + 256, :]
            dma("sp", w.v().ap, ws_d[r0:r0 + 256, :].rearrange("(p a) c -> p (a c)", a=2), reads=[src], writes=[w.v()])
            return w

        tr_rr = [0]

        def trhalf():
            hf = tr_rr[0] % 2
            tr_rr[0] += 1
            return PT[hf]

        csl = lambda a, n: consts_in[:, a:a + n]
        dma("pool", identb.v().ap, csl(C_ID, 128), writes=[identb.v()])
        dma("pool", trineg.v().ap, csl(C_TRI, 128), writes=[trineg.v()])
        dma("pool", negones.v().ap, csl(C_NONE, 128), writes=[negones.v()])
        dma("pool", negm.v().ap, csl(C_NEGM, 2048).rearrange("p (a b) -> p a b", a=4), writes=[negm.v()])
        dma("sp", DTt.v().ap, csl(C_DT, 512), writes=[DTt.v()])
        dma("sp", QDt.v().ap, csl(C_QD, 512).rearrange("p (a b) -> p a b", a=4), writes=[QDt.v()])
        dma("sp", KDTt.v().ap, csl(C_KDT, 512), writes=[KDTt.v()])
        dma("sp", tril.v().ap, csl(C_TRIL, 128), writes=[tril.v()])
        dma("sp", smalls.v().ap, smalls_in.rearrange("l p c -> p l c"), writes=[smalls.v()])
        dma("pool", wabd.v().ap, wabd_in.rearrange("l p (a b c) -> p l a b c", a=2, b=4), writes=[wabd.v()])
        for l in range(NL):
            dma("pool", browd_d[l], brow_in[l], writes=[BROWD[l:l + 1, :, :]])
        CH = 512
        for r0 in range(0, NWROWS, CH):
            pending_casts.append((r0, min(NWROWS, r0 + CH)))

        def issue_cast():
            if pending_casts:
                r0, r1 = pending_casts.pop(0)
                cast_upto[0] = r1
                dst = WS[r0:r1, :]
                dma("pool", dst.ap, wpack[r0:r1, :], writes=[dst])

        for _ in range(DMA_RING - 2):
            issue_cast()
        mset("dve", ones_row.v(), 1.0)
        mset("dve", st.v(), 0.0)
        mset("dve", stbf.v(), 0.0)
        mset("dve", hstate.v(), 0.0)
        mset("dve", lxtail.v(), 0.0)
        for l in range(NL):
            lam = smalls[:, l, SM_LAM:SM_LAM + 4]
            act(lrc[:, l, :, 0], lam, AF.Exp, scale=-1.0)
            act(lrc[:, l, :, 0], lrc[:, l, :, 0], AF.Ln, bias=1.0)
            ts("dve", lrc[:, l, :, 1], lrc[:, l, :, 0], -8.0, None, ALU.mult)
            ts("dve", lrc[:, l, :, 0], lrc[:, l, :, 0], -4.0, None, ALU.mult)
            ts("dve", lrc[:, l, :, 2], smalls[:, l, SM_BA:SM_BA + 4], 0.5, None, ALU.mult)
            ts("dve", lrc[:, l, :, 3], smalls[:, l, SM_BX:SM_BX + 4], 0.5, None, ALU.mult)
            wtmp = sbuf(0, [4, 128], F32)
            wtb = sbuf(2048, [4, 128], BF16)
            dma("sp", wtmp.v().ap, sgws_in[l].rearrange("p (a b) -> p a b", a=4), writes=[wtmp.v()])
            for g in range(4):
                tt("dve", wtb[:, g, :], wtmp[:, g, :], tril.v(), ALU.mult)
            for g in range(4):
                tr(PT[0][:, g * 128:(g + 1) * 128], wtb[:, g, :])
            cp("dve", wsT[:, l, :, :], Buf(PT[0].ap[:, 0:512].rearrange("p (a b) -> p a b", a=4), PT[0].key, [128, 4, 128], whole=True).v())

        def ln_tile(xbuf, gtab, btab):
            for blk in range(4):
                for c in range(2):
                    sc = xbuf[:, blk, c * 512:(c + 1) * 512]
                    S.op("dve", lambda h, c=c, sc=sc, blk=blk: h.bn_stats(out=i_st[:, blk, c, :].ap, in_=sc.ap),
                         reads=[sc], writes=[i_st[:, blk, c, :]])
            for blk in range(4):
                S.op("dve", lambda h, blk=blk: h.bn_aggr(out=i_mv[:, blk, :].ap, in_=i_st[:, blk, :, :].ap),
                     reads=[i_st[:, blk, :, :]], writes=[i_mv[:, blk, :]])
            ts("dve", i_rs.v(), i_mv[:, :, 1], LN_EPS, None, ALU.add)
            act(i_rs.v(), i_rs.v(), AF.Sqrt)
            S.op("dve", lambda h: h.reciprocal(out=i_rs.v().ap, in_=i_rs.v().ap), reads=[i_rs.v()], writes=[i_rs.v()])
            for blk in range(4):
                src = xbuf[:, blk, :]
                stt("dve", src, src, i_mv[:, blk, 0:1], gtab, ALU.subtract, ALU.mult)
                stt("dve", src, src, i_rs[:, blk:blk + 1], btab, ALU.mult, ALU.add)

        def ln_blk(xbuf, blk, gtab, btab):
            for c in range(2):
                sc = xbuf[:, blk, c * 512:(c + 1) * 512]
                S.op("dve", lambda h, c=c, sc=sc: h.bn_stats(out=i_st[:, blk, c, :].ap, in_=sc.ap),
                     reads=[sc], writes=[i_st[:, blk, c, :]])
            S.op("dve", lambda h: h.bn_aggr(out=i_mv[:, blk, :].ap, in_=i_st[:, blk, :, :].ap),
                 reads=[i_st[:, blk, :, :]], writes=[i_mv[:, blk, :]])
            rsv = i_rs[:, blk:blk + 1]
            ts("dve", rsv, i_mv[:, blk, 1:2], LN_EPS, None, ALU.add)
            act(rsv, rsv, AF.Sqrt)
            S.op("dve", lambda h: h.reciprocal(out=rsv.ap, in_=rsv.ap), reads=[rsv], writes=[rsv])
            src = xbuf[:, blk, :]
            stt("dve", src, src, i_mv[:, blk, 0:1], gtab, ALU.subtract, ALU.mult)
            stt("dve", src, src, rsv, btab, ALU.mult, ALU.add)

        def transpose_blk(src_f32, blk, dst):
            cp("pool", xb[:, blk, :], src_f32[:, blk, :])
            hf = trhalf()
            for dc in range(8):
                tr(hf[:, dc * 128:(dc + 1) * 128], xb[:, blk, dc * 128:(dc + 1) * 128])
            cp("act" if blk % 2 == 0 else "dve", dst[:, :, blk * 128:(blk + 1) * 128],
               Buf(hf.ap.rearrange("p (a b) -> p a b", a=8), hf.key, [128, 8, 128], whole=True).v())

        def transpose_tile(src_f32, dst):
            for blk in range(4):
                cp("pool", xb[:, blk, :], src_f32[:, blk, :])
                hf = trhalf()
                for dc in range(8):
                    tr(hf[:, dc * 128:(dc + 1) * 128], xb[:, blk, dc * 128:(dc + 1) * 128])
                cp("act" if blk % 2 == 0 else "dve", dst[:, :, blk * 128:(blk + 1) * 128],
                   Buf(hf.ap.rearrange("p (a b) -> p a b", a=8), hf.key, [128, 8, 128], whole=True).v())

        def tile_layer(t, l, last):
            xt_, x1_ = XA, XB
            bc = lambda j: smalls[:, l, SM_BC + j:SM_BC + j + 1]
            transpose_tile(xt_, xT)
            dma("pool", browb.v().ap, browd_d[l], reads=[BROWD[l:l + 1, :, :]], writes=[browb.v()])
            for g in range(5):
                w = wload(l, U_TM + g)
                for blk in range(4):
                    pb = nbank()
                    for kc in range(8):
                        mm(pb.v(), xT[:, kc, blk * 128:(blk + 1) * 128], w[:, kc * 512:(kc + 1) * 512], kc == 0, False)
                    mm(pb.v(), ones_row.v(), browb[:, g * 512:(g + 1) * 512], False, True)
                    if g == 0:
                        cp("act", rv[:, blk, :], pb.v())
                    elif g == 1:
                        act(rgs[:, blk, :], pb.v(), AF.Silu)
                    elif g == 2:
                        cp("act", sv[:, blk, :], pb.v())
                    elif g == 3:
                        act(su[:, blk, :], pb.v(), AF.Gelu_apprx_tanh)
                    else:
                        act(sgv[:, blk, :], pb.v(), AF.Gelu_apprx_tanh)
                if g == 2:
                    r0 = l * NBT + t * 4
                    dst = VH[r0:r0 + 4, :, :]
                    dma("pool", vh_d[r0:r0 + 4].rearrange("b p c -> p b c"), sv.v().ap, reads=[sv.v()], writes=[dst])
            for i in range(4):
                dma("pool", rot_t[i].v().ap, rot_in[i, :, t * T:(t + 1) * T], writes=[rot_t[i].v()])
            for qk in range(2):
                w0 = wload(l, U_FM + 2 * qk)
                w1 = wload(l, U_FM + 2 * qk + 1)
                dstb = rq if qk == 0 else rk
                for h in range(4):
                    pa = nbank()
                    pbk = nbank()
                    for kc in range(8):
                        mm(pa.v(), w0[:, kc * 512 + h * 128: kc * 512 + (h + 1) * 128], xT[:, kc, :], kc == 0, kc == 7)
                    for kc in range(8):
                        mm(pbk.v(), w1[:, kc * 512 + h * 128: kc * 512 + (h + 1) * 128], xT[:, kc, :], kc == 0, kc == 7)
                    j0 = (2 * qk) * 4 + h
                    j1 = (2 * qk + 1) * 4 + h
                    ta = rtmp[(h % 2) * 2]
                    tb = rtmp[(h % 2) * 2 + 1]
                    stt("dve", ta.v(), pa.v(), bc(j0), rot_t[2 * qk].v(), ALU.add, ALU.mult)
                    stt("dve", tb.v(), pbk.v(), bc(j1), rot_t[2 * qk + 1].v(), ALU.add, ALU.mult)
                    tt("pool", dstb[:, h, :], ta.v(), tb.v(), ALU.add)
            w = wload(l, U_FM + 4)
            for cc in range(4):
                pb = nbank()
                for kc in range(8):
                    mm(pb.v(), w[:, kc * 512 + cc * 128: kc * 512 + (cc + 1) * 128], xT[:, kc, :], kc == 0, kc == 7)
                act(lx_ext[:, cc, 3:515], pb.v(), AF.Identity, bias=bc(16 + cc))
            w = wload(l, U_FM + 5)
            for cc in range(4):
                pb = nbank()
                for kc in range(8):
                    mm(pb.v(), w[:, kc * 512 + cc * 128: kc * 512 + (cc + 1) * 128], xT[:, kc, :], kc == 0, kc == 7)
                act(gg[:, cc, :], pb.v(), AF.Gelu_apprx_tanh, bias=bc(20 + cc))
            w = wload(l, U_FM + 6)
            for h in range(4):
                pb = nbank()
                for kc in range(8):
                    mm(pb.v(), w[:, kc * 512 + h * 128: kc * 512 + (h + 1) * 128], xT[:, kc, :], kc == 0, kc == 7)
                ts("dve", sq[:, h, :], pb.v(), bc(24 + h), 128.0 ** -0.5, ALU.add, ALU.mult)
            w = wload(l, U_FM + 7)
            for h in range(4):
                pb = nbank()
                for kc in range(8):
                    mm(pb.v(), w[:, kc * 512 + h * 128: kc * 512 + (h + 1) * 128], xT[:, kc, :], kc == 0, kc == 7)
                act(sk[:, h, :], pb.v(), AF.Identity, bias=bc(28 + h))
            for h in range(4):
                r = (l * 4 + h) * NT + t
                dma("pool", kh_d[r], sk[:, h, :].ap, reads=[sk[:, h, :]], writes=[KH[r:r + 1, :, :]])

            for c in range(4):
                cs = slice(c * 128, (c + 1) * 128)
                tt("pool", qd[:, :, cs], rq[:, :, cs], QDt.v(), ALU.mult)
                tt("pool", vdec[:, c, :], rv[:, c, :], KDTt.v(), ALU.mult)
            for c in range(4):
                cs = slice(c * 128, (c + 1) * 128)
                hf = trhalf()
                for h in range(4):
                    tr(hf[:, h * 128:(h + 1) * 128], rk[:, h, cs])
                cp("dve", rkt[:, c, :], hf[:, 0:512])
                sb_ = banks[3]
                for h in range(4):
                    mm(sb_[:, h * 128:(h + 1) * 128], rk[:, h, cs], rq[:, h, cs], True, True, sig=(h == 3))
                sdc = sd_[c % 2]
                tt("dve", sdc.v(), Buf(sb_.ap.rearrange("p (a b) -> p a b", a=4), sb_.key, [128, 4, 128], whole=True).v(),
                   Buf(DTt.ap.rearrange("p (a b) -> p a b", a=4), "AR", [128, 4, 128], base=DTt.base, esz=4).v(), ALU.mult)
                yb_ = banks[4]
                for h in range(4):
                    hs = slice(h * 128, (h + 1) * 128)
                    mm(yb_[:, hs], sdc[:, h, :], rv[:, c, hs], True, False, sig=False)
                    mm(yb_[:, hs], qd[:, h, cs], stbf[:, l, h, :], False, True, sig=(h == 3))
                kvb = banks[5]
                for h in range(4):
                    hs = slice(h * 128, (h + 1) * 128)
                    mm(kvb[:, hs], rkt[:, c, hs], vdec[:, c, hs], True, True, sig=(h == 3))
                for h in range(4):
                    hs = slice(h * 128, (h + 1) * 128)
                    cdk = float(np.exp(np.float64(128.0) * np.log1p(-(2.0 ** (-5.0 - h)))))
                    stt("dve", st[:, l, h, :], st[:, l, h, :], cdk, kvb[:, hs], ALU.mult, ALU.add)
                cp("act", stbf[:, l, :, :], st[:, l, :, :])
                for h in range(4):
                    hs = slice(h * 128, (h + 1) * 128)
                    S.op("dve", lambda hh, h=h, hs=hs: hh.bn_stats(out=stats[:, h, :].ap, in_=yb_[:, hs].ap),
                         reads=[yb_[:, hs]], writes=[stats[:, h, :]])
                    S.op("dve", lambda hh, h=h: hh.bn_aggr(out=mv[:, h, :].ap, in_=stats[:, h, :].ap),
                         reads=[stats[:, h, :]], writes=[mv[:, h, :]])
                ts("dve", rs4.v(), mv[:, :, 1], LN_EPS, None, ALU.add)
                act(rs4.v(), rs4.v(), AF.Sqrt)
                S.op("dve", lambda hh: hh.reciprocal(out=rs4.v().ap, in_=rs4.v().ap), reads=[rs4.v()], writes=[rs4.v()])
                for h in range(4):
                    hs = slice(h * 128, (h + 1) * 128)
                    ts("dve", yn[:, hs], yb_[:, hs], mv[:, h, 0:1], rs4[:, h:h + 1], ALU.subtract, ALU.mult)
                tt("pool", ya.v(), yn.v(), rgs[:, c, :], ALU.mult)
                hf = trhalf()
                for wc in range(4):
                    tr(hf[:, wc * 128:(wc + 1) * 128], ya[:, wc * 128:(wc + 1) * 128])
                cp("act", ybT[0][:, :, cs], Buf(hf.ap[:, 0:512].rearrange("p (a b) -> p a b", a=4), hf.key, [128, 4, 128], whole=True).v())

            for cc in range(4):
                cp("pool", lx_ext[:, cc, 0:3], lxtail[:, l, cc, :])
            for cc in range(4):
                cw = lambda j: smalls[:, l, SM_CW + cc * 4 + j: SM_CW + cc * 4 + j + 1]
                ts("dve", e_xc.v(), lx_ext[:, cc, 0:512], cw(0), smalls[:, l, SM_CB + cc:SM_CB + cc + 1], ALU.mult, ALU.add)
                for j in range(1, 4):
                    stt("dve", e_xc.v(), lx_ext[:, cc, j:j + 512], cw(j), e_xc.v(), ALU.mult, ALU.add)
                cp("pool", lxtail[:, l, cc, :], lx_ext[:, cc, 512:515])
                cp("pool", e_xcb.v(), e_xc.v())
                pr = nbank()
                pi = nbank()
                mm(pr.v(), wabd[:, l, 0, cc, :], e_xcb.v(), True, True)
                mm(pi.v(), wabd[:, l, 1, cc, :], e_xcb.v(), True, True)
                act(e_tr.v(), pr.v(), AF.Tanh, bias=lrc[:, l, cc, 2:3], scale=0.5)
                act(e_ti.v(), pi.v(), AF.Tanh, bias=lrc[:, l, cc, 3:4], scale=0.5)
                act(e_a.v(), e_tr.v(), AF.Exp, bias=lrc[:, l, cc, 0:1], scale=lrc[:, l, cc, 0:1])
                act(e_s.v(), e_tr.v(), AF.Exp, bias=lrc[:, l, cc, 1:2], scale=lrc[:, l, cc, 1:2])
                act(e_s.v(), e_s.v(), AF.Sqrt, bias=0.25, scale=-0.25)
                tt("dve", e_u.v(), e_s.v(), e_xc.v(), ALU.mult)
                stt("dve", e_u.v(), e_ti.v(), 1.0, e_u.v(), ALU.add, ALU.mult)
                eh = e_h[cc % 2]
                S.op("dve", lambda hh, eh=eh, cc=cc: hh.tensor_tensor_scan(out=eh.v().ap, data0=e_a.v().ap, data1=e_u.v().ap,
                                                                          initial=hstate[:, l, cc:cc + 1].ap,
                                                                          op0=ALU.mult, op1=ALU.add),
                     reads=[e_a.v(), e_u.v(), hstate[:, l, cc:cc + 1]], writes=[eh.v()])
                cp("dve", hstate[:, l, cc:cc + 1], eh[:, 511:512])
                tt("pool", ybT[1][:, cc, :], eh.v(), gg[:, cc, :], ALU.mult)

            nkb = 4 * (t + 1)
            nch = (t + 4) // 4
            steps = []
            for h in range(4):
                kbi = 0
                for ch in range(nch - 1, -1, -1):
                    t0 = ch * 4
                    ntl = min(t + 1, t0 + 4) - t0
                    for kl in range(ntl * 4 - 1, -1, -1):
                        steps.append((h, ch, t0, ntl, kl, kbi, kl == ntl * 4 - 1))
                        kbi += 1

            def kv_bufs(h, ch):
                return f_k[(h * nch + ch) % 2], f_v[(h * nch + ch) % 2]

            def s1(i):
                h, ch, t0, ntl, kl, kbi, first = steps[i]
                fk, fv = kv_bufs(h, ch)
                if first:
                    r = (l * 4 + h) * NT + t0
                    dma("sp", fk[:, 0:ntl * 512].ap.rearrange("p (a b) -> p a b", a=ntl),
                        kh_d[r:r + ntl].rearrange("a p c -> p a c"), reads=[KH[r:r + ntl, :, :]], writes=[fk[:, 0:ntl * 512]])
                    rb = l * NBT + t0 * 4
                    dma("sp", fv[:, 0:ntl * 4, :].ap, vh_d[rb:rb + ntl * 4, :, h * 128:(h + 1) * 128].rearrange("b p c -> p b c"),
                        reads=[VH[rb:rb + ntl * 4, :, :]], writes=[fv[:, 0:ntl * 4, :]])
                kb = t0 * 4 + kl
                diag = kb >= 4 * t
                a_ = kb - 4 * t
                par = i % 2
                kT = fk[:, kl * 128:(kl + 1) * 128]
                pa = banks[par]
                mm(pa.v(), kT, sq[:, h, :], True, not diag)
                if diag:
                    mm(pa.v(), identb.v(), negm[:, a_, :], False, True)
                act(f_e[par].v(), pa.v(), AF.Exp)
                act(f_lp[par].v(), f_e[par].v(), AF.Ln, bias=1.0)
                if kbi < nkb - 1:
                    if kbi == 0:
                        cp("dve", f_r.v(), f_lp[par].v())
                    else:
                        tt("dve", f_r.v(), f_r.v(), f_lp[par].v(), ALU.add)
                    cp("dve", f_rb[(i + 1) % 3].v(), f_r.v())

            def s2(i):
                h, ch, t0, ntl, kl, kbi, first = steps[i]
                fk, fv = kv_bufs(h, ch)
                kb = t0 * 4 + kl
                diag = kb >= 4 * t
                a_ = kb - 4 * t
                par = i % 2
                kT = fk[:, kl * 128:(kl + 1) * 128]
                pbk = banks[2 + par]
                ob = banks[4 + h % 2]
                mm(pbk.v(), kT, sq[:, h, :], True, False, sig=False)
                lastmm = "tri"
                if kbi > 0:
                    lastmm = "ones"
                if diag:
                    lastmm = "neg"
                mm(pbk.v(), trineg.v(), f_lp[par].v(), False, lastmm == "tri", sig=(lastmm == "tri"))
                if kbi > 0:
                    mm(pbk.v(), negones.v(), f_rb[i % 3].v(), False, lastmm == "ones", sig=(lastmm == "ones"))
                if diag:
                    mm(pbk.v(), identb.v(), negm[:, a_, :], False, True)
                act(f_w[par].v(), pbk.v(), AF.Exp)

            def s3(i):
                h, ch, t0, ntl, kl, kbi, first = steps[i]
                fk, fv = kv_bufs(h, ch)
                par = i % 2
                ob = banks[4 + h % 2]
                mm(ob.v(), fv[:, kl, :], f_w[par].v(), kbi == 0, kbi == nkb - 1)
                if kbi == nkb - 1:
                    cp("dve", ybT[2][:, h, :], ob.v())

            s1(0)
            for i in range(len(steps)):
                if i + 1 < len(steps):
                    s1(i + 1)
                s2(i)
                if i >= 1:
                    s3(i - 1)
            s3(len(steps) - 1)

            dma("pool", g_tab.v().ap, rowtab_in[l, :, 0:1024].rearrange("p (a b) -> p a b", a=2), writes=[g_tab.v()])
            for blk in range(4):
                bs_ = slice(blk * 128, (blk + 1) * 128)
                src = sgv[:, blk, :]
                S.op("dve", lambda hh, src=src: hh.bn_stats(out=g_st.v().ap, in_=src.ap), reads=[src], writes=[g_st.v()])
                S.op("dve", lambda hh: hh.bn_aggr(out=g_mv.v().ap, in_=g_st.v().ap), reads=[g_st.v()], writes=[g_mv.v()])
                ts("dve", g_rs.v(), g_mv[:, 1:2], LN_EPS, None, ALU.add)
                act(g_rs.v(), g_rs.v(), AF.Sqrt)
                S.op("dve", lambda hh: hh.reciprocal(out=g_rs.v().ap, in_=g_rs.v().ap), reads=[g_rs.v()], writes=[g_rs.v()])
                stt("dve", g_vn.v(), src, g_mv[:, 0:1], g_tab[:, 0, :], ALU.subtract, ALU.mult)
                stt("dve", g_vl.v(), g_vn.v(), g_rs.v(), g_tab[:, 1, :], ALU.mult, ALU.add)
                pb = nbank()
                for g in range(4):
                    gs = slice(g * 128, (g + 1) * 128)
                    mm(pb[:, gs], wsT[:, l, g, :], g_vl[:, gs], True, True, sig=(g == 3))
                for g in range(4):
                    gs = slice(g * 128, (g + 1) * 128)
                    stt("dve", g_yd[:, gs], pb[:, gs], smalls[:, l, SM_BS + g:SM_BS + g + 1], su[:, blk, gs], ALU.add, ALU.mult)
                hf = trhalf()
                for wc in range(4):
                    tr(hf[:, wc * 128:(wc + 1) * 128], g_yd[:, wc * 128:(wc + 1) * 128])
                cp("act", ybT[3][:, :, bs_], Buf(hf.ap[:, 0:512].rearrange("p (a b) -> p a b", a=4), hf.key, [128, 4, 128], whole=True).v())

            for dc in range(8):
                wg = wload(l, U_GT + dc)
                if dc % 2 == 0:
                    wbr = wload(l, U_BR + dc // 2)
                dcl = dc % 2
                for b in range(4):
                    pp = nbank()
                    pg = nbank()
                    for wc in range(4):
                        o_ = dcl * 2048 + (b * 4 + wc) * 128
                        mm(pp.v(), wbr[:, o_:o_ + 128], ybT[b][:, wc, :], wc == 0, wc == 3)
                    for kc in range(8):
                        mm(pg.v(), wg[:, kc * 512 + b * 128: kc * 512 + (b + 1) * 128], xT[:, kc, :], kc == 0, kc == 7)
                    gb = h_g[b % 2]
                    act(gb.v(), pg.v(), AF.Sigmoid, bias=bc(32 + dc * 4 + b))
                    if b == 0:
                        tt("dve", h_acc.v(), gb.v(), pp.v(), ALU.mult)
                    else:
                        tmp = h_tmp[b % 2]
                        tt("dve", tmp.v(), gb.v(), pp.v(), ALU.mult)
                        if b < 3:
                            tt("pool", h_acc.v(), h_acc.v(), tmp.v(), ALU.add)
                        else:
                            tt("pool", mergedT[:, dc, :], h_acc.v(), tmp.v(), ALU.add)

            dma("pool", i_tab.v().ap, rowtab_in[l, :, 1024:3072].rearrange("p (a b) -> p a b", a=2), writes=[i_tab.v()])
            wos = [wload(l, U_WO), wload(l, U_WO + 1)]
            for blk in range(4):
                for half in range(2):
                    bk = banks[(blk % 2) * 2 + half]
                    w = wos[half]
                    hsl = slice(half * 512, (half + 1) * 512)
                    for dc in range(8):
                        mm(bk.v(), mergedT[:, dc, blk * 128:(blk + 1) * 128], w[:, dc * 512:(dc + 1) * 512], dc == 0, dc == 7)
                    stt("dve", x1_[:, blk, hsl], xt_[:, blk, hsl], ALPHA, bk.v(), ALU.mult, ALU.add)
                ln_blk(x1_, blk, i_tab[:, 0, :], i_tab[:, 1, :])
                transpose_blk(x1_, blk, xT)

            dma("pool", i_tab.v().ap, rowtab_in[l, :, 3072:5120].rearrange("p (a b) -> p a b", a=2), writes=[i_tab.v()])
            dma("pool", browb.v().ap, browd_d[l], reads=[BROWD[l:l + 1, :, :]], writes=[browb.v()])
            for u in range(8):
                w = wload(l, U_W1 + u)
                for f in range(4):
                    fc = u * 4 + f
                    pb = nbank()
                    for kc in range(8):
                        mm(pb.v(), w[:, kc * 512 + f * 128: kc * 512 + (f + 1) * 128], xT[:, kc, :], kc == 0, kc == 7)
                    jr = j_r[fc % 2]
                    act(jr.v(), pb.v(), AF.Relu, bias=smalls[:, l, SM_B1 + fc:SM_B1 + fc + 1])
                    tt("pool", hT[:, fc, :], jr.v(), jr.v(), ALU.mult)
            for half in range(2):
                for u4 in range(4):
                    w = wload(l, U_W2 + half * 4 + u4)
                    for j in range(8):
                        fc = u4 * 8 + j
                        for blk in range(4):
                            mm(banks[blk].v(), hT[:, fc, blk * 128:(blk + 1) * 128], w[:, j * 512:(j + 1) * 512], fc == 0, False, sig=False)
                for blk in range(4):
                    mm(banks[blk].v(), ones_row.v(), browb[:, 2560 + half * 512: 2560 + (half + 1) * 512], False, True)
                for blk in range(4):
                    hsl = slice(half * 512, (half + 1) * 512)
                    stt("dve", xt_[:, blk, hsl], x1_[:, blk, hsl], ALPHA, banks[blk].v(), ALU.mult, ALU.add)
            ln_tile(xt_, i_tab[:, 0, :], i_tab[:, 1, :])
            if last:
                dma("sp", y_out[t * T:(t + 1) * T, :].rearrange("(b p) c -> p b c", p=128), xt_.v().ap, reads=[xt_.v()])

        for t in range(NT):
            dma("sp", XA.v().ap, x_in[t * T:(t + 1) * T, :].rearrange("(b p) c -> p b c", p=128), writes=[XA.v()])
            for l in range(NL):
                tile_layer(t, l, l == NL - 1)

        S.emit({"pe": block.tensor, "act": block.scalar, "dve": block.vector, "pool": block.gpsimd, "sp": block.sync},
               csem, dsem)
    return nc, S


def _unit_cols(Wc):
    return np.ascontiguousarray(Wc.reshape(8, 128, 512).transpose(1, 0, 2)).reshape(128, 4096)


def _swap_half(a):
    s = a.reshape(a.shape[:-1] + (4, 2, 64))
    return np.ascontiguousarray(s[..., ::-1, :]).reshape(a.shape)


def pack_layer(inp, l):
    w_in = inp["w_in"][l]
    b_in = inp["b_in"][l]
    sec = lambda i, n=512: slice(i, i + n)
    RQ, RK, RV, RG, LX, LG, SQ, SK, SV, SU, SGV, GT = 0, 512, 1024, 1536, 2048, 2560, 3072, 3584, 4096, 4608, 5120, 5632
    units = []
    for c0 in (RV, RG, SV, SU, SGV):
        units.append(_unit_cols(w_in[:, sec(c0)]))
    units.append(_unit_cols(w_in[:, sec(RQ)]))
    units.append(_unit_cols(_swap_half(w_in[:, sec(RQ)])))
    units.append(_unit_cols(w_in[:, sec(RK)]))
    units.append(_unit_cols(_swap_half(w_in[:, sec(RK)])))
    for c0 in (LX, LG, SQ, SK):
        units.append(_unit_cols(w_in[:, sec(c0)]))
    for dc in range(8):
        cols = np.concatenate([w_in[:, GT + b * 1024 + dc * 128: GT + b * 1024 + (dc + 1) * 128] for b in range(4)], axis=1)
        units.append(_unit_cols(cols))
    wb = inp["w_branch"][l].reshape(4, 4, 128, 8, 128).transpose(2, 3, 0, 1, 4).reshape(128, 8, 16, 128)
    for j in range(4):
        units.append(np.ascontiguousarray(wb[:, 2 * j:2 * j + 2]).reshape(128, 4096))
    for half in range(2):
        units.append(_unit_cols(inp["w_out"][l][:, half * 512:(half + 1) * 512]))
    for u in range(8):
        units.append(_unit_cols(inp["w1"][l][:, u * 512:(u + 1) * 512]))
    w2 = inp["w2"][l]
    for half in range(2):
        wh = w2[:, half * 512:(half + 1) * 512].reshape(32, 128, 512)
        for u4 in range(4):
            units.append(np.ascontiguousarray(wh[8 * u4:8 * u4 + 8].transpose(1, 0, 2)).reshape(128, 4096))
    assert len(units) == NUNIT
    wp = np.stack(units, 0).astype(np.float32)
    sm = np.zeros((128, NSM), np.float32)
    colv = lambda v: np.ascontiguousarray(v.reshape(-1, 128).T)
    fm_b = [b_in[sec(RQ)], _swap_half(b_in[sec(RQ)]), b_in[sec(RK)], _swap_half(b_in[sec(RK)]),
            b_in[sec(LX)], b_in[sec(LG)], b_in[sec(SQ)], b_in[sec(SK)]]
    sm[:, SM_BC:SM_BC + 32] = np.concatenate([colv(v) for v in fm_b], axis=1)
    gcols = []
    for dc in range(8):
        for b in range(4):
            gcols.append(b_in[GT + b * 1024 + dc * 128: GT + b * 1024 + (dc + 1) * 128][:, None])
    sm[:, SM_BC + 32:SM_BC + 64] = np.concatenate(gcols, axis=1)
    cw = inp["conv_w"][l]
    sm[:, SM_CW:SM_CW + 16] = cw.reshape(4, 4, 128).transpose(2, 1, 0).reshape(128, 16)
    sm[:, SM_CB:SM_CB + 4] = colv(inp["conv_b"][l])
    sm[:, SM_BA:SM_BA + 4] = colv(inp["lru_ba"][l])
    sm[:, SM_BX:SM_BX + 4] = colv(inp["lru_bx"][l])
    sm[:, SM_LAM:SM_LAM + 4] = colv(inp["lru_lambda"][l])
    sm[:, SM_BS:SM_BS + 4] = inp["sg_bs"][l].T
    sm[:, SM_B1:SM_B1 + 32] = colv(inp["b1"][l])
    rep = lambda v: np.broadcast_to(v[None, :], (128, v.shape[0]))
    rowtab = np.concatenate([rep(inp["sg_ln_g"][l]), rep(inp["sg_ln_b"][l]), rep(inp["ln1_g"][l]), rep(inp["ln1_b"][l]),
                             rep(inp["ln2_g"][l]), rep(inp["ln2_b"][l])], axis=1).astype(np.float32)
    wabd = np.zeros((128, 2, 4, 128), np.float32)
    for k, nm in enumerate(("lru_wa", "lru_wx")):
        wm = inp[nm][l]
        for cc in range(4):
            wabd[0:64, k, cc, 0:64] = wm[2 * cc]
            wabd[64:128, k, cc, 64:128] = wm[2 * cc + 1]
    sgws = np.ascontiguousarray(inp["sg_ws"][l].transpose(1, 0, 2)).reshape(128, 512)
    brow = np.concatenate([b_in[sec(RV)], b_in[sec(RG)], b_in[sec(SV)], b_in[sec(SU)], b_in[sec(SGV)], inp["b2"][l]])[None, :]
    return wp, sm, rowtab, wabd.reshape(128, 1024), sgws, brow.astype(np.float32)


def make_consts(S_LEN):
    c = np.zeros((128, NCONST), np.float32)
    idx = np.arange(128)
    c[:, C_ID:C_ID + 128] = np.eye(128)
    c[:, C_TRI:C_TRI + 128] = -(idx[:, None] >= idx[None, :]).astype(np.float32)
    c[:, C_NONE:C_NONE + 128] = -1.0
    for a in range(4):
        key = a * 128 + idx[:, None]
        qry = np.arange(512)[None, :]
        c[:, C_NEGM + a * 512:C_NEGM + (a + 1) * 512] = np.where(key >= qry, NEG, 0.0)
    log_g = np.log1p(-(2.0 ** (-5.0 - np.arange(4, dtype=np.float64))))
    for h in range(4):
        diff = idx[None, :] - idx[:, None]
        c[:, C_DT + h * 128:C_DT + (h + 1) * 128] = np.where(diff >= 0, np.exp(log_g[h] * np.maximum(diff, 0)), 0.0)
        c[:, C_QD + h * 128:C_QD + (h + 1) * 128] = np.exp(log_g[h] * (idx + 1.0))[None, :]
        c[:, C_KDT + h * 128:C_KDT + (h + 1) * 128] = np.exp(log_g[h] * (127.0 - idx))[:, None]
    c[:, C_TRIL:C_TRIL + 128] = (idx[None, :] <= idx[:, None]).astype(np.float32)
    inv_freq = (np.float32(10000.0) ** (-np.arange(64, dtype=np.float32) / np.float32(64))).astype(np.float32)
    pos = np.arange(S_LEN, dtype=np.float32)
    ang = (pos[None, :] * inv_freq[:, None]).astype(np.float32).astype(np.float64)
    cos = np.cos(ang)
    sin = np.sin(ang)
    C = np.concatenate([cos, cos], 0)
    Sg = np.concatenate([-sin, sin], 0)
    ks = 128.0 ** -0.5
    rot = np.stack([C, Sg, C * ks, Sg * ks], 0).astype(np.float32)
    return c, rot


_CACHE = {}


def run_model(inputs, S_LEN, NL, n_cores, batch_of_core):
    key = (S_LEN, NL)
    if key not in _CACHE:
        _CACHE[key] = build_program(S_LEN, NL)
    nc, _ = _CACHE[key]
    inp = {k: np.asarray(v) for k, v in inputs.items()}
    packs = [pack_layer(inp, l) for l in range(NL)]
    wpack = np.concatenate([p[0].reshape(NUNIT * 256, 2048) for p in packs], 0)
    smalls = np.stack([p[1] for p in packs], 0)
    rowtab = np.stack([p[2] for p in packs], 0)
    wabd = np.stack([p[3] for p in packs], 0)
    sgws = np.stack([p[4] for p in packs], 0)
    brow = np.stack([p[5] for p in packs], 0)
    consts, rot = make_consts(S_LEN)
    shared = {"wpack": wpack, "smalls": smalls, "rowtab": rowtab, "wabd": wabd, "sgws": sgws,
              "consts": consts, "rot": rot, "brow": brow}
    in_maps = []
    for c in range(n_cores):
        m = dict(shared)
        m["x"] = np.ascontiguousarray(inp["x"][batch_of_core[c]], dtype=np.float32)
        in_maps.append(m)
    res = run_bass_kernel_spmd(nc, in_maps, core_ids=list(range(n_cores)))
    return [r["y"] for r in res.results]


def kernel(**inputs):
    x = np.asarray(inputs["x"])
    B, S_LEN, _ = x.shape
    boc = [c % B for c in range(8)]
    outs = run_model(inputs, S_LEN, DEPTH, 8, boc)
    return np.stack([outs[b] for b in range(B)], 0).astype(np.float32)
```

```python
import math
from contextlib import ExitStack

import numpy as np
import concourse.bass as bass
import concourse.mybir as mybir
from concourse.bass_utils import run_bass_kernel_spmd

F32 = mybir.dt.float32
BF16 = mybir.dt.bfloat16
AF = mybir.ActivationFunctionType
ALU = mybir.AluOpType

D_MODEL = 1024
BW = 512
DFF = 4096
DEPTH = 2
ALPHA = (2 * DEPTH) ** 0.25
LN_EPS = 1e-5
T = 512
NUNIT = 43
U_TM, U_FM, U_GT, U_BR, U_WO, U_W1, U_W2 = 0, 5, 13, 21, 25, 27, 35
NSM = 132
SM_BC, SM_CW, SM_CB, SM_BA, SM_BX, SM_LAM, SM_BS, SM_B1 = 0, 64, 80, 84, 88, 92, 96, 100
NCONST = 4096
C_ID, C_TRI, C_NONE, C_NEGM, C_DT, C_QD, C_KDT, C_TRIL = 0, 128, 256, 384, 2432, 2944, 3456, 3968
NEG = -30000.0


class View:
    __slots__ = ("ap", "key", "lo", "hi")

    def __init__(self, ap, key, lo, hi):
        self.ap, self.key, self.lo, self.hi = ap, key, lo, hi


class Buf:
    def __init__(self, ap, key, shape, part=True, base=0, esz=1, whole=False):
        self.whole = whole
        self.ap = ap
        self.key = key
        self.shape = list(shape)
        self.part = part
        self.base = base
        self.esz = esz
        free = self.shape[1:] if part else self.shape
        st = []
        s = 1
        for d in reversed(free):
            st.append(s)
            s *= d
        self.strides = list(reversed(st))

    def __getitem__(self, idx):
        if not isinstance(idx, tuple):
            idx = (idx,)
        idx = list(idx) + [slice(None)] * (len(self.shape) - len(idx))
        ap = self.ap[tuple(idx)]
        fi = idx[1:] if self.part else idx
        fs = self.shape[1:] if self.part else self.shape
        lo = 0
        hi = 0
        for ix, st, d in zip(fi, self.strides, fs):
            if isinstance(ix, int):
                a, b = ix, ix + 1
            else:
                a = 0 if ix.start is None else ix.start
                b = d if ix.stop is None else ix.stop
            lo += a * st
            hi += (b - 1) * st
        if self.whole:
            return View(ap, self.key, 0, 1)
        return View(ap, self.key, self.base + lo * self.esz, self.base + (hi + 1) * self.esz)

    def v(self):
        return self[tuple(slice(None) for _ in self.shape)]


class _Op:
    __slots__ = ("eng", "idx", "fn", "sig", "dma", "deps", "seq", "dman")


DMA_RING = 8
ENGS = ("pe", "act", "dve", "pool", "sp")


class Sched:
    def __init__(self):
        self.ops = {e: [] for e in ENGS}
        self.reg = {}
        self.seq = 0
        self.ndma = {e: 0 for e in ENGS}

    def op(self, eng, fn, reads=(), writes=(), sig=True, dma=False):
        o = _Op()
        o.eng, o.fn, o.sig, o.dma = eng, fn, sig, dma
        o.idx = len(self.ops[eng])
        o.seq = self.seq
        self.seq += 1
        o.dman = -1
        deps = {}
        if dma:
            o.dman = self.ndma[eng]
            self.ndma[eng] += 1
            o.sig = True
        for v in reads:
            self._read(v, o, deps)
        for v in writes:
            self._write(v, o, deps)
        out = []
        for p, raw in deps.items():
            if p is o:
                continue
            if p.eng == eng and not p.dma and not dma:
                if eng == "pe":
                    continue
            out.append(p)
        o.deps = out
        self.ops[eng].append(o)
        return o

    def _read(self, v, o, deps):
        recs = self.reg.setdefault(v.key, {})
        for (lo, hi), r in recs.items():
            if lo < v.hi and v.lo < hi and r[0] is not None:
                deps[r[0]] = True
        r = recs.get((v.lo, v.hi))
        if r is None:
            r = [None, {}, []]
            recs[(v.lo, v.hi)] = r
        if o.dma:
            r[2].append(o)
        else:
            r[1][o.eng] = o

    def _write(self, v, o, deps):
        recs = self.reg.setdefault(v.key, {})
        dead = []
        for (lo, hi), r in recs.items():
            if lo < v.hi and v.lo < hi:
                if r[0] is not None:
                    deps.setdefault(r[0], False)
                for p in r[1].values():
                    deps.setdefault(p, False)
                for p in r[2]:
                    deps.setdefault(p, False)
                if v.lo <= lo and hi <= v.hi:
                    dead.append((lo, hi))
        for k in dead:
            del recs[k]
        recs[(v.lo, v.hi)] = [o, {}, []]

    def emit(self, block_fns, csem, dsem):
        allops = sorted((o for e in ENGS for o in self.ops[e]), key=lambda o: o.seq)
        for o in allops:
            for p in o.deps:
                if p.dma or p.sig:
                    continue
                lst = self.ops[p.eng]
                k = p.idx
                while k < len(lst) and (lst[k].dma or not lst[k].sig):
                    k += 1
                if k >= len(lst) or lst[k].seq >= o.seq:
                    p.sig = True
        sigcount = {}
        nextsig = {}
        for e in ENGS:
            ops = self.ops[e]
            comp = [o for o in ops if not o.dma]
            if comp and not comp[-1].sig:
                comp[-1].sig = True
            c = 0
            sc = {}
            for o in ops:
                if not o.dma and o.sig:
                    c += 1
                sc[o.idx] = c
            sigcount[e] = sc
            ns = {}
            nxt = None
            for o in reversed(ops):
                if not o.dma and o.sig:
                    nxt = o
                ns[o.idx] = nxt
            nextsig[e] = ns

        def event(p, consumer):
            if p.dma:
                n = p.dman
                return dsem[p.eng][n % DMA_RING], 16 * (n // DMA_RING + 1)
            j = nextsig[p.eng][p.idx]
            assert j is not None
            assert j is p or j.seq < consumer.seq, "signal recorded after its consumer"
            return csem[p.eng], sigcount[p.eng][j.idx]

        def build(e):
            def body(h):
                waited = {}
                for o in self.ops[e]:
                    need = {}
                    for p in o.deps:
                        s, val = event(p, o)
                        k = id(s)
                        if need.get(k, (None, 0))[1] < val:
                            need[k] = (s, val)
                    if o.dma and o.dman >= DMA_RING:
                        n = o.dman - DMA_RING
                        s = dsem[e][n % DMA_RING]
                        val = 16 * (n // DMA_RING + 1)
                        k = id(s)
                        if need.get(k, (None, 0))[1] < val:
                            need[k] = (s, val)
                    for k, (s, val) in need.items():
                        if waited.get(k, 0) < val:
                            h.wait_ge(s, val)
                            waited[k] = val
                    inst = o.fn(h)
                    if o.dma:
                        inst.then_inc(dsem[e][o.dman % DMA_RING], 16)
                    elif o.sig:
                        inst.then_inc(csem[e], 1)
                nd = self.ndma[e]
                for n in range(max(0, nd - DMA_RING), nd):
                    s = dsem[e][n % DMA_RING]
                    val = 16 * (n // DMA_RING + 1)
                    if waited.get(id(s), 0) < val:
                        h.wait_ge(s, val)
                        waited[id(s)] = val
            return body

        for e in ENGS:
            if self.ops[e]:
                block_fns[e](build(e))


ARENA_BYTES = 192 * 1024


def build_program(S_LEN, NL):
    NT = S_LEN // T
    NBT = S_LEN // 128
    nc = bass.Bass("TRN2", target_bir_lowering=False)
    x_in = nc.dram_tensor("x", [S_LEN, D_MODEL], F32, kind="ExternalInput").ap()
    NWROWS = NL * NUNIT * 256
    wpack = nc.dram_tensor("wpack", [NWROWS, 2048], F32, kind="ExternalInput").ap()
    smalls_in = nc.dram_tensor("smalls", [NL, 128, NSM], F32, kind="ExternalInput").ap()
    rowtab_in = nc.dram_tensor("rowtab", [NL, 128, 5120], F32, kind="ExternalInput").ap()
    wabd_in = nc.dram_tensor("wabd", [NL, 128, 1024], F32, kind="ExternalInput").ap()
    sgws_in = nc.dram_tensor("sgws", [NL, 128, 512], F32, kind="ExternalInput").ap()
    consts_in = nc.dram_tensor("consts", [128, NCONST], F32, kind="ExternalInput").ap()
    rot_in = nc.dram_tensor("rot", [4, 128, S_LEN], F32, kind="ExternalInput").ap()
    brow_in = nc.dram_tensor("brow", [NL, 1, 3584], F32, kind="ExternalInput").ap()
    y_out = nc.dram_tensor("y", [S_LEN, D_MODEL], F32, kind="ExternalOutput").ap()
    ws_d = nc.dram_tensor("ws", [NWROWS, 2048], BF16, kind="Internal").ap()
    kh_d = nc.dram_tensor("kh", [NL * 4 * NT, 128, 512], BF16, kind="Internal").ap()
    vh_d = nc.dram_tensor("vh", [NL * NBT, 128, 512], BF16, kind="Internal").ap()
    browd_d = nc.dram_tensor("browd", [NL, 1, 3584], BF16, kind="Internal").ap()

    S = Sched()
    es = ExitStack()
    with es:
        art = es.enter_context(nc.sbuf_tensor("arena", [128, ARENA_BYTES // 2], BF16))
        banks = []
        for i in range(6):
            pt_ = es.enter_context(nc.psum_tensor(f"pb{i}", [128, 512], F32))
            banks.append(Buf(pt_, f"pb{i}", [128, 512], whole=True))
        PT = []
        for i in range(2):
            ptt_ = es.enter_context(nc.psum_tensor(f"ptr{i}", [128, 1024], BF16))
            PT.append(Buf(ptt_, f"ptr{i}", [128, 1024], whole=True))
        csem = {e: es.enter_context(nc.semaphore("c_" + e)) for e in ENGS}
        dsem = {e: [es.enter_context(nc.semaphore(f"d_{e}{i}")) for i in range(DMA_RING)] for e in ENGS}
        block = es.enter_context(nc.Block())

        WS = Buf(ws_d, "ws", [NWROWS, 2048], part=False)
        KH = Buf(kh_d, "kh", [NL * 4 * NT, 128, 512], part=False)
        VH = Buf(vh_d, "vh", [NL * NBT, 128, 512], part=False)
        BROWD = Buf(browd_d, "browd", [NL, 1, 3584], part=False)

        def abuf(off, shape, dt, parts=128):
            esz = 4 if dt == F32 else 2
            n = 1
            for d in shape:
                n *= d
            assert off % 4 == 0 and off + n * esz <= ARENA_BYTES, (off, shape)
            ap = art[0:parts, off // 2: off // 2 + n * esz // 2]
            if dt == F32:
                ap = ap.bitcast(F32)
            if len(shape) > 1:
                names = "abcdefg"[:len(shape)]
                pat = "p (" + " ".join(names) + ") -> p " + " ".join(names)
                ap = ap.rearrange(pat, **{n_: d_ for n_, d_ in zip(names[:-1], shape[:-1])})
            return Buf(ap, "AR", [parts] + list(shape), base=off, esz=esz)

        cur = [0]

        def palloc(shape, dt, parts=128):
            esz = 4 if dt == F32 else 2
            n = 1
            for d in shape:
                n *= d
            b = abuf(cur[0], shape, dt, parts)
            cur[0] += (n * esz + 31) // 32 * 32
            return b

        XA = palloc([4, 1024], F32)
        XB = palloc([4, 1024], F32)
        WR = [palloc([4096], BF16) for _ in range(3)]
        identb = palloc([128], BF16)
        trineg = palloc([128], BF16)
        negones = palloc([128], BF16)
        negm = palloc([4, 512], BF16)
        DTt = palloc([512], F32)
        QDt = palloc([4, 128], F32)
        KDTt = palloc([512], F32)
        tril = palloc([128], F32)
        smalls = palloc([NL, NSM], F32)
        lrc = palloc([NL, 4, 4], F32)
        wabd = palloc([NL, 2, 4, 128], BF16)
        wsT = palloc([NL, 4, 128], BF16)
        ones_row = palloc([128], BF16, parts=1)
        st = palloc([NL, 4, 128], F32)
        stbf = palloc([NL, 4, 128], BF16)
        hstate = palloc([NL, 4], F32)
        lxtail = palloc([NL, 4, 3], F32)
        SCR = cur[0]

        def sbuf(off, shape, dt, parts=128):
            return abuf(SCR + off, shape, dt, parts)

        xT = sbuf(0, [8, 512], BF16)
        xb = sbuf(8192, [4, 1024], BF16)
        ybT = [sbuf(16384 + 4096 * b, [4, 512], BF16) for b in range(4)]
        su = sbuf(32768, [4, 512], F32)
        sgv = sbuf(40960, [4, 512], F32)
        sq = sbuf(49152, [4, 512], BF16)
        lx_ext = sbuf(53248, [4, 516], F32)
        gg = sbuf(61504, [4, 512], F32)
        rv = sbuf(69696, [4, 512], BF16)
        rgs = sbuf(73792, [4, 512], F32)
        rq = sbuf(81984, [4, 512], BF16)
        rk = sbuf(86080, [4, 512], BF16)
        LOC_D = 90176
        rot_t = [sbuf(8192 + 2048 * i, [512], F32) for i in range(4)]
        rtmp = [sbuf(16384 + 2048 * i, [512], F32) for i in range(4)]
        sv = sbuf(24576, [4, 512], BF16)
        sk = sbuf(28672, [4, 512], BF16)
        bslot = [sbuf(LOC_D + 17600 + 1024 * i, [512], BF16, parts=1) for i in range(2)]
        qd = sbuf(LOC_D, [4, 512], BF16)
        rkt = sbuf(LOC_D + 4096, [4, 512], BF16)
        vdec = sbuf(LOC_D + 8192, [4, 512], BF16)
        sd_ = [sbuf(LOC_D + 12288 + 1024 * i, [4, 128], BF16) for i in range(2)]
        yn = sbuf(LOC_D + 14336, [512], F32)
        ya = sbuf(LOC_D + 16384, [512], BF16)
        stats = sbuf(LOC_D + 17408, [4, 6], F32)
        mv = sbuf(LOC_D + 17408 + 96, [4, 2], F32)
        rs4 = sbuf(LOC_D + 17408 + 128, [4], F32)
        END_D = LOC_D + 17408 + 160
        LOC_E = 69696
        e_xc = sbuf(LOC_E, [512], F32)
        e_xcb = sbuf(LOC_E + 2048, [512], BF16)
        e_tr = sbuf(LOC_E + 3072, [512], F32)
        e_ti = sbuf(LOC_E + 5120, [512], F32)
        e_a = sbuf(LOC_E + 7168, [512], F32)
        e_s = sbuf(LOC_E + 9216, [512], F32)
        e_u = sbuf(LOC_E + 11264, [512], F32)
        e_h = [sbuf(LOC_E + 13312 + 2048 * i, [512], F32) for i in range(2)]
        LOC_F = 53248
        f_k = [sbuf(LOC_F + 4096 * i, [2048], BF16) for i in range(2)]
        f_v = [sbuf(LOC_F + 8192 + 4096 * i, [16, 128], BF16) for i in range(2)]
        f_e = [sbuf(LOC_F + 16384 + 2048 * i, [512], F32) for i in range(2)]
        f_lp = [sbuf(LOC_F + 20480 + 1024 * i, [512], BF16) for i in range(2)]
        f_w = [sbuf(LOC_F + 22528 + 1024 * i, [512], BF16) for i in range(2)]
        f_r = sbuf(LOC_F + 24576, [512], F32)
        f_rb = [sbuf(LOC_F + 26624 + 1024 * i, [512], BF16) for i in range(3)]
        g_tab = sbuf(8192, [2, 512], F32)
        g_vn = sbuf(12288, [512], F32)
        g_vl = sbuf(14336, [512], BF16)
        g_yd = sbuf(15360, [512], BF16)
        g_st = sbuf(LOC_D + 19648, [6], F32)
        g_mv = sbuf(LOC_D + 19648 + 32, [2], F32)
        g_rs = sbuf(LOC_D + 19648 + 64, [1], F32)
        mergedT = sbuf(32768, [8, 512], BF16)
        h_g = [sbuf(40960 + 2048 * i, [512], F32) for i in range(2)]
        h_acc = sbuf(40960 + 4096, [512], F32)
        h_tmp = [sbuf(40960 + 6144 + 2048 * i, [512], F32) for i in range(2)]
        LOC_I = 53248
        i_tab = sbuf(LOC_I, [2, 1024], F32)
        i_st = sbuf(LOC_I + 8192, [4, 2, 6], F32)
        i_mv = sbuf(LOC_I + 8192 + 192, [4, 2], F32)
        i_rs = sbuf(LOC_I + 8192 + 224, [4], F32)
        hT = sbuf(16384, [32, 512], BF16)
        j_r = [sbuf(49152 + 2048 * i, [512], F32) for i in range(2)]
        assert LOC_D + 19648 + 96 <= ARENA_BYTES - SCR, (END_D, ARENA_BYTES - SCR)

        def vw(x):
            return x.v() if isinstance(x, Buf) else x

        def mm(out, lhsT, rhs, start, stop, sig=None):
            S.op("pe", lambda h: h.matmul(out.ap, lhsT=lhsT.ap, rhs=rhs.ap, start=start, stop=stop),
                 reads=[lhsT, rhs], writes=[out], sig=(stop if sig is None else sig))

        def tr(out, in_):
            idv = identb.v()
            S.op("pe", lambda h: h.transpose(out=out.ap, in_=in_.ap, identity=idv.ap), reads=[in_, idv], writes=[out])

        def act(out, in_, func, bias=None, scale=None):
            reads = [in_]
            kw = {}
            if bias is not None:
                if isinstance(bias, View):
                    reads.append(bias)
                    kw["bias"] = bias.ap
                else:
                    kw["bias"] = float(bias)
            if scale is not None:
                if isinstance(scale, View):
                    reads.append(scale)
                    kw["scale"] = scale.ap
                else:
                    kw["scale"] = float(scale)
            S.op("act", lambda h: h.activation(out=out.ap, in_=in_.ap, func=func, **kw), reads=reads, writes=[out])

        def tt(eng, out, a, b, op):
            S.op(eng, lambda h: h.tensor_tensor(out=out.ap, in0=a.ap, in1=b.ap, op=op), reads=[a, b], writes=[out])

        def ts(eng, out, a, s1, s2, op0, op1=None):
            reads = [a]
            k1 = s1.ap if isinstance(s1, View) else float(s1)
            if isinstance(s1, View):
                reads.append(s1)
            if op1 is None:
                S.op(eng, lambda h: h.tensor_scalar(out=out.ap, in0=a.ap, scalar1=k1, scalar2=None, op0=op0),
                     reads=reads, writes=[out])
                return
            k2 = s2.ap if isinstance(s2, View) else float(s2)
            if isinstance(s2, View):
                reads.append(s2)
            S.op(eng, lambda h: h.tensor_scalar(out=out.ap, in0=a.ap, scalar1=k1, scalar2=k2, op0=op0, op1=op1),
                 reads=reads, writes=[out])

        def stt(eng, out, in0, sc, in1, op0, op1):
            reads = [in0, in1]
            k = sc.ap if isinstance(sc, View) else float(sc)
            if isinstance(sc, View):
                reads.append(sc)
            S.op(eng, lambda h: h.scalar_tensor_tensor(out=out.ap, in0=in0.ap, scalar=k, in1=in1.ap, op0=op0, op1=op1),
                 reads=reads, writes=[out])

        def cp(eng, out, in_):
            if eng == "act":
                S.op("act", lambda h: h.copy(out=out.ap, in_=in_.ap), reads=[in_], writes=[out])
            else:
                S.op(eng, lambda h: h.tensor_copy(out=out.ap, in_=in_.ap), reads=[in_], writes=[out])

        _tt0, _cp0 = tt, cp

        def tt(eng, out, a, b, op):
            _tt0(eng, out, a, b, op)
            if eng == "pool":
                issue_cast()

        def cp(eng, out, in_):
            _cp0(eng, out, in_)
            if eng == "pool":
                issue_cast()

        def mset(eng, out, val):
            S.op(eng, lambda h: h.memset(out.ap, val), writes=[out])

        def dma(eng, out_ap, in_ap, reads=(), writes=(), **kw):
            S.op(eng, lambda h: h.dma_start(out=out_ap, in_=in_ap, **kw), reads=reads, writes=writes, dma=True)

        bank_rr = [0]

        def nbank():
            b = banks[bank_rr[0] % 6]
            bank_rr[0] += 1
            return b

        def nbank3():
            b = banks[bank_rr[0] % 3]
            bank_rr[0] += 1
            return b

        bs_rr = [0]

        def bias_row(l, c0):
            b = bslot[bs_rr[0] % 2]
            bs_rr[0] += 1
            dma("pool", b.v().ap, browd_d[l][:, c0:c0 + 512], reads=[BROWD[l:l + 1, :, :]], writes=[b.v()])
            return b

        wr_rr = [0]
        cast_upto = [0]
        pending_casts = []

        def wload(l, u):
            w = WR[wr_rr[0] % 3]
            wr_rr[0] += 1
            r0 = (l * NUNIT + u) * 256
            while cast_upto[0] < r0 + 256:
                issue_cast()
            src = WS[r0:r0 + 256, :]
            dma("sp", w.v().ap, ws_d[r0:r0 + 256, :].rearrange("(p a) c -> p (a c)", a=2), reads=[src], writes=[w.v()])
            return w

        tr_rr = [0]

        def trhalf():
            hf = tr_rr[0] % 2
            tr_rr[0] += 1
            return PT[hf]

        csl = lambda a, n: consts_in[:, a:a + n]
        dma("pool", identb.v().ap, csl(C_ID, 128), writes=[identb.v()])
        dma("pool", trineg.v().ap, csl(C_TRI, 128), writes=[trineg.v()])
        dma("pool", negones.v().ap, csl(C_NONE, 128), writes=[negones.v()])
        dma("pool", negm.v().ap, csl(C_NEGM, 2048).rearrange("p (a b) -> p a b", a=4), writes=[negm.v()])
        dma("sp", DTt.v().ap, csl(C_DT, 512), writes=[DTt.v()])
        dma("sp", QDt.v().ap, csl(C_QD, 512).rearrange("p (a b) -> p a b", a=4), writes=[QDt.v()])
        dma("sp", KDTt.v().ap, csl(C_KDT, 512), writes=[KDTt.v()])
        dma("sp", tril.v().ap, csl(C_TRIL, 128), writes=[tril.v()])
        dma("sp", smalls.v().ap, smalls_in.rearrange("l p c -> p l c"), writes=[smalls.v()])
        dma("pool", wabd.v().ap, wabd_in.rearrange("l p (a b c) -> p l a b c", a=2, b=4), writes=[wabd.v()])
        for l in range(NL):
            dma("pool", browd_d[l], brow_in[l], writes=[BROWD[l:l + 1, :, :]])
        CH = 512
        for r0 in range(0, NWROWS, CH):
            pending_casts.append((r0, min(NWROWS, r0 + CH)))

        def issue_cast():
            if pending_casts:
                r0, r1 = pending_casts.pop(0)
                cast_upto[0] = r1
                dst = WS[r0:r1, :]
                dma("pool", dst.ap, wpack[r0:r1, :], writes=[dst])

        for _ in range(DMA_RING - 2):
            issue_cast()
        mset("dve", ones_row.v(), 1.0)
        mset("dve", st.v(), 0.0)
        mset("dve", stbf.v(), 0.0)
        mset("dve", hstate.v(), 0.0)
        mset("dve", lxtail.v(), 0.0)
        for l in range(NL):
            lam = smalls[:, l, SM_LAM:SM_LAM + 4]
            act(lrc[:, l, :, 0], lam, AF.Exp, scale=-1.0)
            act(lrc[:, l, :, 0], lrc[:, l, :, 0], AF.Ln, bias=1.0)
            ts("dve", lrc[:, l, :, 1], lrc[:, l, :, 0], -8.0, None, ALU.mult)
            ts("dve", lrc[:, l, :, 0], lrc[:, l, :, 0], -4.0, None, ALU.mult)
            ts("dve", lrc[:, l, :, 2], smalls[:, l, SM_BA:SM_BA + 4], 0.5, None, ALU.mult)
            ts("dve", lrc[:, l, :, 3], smalls[:, l, SM_BX:SM_BX + 4], 0.5, None, ALU.mult)
            wtmp = sbuf(0, [4, 128], F32)
            wtb = sbuf(2048, [4, 128], BF16)
            dma("sp", wtmp.v().ap, sgws_in[l].rearrange("p (a b) -> p a b", a=4), writes=[wtmp.v()])
            for g in range(4):
                tt("dve", wtb[:, g, :], wtmp[:, g, :], tril.v(), ALU.mult)
            for g in range(4):
                tr(PT[0][:, g * 128:(g + 1) * 128], wtb[:, g, :])
            cp("dve", wsT[:, l, :, :], Buf(PT[0].ap[:, 0:512].rearrange("p (a b) -> p a b", a=4), PT[0].key, [128, 4, 128], whole=True).v())

        def ln_tile(xbuf, gtab, btab):
            for blk in range(4):
                for c in range(2):
                    sc = xbuf[:, blk, c * 512:(c + 1) * 512]
                    S.op("dve", lambda h, c=c, sc=sc, blk=blk: h.bn_stats(out=i_st[:, blk, c, :].ap, in_=sc.ap),
                         reads=[sc], writes=[i_st[:, blk, c, :]])
            for blk in range(4):
                S.op("dve", lambda h, blk=blk: h.bn_aggr(out=i_mv[:, blk, :].ap, in_=i_st[:, blk, :, :].ap),
                     reads=[i_st[:, blk, :, :]], writes=[i_mv[:, blk, :]])
            ts("dve", i_rs.v(), i_mv[:, :, 1], LN_EPS, None, ALU.add)
            act(i_rs.v(), i_rs.v(), AF.Sqrt)
            S.op("dve", lambda h: h.reciprocal(out=i_rs.v().ap, in_=i_rs.v().ap), reads=[i_rs.v()], writes=[i_rs.v()])
            for blk in range(4):
                src = xbuf[:, blk, :]
                stt("dve", src, src, i_mv[:, blk, 0:1], gtab, ALU.subtract, ALU.mult)
                stt("dve", src, src, i_rs[:, blk:blk + 1], btab, ALU.mult, ALU.add)

        def ln_blk(xbuf, blk, gtab, btab):
            for c in range(2):
                sc = xbuf[:, blk, c * 512:(c + 1) * 512]
                S.op("dve", lambda h, c=c, sc=sc: h.bn_stats(out=i_st[:, blk, c, :].ap, in_=sc.ap),
                     reads=[sc], writes=[i_st[:, blk, c, :]])
            S.op("dve", lambda h: h.bn_aggr(out=i_mv[:, blk, :].ap, in_=i_st[:, blk, :, :].ap),
                 reads=[i_st[:, blk, :, :]], writes=[i_mv[:, blk, :]])
            rsv = i_rs[:, blk:blk + 1]
            ts("dve", rsv, i_mv[:, blk, 1:2], LN_EPS, None, ALU.add)
            act(rsv, rsv, AF.Sqrt)
            S.op("dve", lambda h: h.reciprocal(out=rsv.ap, in_=rsv.ap), reads=[rsv], writes=[rsv])
            src = xbuf[:, blk, :]
            stt("dve", src, src, i_mv[:, blk, 0:1], gtab, ALU.subtract, ALU.mult)
            stt("dve", src, src, rsv, btab, ALU.mult, ALU.add)

        def transpose_blk(src_f32, blk, dst):
            cp("pool", xb[:, blk, :], src_f32[:, blk, :])
            hf = trhalf()
            for dc in range(8):
                tr(hf[:, dc * 128:(dc + 1) * 128], xb[:, blk, dc * 128:(dc + 1) * 128])
            cp("act" if blk % 2 == 0 else "dve", dst[:, :, blk * 128:(blk + 1) * 128],
               Buf(hf.ap.rearrange("p (a b) -> p a b", a=8), hf.key, [128, 8, 128], whole=True).v())

        def transpose_tile(src_f32, dst):
            for blk in range(4):
                cp("pool", xb[:, blk, :], src_f32[:, blk, :])
                hf = trhalf()
                for dc in range(8):
                    tr(hf[:, dc * 128:(dc + 1) * 128], xb[:, blk, dc * 128:(dc + 1) * 128])
                cp("act" if blk % 2 == 0 else "dve", dst[:, :, blk * 128:(blk + 1) * 128],
                   Buf(hf.ap.rearrange("p (a b) -> p a b", a=8), hf.key, [128, 8, 128], whole=True).v())

        def tile_layer(t, l, last):
            xt_, x1_ = XA, XB
            bc = lambda j: smalls[:, l, SM_BC + j:SM_BC + j + 1]
            transpose_tile(xt_, xT)
            def tm_unit(g, bankf):
                w = wload(l, U_TM + g)
                br = bias_row(l, g * 512)
                for blk in range(4):
                    pb = bankf()
                    for kc in range(8):
                        mm(pb.v(), xT[:, kc, blk * 128:(blk + 1) * 128], w[:, kc * 512:(kc + 1) * 512], kc == 0, False)
                    mm(pb.v(), ones_row.v(), br.v(), False, True)
                    if g == 0:
                        cp("act", rv[:, blk, :], pb.v())
                    elif g == 1:
                        act(rgs[:, blk, :], pb.v(), AF.Silu)
                    elif g == 2:
                        cp("act", sv[:, blk, :], pb.v())
                    elif g == 3:
                        act(su[:, blk, :], pb.v(), AF.Gelu_apprx_tanh)
                    else:
                        act(sgv[:, blk, :], pb.v(), AF.Gelu_apprx_tanh)
                if g == 2:
                    r0 = l * NBT + t * 4
                    dst = VH[r0:r0 + 4, :, :]
                    dma("pool", vh_d[r0:r0 + 4].rearrange("b p c -> p b c"), sv.v().ap, reads=[sv.v()], writes=[dst])

            def fm_unit(k, bankf):
                w = wload(l, U_FM + 4 + k)
                for c4 in range(4):
                    pb = bankf()
                    for kc in range(8):
                        mm(pb.v(), w[:, kc * 512 + c4 * 128: kc * 512 + (c4 + 1) * 128], xT[:, kc, :], kc == 0, kc == 7)
                    if k == 0:
                        act(lx_ext[:, c4, 3:515], pb.v(), AF.Identity, bias=bc(16 + c4))
                    elif k == 1:
                        act(gg[:, c4, :], pb.v(), AF.Gelu_apprx_tanh, bias=bc(20 + c4))
                    elif k == 2:
                        ts("dve", sq[:, c4, :], pb.v(), bc(24 + c4), 128.0 ** -0.5, ALU.add, ALU.mult)
                    else:
                        act(sk[:, c4, :], pb.v(), AF.Identity, bias=bc(28 + c4))
                if k == 3:
                    for h in range(4):
                        r = (l * 4 + h) * NT + t
                        dma("pool", kh_d[r], sk[:, h, :].ap, reads=[sk[:, h, :]], writes=[KH[r:r + 1, :, :]])

            tm_unit(0, nbank)
            tm_unit(1, nbank)
            for i in range(4):
                dma("pool", rot_t[i].v().ap, rot_in[i, :, t * T:(t + 1) * T], writes=[rot_t[i].v()])
            for qk in range(2):
                w0 = wload(l, U_FM + 2 * qk)
                w1 = wload(l, U_FM + 2 * qk + 1)
                dstb = rq if qk == 0 else rk
                for h in range(4):
                    pa = nbank()
                    pbk = nbank()
                    for kc in range(8):
                        mm(pa.v(), w0[:, kc * 512 + h * 128: kc * 512 + (h + 1) * 128], xT[:, kc, :], kc == 0, kc == 7)
                    for kc in range(8):
                        mm(pbk.v(), w1[:, kc * 512 + h * 128: kc * 512 + (h + 1) * 128], xT[:, kc, :], kc == 0, kc == 7)
                    j0 = (2 * qk) * 4 + h
                    j1 = (2 * qk + 1) * 4 + h
                    ta = rtmp[(h % 2) * 2]
                    tb = rtmp[(h % 2) * 2 + 1]
                    stt("dve", ta.v(), pa.v(), bc(j0), rot_t[2 * qk].v(), ALU.add, ALU.mult)
                    stt("dve", tb.v(), pbk.v(), bc(j1), rot_t[2 * qk + 1].v(), ALU.add, ALU.mult)
                    tt("pool", dstb[:, h, :], ta.v(), tb.v(), ALU.add)
            deferred = [lambda: fm_unit(0, nbank3), lambda: fm_unit(1, nbank3), lambda: tm_unit(2, nbank3),
                        lambda: fm_unit(3, nbank3), lambda: fm_unit(2, nbank3), lambda: tm_unit(3, nbank3),
                        lambda: tm_unit(4, nbank3)]

            for c in range(4):
                cs = slice(c * 128, (c + 1) * 128)
                tt("pool", qd[:, :, cs], rq[:, :, cs], QDt.v(), ALU.mult)
                tt("pool", vdec[:, c, :], rv[:, c, :], KDTt.v(), ALU.mult)
            for c in range(4):
                cs = slice(c * 128, (c + 1) * 128)
                hf = trhalf()
                for h in range(4):
                    tr(hf[:, h * 128:(h + 1) * 128], rk[:, h, cs])
                cp("dve", rkt[:, c, :], hf[:, 0:512])
                sb_ = banks[3]
                for h in range(4):
                    mm(sb_[:, h * 128:(h + 1) * 128], rk[:, h, cs], rq[:, h, cs], True, True, sig=(h == 3))
                sdc = sd_[c % 2]
                tt("dve", sdc.v(), Buf(sb_.ap.rearrange("p (a b) -> p a b", a=4), sb_.key, [128, 4, 128], whole=True).v(),
                   Buf(DTt.ap.rearrange("p (a b) -> p a b", a=4), "AR", [128, 4, 128], base=DTt.base, esz=4).v(), ALU.mult)
                yb_ = banks[4]
                for h in range(4):
                    hs = slice(h * 128, (h + 1) * 128)
                    mm(yb_[:, hs], sdc[:, h, :], rv[:, c, hs], True, False, sig=False)
                    mm(yb_[:, hs], qd[:, h, cs], stbf[:, l, h, :], False, True, sig=(h == 3))
                kvb = banks[5]
                for h in range(4):
                    hs = slice(h * 128, (h + 1) * 128)
                    mm(kvb[:, hs], rkt[:, c, hs], vdec[:, c, hs], True, True, sig=(h == 3))
                for h in range(4):
                    hs = slice(h * 128, (h + 1) * 128)
                    cdk = float(np.exp(np.float64(128.0) * np.log1p(-(2.0 ** (-5.0 - h)))))
                    stt("dve", st[:, l, h, :], st[:, l, h, :], cdk, kvb[:, hs], ALU.mult, ALU.add)
                cp("act", stbf[:, l, :, :], st[:, l, :, :])
                for h in range(4):
                    hs = slice(h * 128, (h + 1) * 128)
                    S.op("dve", lambda hh, h=h, hs=hs: hh.bn_stats(out=stats[:, h, :].ap, in_=yb_[:, hs].ap),
                         reads=[yb_[:, hs]], writes=[stats[:, h, :]])
                    S.op("dve", lambda hh, h=h: hh.bn_aggr(out=mv[:, h, :].ap, in_=stats[:, h, :].ap),
                         reads=[stats[:, h, :]], writes=[mv[:, h, :]])
                ts("dve", rs4.v(), mv[:, :, 1], LN_EPS, None, ALU.add)
                act(rs4.v(), rs4.v(), AF.Sqrt)
                S.op("dve", lambda hh: hh.reciprocal(out=rs4.v().ap, in_=rs4.v().ap), reads=[rs4.v()], writes=[rs4.v()])
                for h in range(4):
                    hs = slice(h * 128, (h + 1) * 128)
                    ts("dve", yn[:, hs], yb_[:, hs], mv[:, h, 0:1], rs4[:, h:h + 1], ALU.subtract, ALU.mult)
                tt("pool", ya.v(), yn.v(), rgs[:, c, :], ALU.mult)
                hf = trhalf()
                for wc in range(4):
                    tr(hf[:, wc * 128:(wc + 1) * 128], ya[:, wc * 128:(wc + 1) * 128])
                cp("act", ybT[0][:, :, cs], Buf(hf.ap[:, 0:512].rearrange("p (a b) -> p a b", a=4), hf.key, [128, 4, 128], whole=True).v())
                for _ in range(2):
                    if deferred:
                        deferred.pop(0)()

            for cc in range(4):
                cp("pool", lx_ext[:, cc, 0:3], lxtail[:, l, cc, :])
            for cc in range(4):
                cw = lambda j: smalls[:, l, SM_CW + cc * 4 + j: SM_CW + cc * 4 + j + 1]
                ts("dve", e_xc.v(), lx_ext[:, cc, 0:512], cw(0), smalls[:, l, SM_CB + cc:SM_CB + cc + 1], ALU.mult, ALU.add)
                for j in range(1, 4):
                    stt("dve", e_xc.v(), lx_ext[:, cc, j:j + 512], cw(j), e_xc.v(), ALU.mult, ALU.add)
                cp("pool", lxtail[:, l, cc, :], lx_ext[:, cc, 512:515])
                cp("pool", e_xcb.v(), e_xc.v())
                pr = nbank()
                pi = nbank()
                mm(pr.v(), wabd[:, l, 0, cc, :], e_xcb.v(), True, True)
                mm(pi.v(), wabd[:, l, 1, cc, :], e_xcb.v(), True, True)
                act(e_tr.v(), pr.v(), AF.Tanh, bias=lrc[:, l, cc, 2:3], scale=0.5)
                act(e_ti.v(), pi.v(), AF.Tanh, bias=lrc[:, l, cc, 3:4], scale=0.5)
                act(e_a.v(), e_tr.v(), AF.Exp, bias=lrc[:, l, cc, 0:1], scale=lrc[:, l, cc, 0:1])
                act(e_s.v(), e_tr.v(), AF.Exp, bias=lrc[:, l, cc, 1:2], scale=lrc[:, l, cc, 1:2])
                act(e_s.v(), e_s.v(), AF.Sqrt, bias=0.25, scale=-0.25)
                tt("dve", e_u.v(), e_s.v(), e_xc.v(), ALU.mult)
                stt("dve", e_u.v(), e_ti.v(), 1.0, e_u.v(), ALU.add, ALU.mult)
                eh = e_h[cc % 2]
                S.op("dve", lambda hh, eh=eh, cc=cc: hh.tensor_tensor_scan(out=eh.v().ap, data0=e_a.v().ap, data1=e_u.v().ap,
                                                                          initial=hstate[:, l, cc:cc + 1].ap,
                                                                          op0=ALU.mult, op1=ALU.add),
                     reads=[e_a.v(), e_u.v(), hstate[:, l, cc:cc + 1]], writes=[eh.v()])
                cp("dve", hstate[:, l, cc:cc + 1], eh[:, 511:512])
                tt("pool", ybT[1][:, cc, :], eh.v(), gg[:, cc, :], ALU.mult)

            nkb = 4 * (t + 1)
            nch = (t + 4) // 4
            steps = []
            for h in range(4):
                kbi = 0
                for ch in range(nch - 1, -1, -1):
                    t0 = ch * 4
                    ntl = min(t + 1, t0 + 4) - t0
                    for kl in range(ntl * 4 - 1, -1, -1):
                        steps.append((h, ch, t0, ntl, kl, kbi, kl == ntl * 4 - 1))
                        kbi += 1

            def kv_bufs(h, ch):
                return f_k[(h * nch + ch) % 2], f_v[(h * nch + ch) % 2]

            def s1(i):
                h, ch, t0, ntl, kl, kbi, first = steps[i]
                fk, fv = kv_bufs(h, ch)
                if first:
                    r = (l * 4 + h) * NT + t0
                    dma("sp", fk[:, 0:ntl * 512].ap.rearrange("p (a b) -> p a b", a=ntl),
                        kh_d[r:r + ntl].rearrange("a p c -> p a c"), reads=[KH[r:r + ntl, :, :]], writes=[fk[:, 0:ntl * 512]])
                    rb = l * NBT + t0 * 4
                    dma("sp", fv[:, 0:ntl * 4, :].ap, vh_d[rb:rb + ntl * 4, :, h * 128:(h + 1) * 128].rearrange("b p c -> p b c"),
                        reads=[VH[rb:rb + ntl * 4, :, :]], writes=[fv[:, 0:ntl * 4, :]])
                kb = t0 * 4 + kl
                diag = kb >= 4 * t
                a_ = kb - 4 * t
                par = i % 2
                kT = fk[:, kl * 128:(kl + 1) * 128]
                pa = banks[par]
                mm(pa.v(), kT, sq[:, h, :], True, not diag)
                if diag:
                    mm(pa.v(), identb.v(), negm[:, a_, :], False, True)
                act(f_e[par].v(), pa.v(), AF.Exp)
                act(f_lp[par].v(), f_e[par].v(), AF.Ln, bias=1.0)
                if kbi < nkb - 1:
                    if kbi == 0:
                        cp("dve", f_r.v(), f_lp[par].v())
                    else:
                        tt("dve", f_r.v(), f_r.v(), f_lp[par].v(), ALU.add)
                    cp("dve", f_rb[(i + 1) % 3].v(), f_r.v())

            def s2(i):
                h, ch, t0, ntl, kl, kbi, first = steps[i]
                fk, fv = kv_bufs(h, ch)
                kb = t0 * 4 + kl
                diag = kb >= 4 * t
                a_ = kb - 4 * t
                par = i % 2
                kT = fk[:, kl * 128:(kl + 1) * 128]
                pbk = banks[2 + par]
                mm(pbk.v(), kT, sq[:, h, :], True, False, sig=False)
                lastmm = "tri"
                if kbi > 0:
                    lastmm = "ones"
                if diag:
                    lastmm = "neg"
                mm(pbk.v(), trineg.v(), f_lp[par].v(), False, lastmm == "tri", sig=(lastmm == "tri"))
                if kbi > 0:
                    mm(pbk.v(), negones.v(), f_rb[i % 3].v(), False, lastmm == "ones", sig=(lastmm == "ones"))
                if diag:
                    mm(pbk.v(), identb.v(), negm[:, a_, :], False, True)
                act(f_w[par].v(), pbk.v(), AF.Exp)

            def s3(i):
                h, ch, t0, ntl, kl, kbi, first = steps[i]
                fk, fv = kv_bufs(h, ch)
                par = i % 2
                ob = banks[4]
                mm(ob.v(), fv[:, kl, :], f_w[par].v(), kbi == 0, kbi == nkb - 1)
                if kbi == nkb - 1:
                    cp("dve", ybT[2][:, h, :], ob.v())

            g_ops = []
            g_ops.append(lambda: dma("pool", g_tab.v().ap, rowtab_in[l, :, 0:1024].rearrange("p (a b) -> p a b", a=2), writes=[g_tab.v()]))

            def g_blk(blk):
                bs_ = slice(blk * 128, (blk + 1) * 128)
                src = sgv[:, blk, :]
                pbg = banks[5]

                def t1():
                    S.op("dve", lambda hh: hh.bn_stats(out=g_st.v().ap, in_=src.ap), reads=[src], writes=[g_st.v()])
                    S.op("dve", lambda hh: hh.bn_aggr(out=g_mv.v().ap, in_=g_st.v().ap), reads=[g_st.v()], writes=[g_mv.v()])
                    ts("dve", g_rs.v(), g_mv[:, 1:2], LN_EPS, None, ALU.add)

                def t2():
                    act(g_rs.v(), g_rs.v(), AF.Ln)
                    act(g_rs.v(), g_rs.v(), AF.Exp, scale=-0.5)

                def t3():
                    stt("dve", g_vn.v(), src, g_mv[:, 0:1], g_tab[:, 0, :], ALU.subtract, ALU.mult)
                    stt("dve", g_vl.v(), g_vn.v(), g_rs.v(), g_tab[:, 1, :], ALU.mult, ALU.add)

                def t4():
                    for g in range(4):
                        gs = slice(g * 128, (g + 1) * 128)
                        mm(pbg[:, gs], wsT[:, l, g, :], g_vl[:, gs], True, True, sig=(g == 3))
                    for g in range(4):
                        gs = slice(g * 128, (g + 1) * 128)
                        stt("dve", g_yd[:, gs], pbg[:, gs], smalls[:, l, SM_BS + g:SM_BS + g + 1], su[:, blk, gs], ALU.add, ALU.mult)

                def t5():
                    hf = trhalf()
                    for wc in range(4):
                        tr(hf[:, wc * 128:(wc + 1) * 128], g_yd[:, wc * 128:(wc + 1) * 128])
                    cp("dve", ybT[3][:, :, bs_], Buf(hf.ap[:, 0:512].rearrange("p (a b) -> p a b", a=4), hf.key, [128, 4, 128], whole=True).v())
                return [t1, t2, t3, t4, t5]

            for blk in range(4):
                g_ops.extend(g_blk(blk))

            s1(0)
            for i in range(len(steps)):
                if i + 1 < len(steps):
                    s1(i + 1)
                s2(i)
                if i >= 1:
                    s3(i - 1)
                if g_ops:
                    g_ops.pop(0)()
            s3(len(steps) - 1)
            while g_ops:
                g_ops.pop(0)()

            for dc in range(8):
                wg = wload(l, U_GT + dc)
                if dc % 2 == 0:
                    wbr = wload(l, U_BR + dc // 2)
                dcl = dc % 2
                for b in range(4):
                    pp = nbank()
                    pg = nbank()
                    for wc in range(4):
                        o_ = dcl * 2048 + (b * 4 + wc) * 128
                        mm(pp.v(), wbr[:, o_:o_ + 128], ybT[b][:, wc, :], wc == 0, wc == 3)
                    for kc in range(8):
                        mm(pg.v(), wg[:, kc * 512 + b * 128: kc * 512 + (b + 1) * 128], xT[:, kc, :], kc == 0, kc == 7)
                    gb = h_g[b % 2]
                    act(gb.v(), pg.v(), AF.Sigmoid, bias=bc(32 + dc * 4 + b))
                    if b == 0:
                        tt("dve", h_acc.v(), gb.v(), pp.v(), ALU.mult)
                    else:
                        tmp = h_tmp[b % 2]
                        tt("dve", tmp.v(), gb.v(), pp.v(), ALU.mult)
                        if b < 3:
                            tt("pool", h_acc.v(), h_acc.v(), tmp.v(), ALU.add)
                        else:
                            tt("pool", mergedT[:, dc, :], h_acc.v(), tmp.v(), ALU.add)

            dma("pool", i_tab.v().ap, rowtab_in[l, :, 1024:3072].rearrange("p (a b) -> p a b", a=2), writes=[i_tab.v()])
            wos = [wload(l, U_WO), wload(l, U_WO + 1)]
            for blk in range(4):
                for half in range(2):
                    bk = banks[(blk % 2) * 2 + half]
                    w = wos[half]
                    hsl = slice(half * 512, (half + 1) * 512)
                    for dc in range(8):
                        mm(bk.v(), mergedT[:, dc, blk * 128:(blk + 1) * 128], w[:, dc * 512:(dc + 1) * 512], dc == 0, dc == 7)
                    stt("dve", x1_[:, blk, hsl], xt_[:, blk, hsl], ALPHA, bk.v(), ALU.mult, ALU.add)
                ln_blk(x1_, blk, i_tab[:, 0, :], i_tab[:, 1, :])
                transpose_blk(x1_, blk, xT)

            dma("pool", i_tab.v().ap, rowtab_in[l, :, 3072:5120].rearrange("p (a b) -> p a b", a=2), writes=[i_tab.v()])
            for u in range(8):
                w = wload(l, U_W1 + u)
                for f in range(4):
                    fc = u * 4 + f
                    pb = nbank()
                    for kc in range(8):
                        mm(pb.v(), w[:, kc * 512 + f * 128: kc * 512 + (f + 1) * 128], xT[:, kc, :], kc == 0, kc == 7)
                    jr = j_r[fc % 2]
                    act(jr.v(), pb.v(), AF.Relu, bias=smalls[:, l, SM_B1 + fc:SM_B1 + fc + 1])
                    tt("pool", hT[:, fc, :], jr.v(), jr.v(), ALU.mult)
            for half in range(2):
                for u4 in range(4):
                    w = wload(l, U_W2 + half * 4 + u4)
                    for j in range(8):
                        fc = u4 * 8 + j
                        for blk in range(4):
                            mm(banks[blk].v(), hT[:, fc, blk * 128:(blk + 1) * 128], w[:, j * 512:(j + 1) * 512], fc == 0, False, sig=False)
                br2 = bias_row(l, 2560 + half * 512)
                for blk in range(4):
                    mm(banks[blk].v(), ones_row.v(), br2.v(), False, True)
                for blk in range(4):
                    hsl = slice(half * 512, (half + 1) * 512)
                    stt("dve", xt_[:, blk, hsl], x1_[:, blk, hsl], ALPHA, banks[blk].v(), ALU.mult, ALU.add)
            ln_tile(xt_, i_tab[:, 0, :], i_tab[:, 1, :])
            if last:
                dma("sp", y_out[t * T:(t + 1) * T, :].rearrange("(b p) c -> p b c", p=128), xt_.v().ap, reads=[xt_.v()])

        for t in range(NT):
            dma("sp", XA.v().ap, x_in[t * T:(t + 1) * T, :].rearrange("(b p) c -> p b c", p=128), writes=[XA.v()])
            for l in range(NL):
                tile_layer(t, l, l == NL - 1)

        S.emit({"pe": block.tensor, "act": block.scalar, "dve": block.vector, "pool": block.gpsimd, "sp": block.sync},
               csem, dsem)
    return nc, S


def _unit_cols(Wc):
    return np.ascontiguousarray(Wc.reshape(8, 128, 512).transpose(1, 0, 2)).reshape(128, 4096)


def _swap_half(a):
    s = a.reshape(a.shape[:-1] + (4, 2, 64))
    return np.ascontiguousarray(s[..., ::-1, :]).reshape(a.shape)


def pack_layer(inp, l):
    w_in = inp["w_in"][l]
    b_in = inp["b_in"][l]
    sec = lambda i, n=512: slice(i, i + n)
    RQ, RK, RV, RG, LX, LG, SQ, SK, SV, SU, SGV, GT = 0, 512, 1024, 1536, 2048, 2560, 3072, 3584, 4096, 4608, 5120, 5632
    units = []
    for c0 in (RV, RG, SV, SU, SGV):
        units.append(_unit_cols(w_in[:, sec(c0)]))
    units.append(_unit_cols(w_in[:, sec(RQ)]))
    units.append(_unit_cols(_swap_half(w_in[:, sec(RQ)])))
    units.append(_unit_cols(w_in[:, sec(RK)]))
    units.append(_unit_cols(_swap_half(w_in[:, sec(RK)])))
    for c0 in (LX, LG, SQ, SK):
        units.append(_unit_cols(w_in[:, sec(c0)]))
    for dc in range(8):
        cols = np.concatenate([w_in[:, GT + b * 1024 + dc * 128: GT + b * 1024 + (dc + 1) * 128] for b in range(4)], axis=1)
        units.append(_unit_cols(cols))
    wb = inp["w_branch"][l].reshape(4, 4, 128, 8, 128).transpose(2, 3, 0, 1, 4).reshape(128, 8, 16, 128)
    for j in range(4):
        units.append(np.ascontiguousarray(wb[:, 2 * j:2 * j + 2]).reshape(128, 4096))
    for half in range(2):
        units.append(_unit_cols(inp["w_out"][l][:, half * 512:(half + 1) * 512]))
    for u in range(8):
        units.append(_unit_cols(inp["w1"][l][:, u * 512:(u + 1) * 512]))
    w2 = inp["w2"][l]
    for half in range(2):
        wh = w2[:, half * 512:(half + 1) * 512].reshape(32, 128, 512)
        for u4 in range(4):
            units.append(np.ascontiguousarray(wh[8 * u4:8 * u4 + 8].transpose(1, 0, 2)).reshape(128, 4096))
    assert len(units) == NUNIT
    wp = np.stack(units, 0).astype(np.float32)
    sm = np.zeros((128, NSM), np.float32)
    colv = lambda v: np.ascontiguousarray(v.reshape(-1, 128).T)
    fm_b = [b_in[sec(RQ)], _swap_half(b_in[sec(RQ)]), b_in[sec(RK)], _swap_half(b_in[sec(RK)]),
            b_in[sec(LX)], b_in[sec(LG)], b_in[sec(SQ)], b_in[sec(SK)]]
    sm[:, SM_BC:SM_BC + 32] = np.concatenate([colv(v) for v in fm_b], axis=1)
    gcols = []
    for dc in range(8):
        for b in range(4):
            gcols.append(b_in[GT + b * 1024 + dc * 128: GT + b * 1024 + (dc + 1) * 128][:, None])
    sm[:, SM_BC + 32:SM_BC + 64] = np.concatenate(gcols, axis=1)
    cw = inp["conv_w"][l]
    sm[:, SM_CW:SM_CW + 16] = cw.reshape(4, 4, 128).transpose(2, 1, 0).reshape(128, 16)
    sm[:, SM_CB:SM_CB + 4] = colv(inp["conv_b"][l])
    sm[:, SM_BA:SM_BA + 4] = colv(inp["lru_ba"][l])
    sm[:, SM_BX:SM_BX + 4] = colv(inp["lru_bx"][l])
    sm[:, SM_LAM:SM_LAM + 4] = colv(inp["lru_lambda"][l])
    sm[:, SM_BS:SM_BS + 4] = inp["sg_bs"][l].T
    sm[:, SM_B1:SM_B1 + 32] = colv(inp["b1"][l])
    rep = lambda v: np.broadcast_to(v[None, :], (128, v.shape[0]))
    rowtab = np.concatenate([rep(inp["sg_ln_g"][l]), rep(inp["sg_ln_b"][l]), rep(inp["ln1_g"][l]), rep(inp["ln1_b"][l]),
                             rep(inp["ln2_g"][l]), rep(inp["ln2_b"][l])], axis=1).astype(np.float32)
    wabd = np.zeros((128, 2, 4, 128), np.float32)
    for k, nm in enumerate(("lru_wa", "lru_wx")):
        wm = inp[nm][l]
        for cc in range(4):
            wabd[0:64, k, cc, 0:64] = wm[2 * cc]
            wabd[64:128, k, cc, 64:128] = wm[2 * cc + 1]
    sgws = np.ascontiguousarray(inp["sg_ws"][l].transpose(1, 0, 2)).reshape(128, 512)
    brow = np.concatenate([b_in[sec(RV)], b_in[sec(RG)], b_in[sec(SV)], b_in[sec(SU)], b_in[sec(SGV)], inp["b2"][l]])[None, :]
    return wp, sm, rowtab, wabd.reshape(128, 1024), sgws, brow.astype(np.float32)


def make_consts(S_LEN):
    c = np.zeros((128, NCONST), np.float32)
    idx = np.arange(128)
    c[:, C_ID:C_ID + 128] = np.eye(128)
    c[:, C_TRI:C_TRI + 128] = -(idx[:, None] >= idx[None, :]).astype(np.float32)
    c[:, C_NONE:C_NONE + 128] = -1.0
    for a in range(4):
        key = a * 128 + idx[:, None]
        qry = np.arange(512)[None, :]
        c[:, C_NEGM + a * 512:C_NEGM + (a + 1) * 512] = np.where(key >= qry, NEG, 0.0)
    log_g = np.log1p(-(2.0 ** (-5.0 - np.arange(4, dtype=np.float64))))
    for h in range(4):
        diff = idx[None, :] - idx[:, None]
        c[:, C_DT + h * 128:C_DT + (h + 1) * 128] = np.where(diff >= 0, np.exp(log_g[h] * np.maximum(diff, 0)), 0.0)
        c[:, C_QD + h * 128:C_QD + (h + 1) * 128] = np.exp(log_g[h] * (idx + 1.0))[None, :]
        c[:, C_KDT + h * 128:C_KDT + (h + 1) * 128] = np.exp(log_g[h] * (127.0 - idx))[:, None]
    c[:, C_TRIL:C_TRIL + 128] = (idx[None, :] <= idx[:, None]).astype(np.float32)
    inv_freq = (np.float32(10000.0) ** (-np.arange(64, dtype=np.float32) / np.float32(64))).astype(np.float32)
    pos = np.arange(S_LEN, dtype=np.float32)
    ang = (pos[None, :] * inv_freq[:, None]).astype(np.float32).astype(np.float64)
    cos = np.cos(ang)
    sin = np.sin(ang)
    C = np.concatenate([cos, cos], 0)
    Sg = np.concatenate([-sin, sin], 0)
    ks = 128.0 ** -0.5
    rot = np.stack([C, Sg, C * ks, Sg * ks], 0).astype(np.float32)
    return c, rot


_CACHE = {}


def run_model(inputs, S_LEN, NL, n_cores, batch_of_core):
    key = (S_LEN, NL)
    if key not in _CACHE:
        _CACHE[key] = build_program(S_LEN, NL)
    nc, _ = _CACHE[key]
    inp = {k: np.asarray(v) for k, v in inputs.items()}
    packs = [pack_layer(inp, l) for l in range(NL)]
    wpack = np.concatenate([p[0].reshape(NUNIT * 256, 2048) for p in packs], 0)
    smalls = np.stack([p[1] for p in packs], 0)
    rowtab = np.stack([p[2] for p in packs], 0)
    wabd = np.stack([p[3] for p in packs], 0)
    sgws = np.stack([p[4] for p in packs], 0)
    brow = np.stack([p[5] for p in packs], 0)
    consts, rot = make_consts(S_LEN)
    shared = {"wpack": wpack, "smalls": smalls, "rowtab": rowtab, "wabd": wabd, "sgws": sgws,
              "consts": consts, "rot": rot, "brow": brow}
    in_maps = []
    for c in range(n_cores):
        m = dict(shared)
        m["x"] = np.ascontiguousarray(inp["x"][batch_of_core[c]], dtype=np.float32)
        in_maps.append(m)
    res = run_bass_kernel_spmd(nc, in_maps, core_ids=list(range(n_cores)))
    return [r["y"] for r in res.results]


def kernel(**inputs):
    x = np.asarray(inputs["x"])
    B, S_LEN, _ = x.shape
    boc = [c % B for c in range(8)]
    outs = run_model(inputs, S_LEN, DEPTH, 8, boc)
    return np.stack([outs[b] for b in range(B)], 0).astype(np.float32)
```

```python
import math
from contextlib import ExitStack

import numpy as np
import concourse.bass as bass
import concourse.mybir as mybir
from concourse.bass_utils import run_bass_kernel_spmd

F32 = mybir.dt.float32
BF16 = mybir.dt.bfloat16
AF = mybir.ActivationFunctionType
ALU = mybir.AluOpType

D_MODEL = 1024
BW = 512
DFF = 4096
DEPTH = 2
ALPHA = (2 * DEPTH) ** 0.25
LN_EPS = 1e-5
T = 512
NUNIT = 43
U_TM, U_FM, U_GT, U_BR, U_WO, U_W1, U_W2 = 0, 5, 13, 21, 25, 27, 35
NSM = 132
SM_BC, SM_CW, SM_CB, SM_BA, SM_BX, SM_LAM, SM_BS, SM_B1 = 0, 64, 80, 84, 88, 92, 96, 100
NCONST = 4096
C_ID, C_TRI, C_NONE, C_NEGM, C_DT, C_QD, C_KDT, C_TRIL = 0, 128, 256, 384, 2432, 2944, 3456, 3968
NEG = -30000.0


class View:
    __slots__ = ("ap", "key", "lo", "hi")

    def __init__(self, ap, key, lo, hi):
        self.ap, self.key, self.lo, self.hi = ap, key, lo, hi


class Buf:
    def __init__(self, ap, key, shape, part=True, base=0, esz=1, whole=False):
        self.whole = whole
        self.ap = ap
        self.key = key
        self.shape = list(shape)
        self.part = part
        self.base = base
        self.esz = esz
        free = self.shape[1:] if part else self.shape
        st = []
        s = 1
        for d in reversed(free):
            st.append(s)
            s *= d
        self.strides = list(reversed(st))

    def __getitem__(self, idx):
        if not isinstance(idx, tuple):
            idx = (idx,)
        idx = list(idx) + [slice(None)] * (len(self.shape) - len(idx))
        ap = self.ap[tuple(idx)]
        fi = idx[1:] if self.part else idx
        fs = self.shape[1:] if self.part else self.shape
        lo = 0
        hi = 0
        for ix, st, d in zip(fi, self.strides, fs):
            if isinstance(ix, int):
                a, b = ix, ix + 1
            else:
                a = 0 if ix.start is None else ix.start
                b = d if ix.stop is None else ix.stop
            lo += a * st
            hi += (b - 1) * st
        if self.whole:
            return View(ap, self.key, 0, 1)
        return View(ap, self.key, self.base + lo * self.esz, self.base + (hi + 1) * self.esz)

    def v(self):
        return self[tuple(slice(None) for _ in self.shape)]


class _Op:
    __slots__ = ("eng", "idx", "fn", "sig", "dma", "deps", "seq", "dman")


DMA_RING = 8
ENGS = ("pe", "act", "dve", "pool", "sp")


class Sched:
    def __init__(self):
        self.ops = {e: [] for e in ENGS}
        self.reg = {}
        self.seq = 0
        self.ndma = {e: 0 for e in ENGS}

    def op(self, eng, fn, reads=(), writes=(), sig=True, dma=False):
        o = _Op()
        o.eng, o.fn, o.sig, o.dma = eng, fn, sig, dma
        o.idx = len(self.ops[eng])
        o.seq = self.seq
        self.seq += 1
        o.dman = -1
        deps = {}
        if dma:
            o.dman = self.ndma[eng]
            self.ndma[eng] += 1
            o.sig = True
        for v in reads:
            self._read(v, o, deps)
        for v in writes:
            self._write(v, o, deps)
        out = []
        for p, raw in deps.items():
            if p is o:
                continue
            if p.eng == eng and not p.dma and not dma:
                if eng == "pe":
                    continue
            out.append(p)
        o.deps = out
        self.ops[eng].append(o)
        return o

    def _read(self, v, o, deps):
        recs = self.reg.setdefault(v.key, {})
        for (lo, hi), r in recs.items():
            if lo < v.hi and v.lo < hi and r[0] is not None:
                deps[r[0]] = True
        r = recs.get((v.lo, v.hi))
        if r is None:
            r = [None, {}, []]
            recs[(v.lo, v.hi)] = r
        if o.dma:
            r[2].append(o)
        else:
            r[1][o.eng] = o

    def _write(self, v, o, deps):
        recs = self.reg.setdefault(v.key, {})
        dead = []
        for (lo, hi), r in recs.items():
            if lo < v.hi and v.lo < hi:
                if r[0] is not None:
                    deps.setdefault(r[0], False)
                for p in r[1].values():
                    deps.setdefault(p, False)
                for p in r[2]:
                    deps.setdefault(p, False)
                if v.lo <= lo and hi <= v.hi:
                    dead.append((lo, hi))
        for k in dead:
            del recs[k]
        recs[(v.lo, v.hi)] = [o, {}, []]

    def emit(self, block_fns, csem, dsem):
        allops = sorted((o for e in ENGS for o in self.ops[e]), key=lambda o: o.seq)
        for o in allops:
            for p in o.deps:
                if p.dma or p.sig:
                    continue
                lst = self.ops[p.eng]
                k = p.idx
                while k < len(lst) and (lst[k].dma or not lst[k].sig):
                    k += 1
                if k >= len(lst) or lst[k].seq >= o.seq:
                    p.sig = True
        sigcount = {}
        nextsig = {}
        for e in ENGS:
            ops = self.ops[e]
            comp = [o for o in ops if not o.dma]
            if comp and not comp[-1].sig:
                comp[-1].sig = True
            c = 0
            sc = {}
            for o in ops:
                if not o.dma and o.sig:
                    c += 1
                sc[o.idx] = c
            sigcount[e] = sc
            ns = {}
            nxt = None
            for o in reversed(ops):
                if not o.dma and o.sig:
                    nxt = o
                ns[o.idx] = nxt
            nextsig[e] = ns

        def event(p, consumer):
            if p.dma:
                n = p.dman
                return dsem[p.eng][n % DMA_RING], 16 * (n // DMA_RING + 1)
            j = nextsig[p.eng][p.idx]
            assert j is not None
            assert j is p or j.seq < consumer.seq, "signal recorded after its consumer"
            return csem[p.eng], sigcount[p.eng][j.idx]

        def build(e):
            def body(h):
                waited = {}
                for o in self.ops[e]:
                    need = {}
                    for p in o.deps:
                        s, val = event(p, o)
                        k = id(s)
                        if need.get(k, (None, 0))[1] < val:
                            need[k] = (s, val)
                    if o.dma and o.dman >= DMA_RING:
                        n = o.dman - DMA_RING
                        s = dsem[e][n % DMA_RING]
                        val = 16 * (n // DMA_RING + 1)
                        k = id(s)
                        if need.get(k, (None, 0))[1] < val:
                            need[k] = (s, val)
                    for k, (s, val) in need.items():
                        if waited.get(k, 0) < val:
                            h.wait_ge(s, val)
                            waited[k] = val
                    inst = o.fn(h)
                    if o.dma:
                        inst.then_inc(dsem[e][o.dman % DMA_RING], 16)
                    elif o.sig:
                        inst.then_inc(csem[e], 1)
                nd = self.ndma[e]
                for n in range(max(0, nd - DMA_RING), nd):
                    s = dsem[e][n % DMA_RING]
                    val = 16 * (n // DMA_RING + 1)
                    if waited.get(id(s), 0) < val:
                        h.wait_ge(s, val)
                        waited[id(s)] = val
            return body

        for e in ENGS:
            if self.ops[e]:
                block_fns[e](build(e))


ARENA_BYTES = 192 * 1024


def build_program(S_LEN, NL):
    NT = S_LEN // T
    NBT = S_LEN // 128
    nc = bass.Bass("TRN2", target_bir_lowering=False)
    x_in = nc.dram_tensor("x", [S_LEN, D_MODEL], F32, kind="ExternalInput").ap()
    NWROWS = NL * NUNIT * 256
    wpack = nc.dram_tensor("wpack", [NWROWS, 2048], F32, kind="ExternalInput").ap()
    smalls_in = nc.dram_tensor("smalls", [NL, 128, NSM], F32, kind="ExternalInput").ap()
    rowtab_in = nc.dram_tensor("rowtab", [NL, 128, 5120], F32, kind="ExternalInput").ap()
    wabd_in = nc.dram_tensor("wabd", [NL, 128, 1024], F32, kind="ExternalInput").ap()
    sgws_in = nc.dram_tensor("sgws", [NL, 128, 512], F32, kind="ExternalInput").ap()
    consts_in = nc.dram_tensor("consts", [128, NCONST], F32, kind="ExternalInput").ap()
    rot_in = nc.dram_tensor("rot", [4, 128, S_LEN], F32, kind="ExternalInput").ap()
    brow_in = nc.dram_tensor("brow", [NL, 1, 3584], F32, kind="ExternalInput").ap()
    y_out = nc.dram_tensor("y", [S_LEN, D_MODEL], F32, kind="ExternalOutput").ap()
    ws_d = nc.dram_tensor("ws", [NWROWS, 2048], BF16, kind="Internal").ap()
    kh_d = nc.dram_tensor("kh", [NL * 4 * NT, 128, 512], BF16, kind="Internal").ap()
    vh_d = nc.dram_tensor("vh", [NL * NBT, 128, 512], BF16, kind="Internal").ap()
    browd_d = nc.dram_tensor("browd", [NL, 1, 3584], BF16, kind="Internal").ap()

    S = Sched()
    es = ExitStack()
    with es:
        art = es.enter_context(nc.sbuf_tensor("arena", [128, ARENA_BYTES // 2], BF16))
        banks = []
        for i in range(6):
            pt_ = es.enter_context(nc.psum_tensor(f"pb{i}", [128, 512], F32))
            banks.append(Buf(pt_, f"pb{i}", [128, 512], whole=True))
        PT = []
        for i in range(2):
            ptt_ = es.enter_context(nc.psum_tensor(f"ptr{i}", [128, 1024], BF16))
            PT.append(Buf(ptt_, f"ptr{i}", [128, 1024], whole=True))
        csem = {e: es.enter_context(nc.semaphore("c_" + e)) for e in ENGS}
        dsem = {e: [es.enter_context(nc.semaphore(f"d_{e}{i}")) for i in range(DMA_RING)] for e in ENGS}
        block = es.enter_context(nc.Block())

        WS = Buf(ws_d, "ws", [NWROWS, 2048], part=False)
        KH = Buf(kh_d, "kh", [NL * 4 * NT, 128, 512], part=False)
        VH = Buf(vh_d, "vh", [NL * NBT, 128, 512], part=False)
        BROWD = Buf(browd_d, "browd", [NL, 1, 3584], part=False)

        def abuf(off, shape, dt, parts=128):
            esz = 4 if dt == F32 else 2
            n = 1
            for d in shape:
                n *= d
            assert off % 4 == 0 and off + n * esz <= ARENA_BYTES, (off, shape)
            ap = art[0:parts, off // 2: off // 2 + n * esz // 2]
            if dt == F32:
                ap = ap.bitcast(F32)
            if len(shape) > 1:
                names = "abcdefg"[:len(shape)]
                pat = "p (" + " ".join(names) + ") -> p " + " ".join(names)
                ap = ap.rearrange(pat, **{n_: d_ for n_, d_ in zip(names[:-1], shape[:-1])})
            return Buf(ap, "AR", [parts] + list(shape), base=off, esz=esz)

        cur = [0]

        def palloc(shape, dt, parts=128):
            esz = 4 if dt == F32 else 2
            n = 1
            for d in shape:
                n *= d
            b = abuf(cur[0], shape, dt, parts)
            cur[0] += (n * esz + 31) // 32 * 32
            return b

        XA = palloc([4, 1024], F32)
        XB = palloc([4, 1024], F32)
        WR = [palloc([4096], BF16) for _ in range(3)]
        identb = palloc([128], BF16)
        trineg = palloc([128], BF16)
        negones = palloc([128], BF16)
        negm = palloc([4, 512], BF16)
        DTt = palloc([512], F32)
        QDt = palloc([4, 128], F32)
        KDTt = palloc([512], F32)
        tril = palloc([128], F32)
        smalls = palloc([NL, NSM], F32)
        lrc = palloc([NL, 4, 4], F32)
        wabd = palloc([NL, 2, 4, 128], BF16)
        wsT = palloc([NL, 4, 128], BF16)
        ones_row = palloc([128], BF16, parts=1)
        st = palloc([NL, 4, 128], F32)
        stbf = palloc([NL, 4, 128], BF16)
        hstate = palloc([NL, 4], F32)
        lxtail = palloc([NL, 4, 3], F32)
        SCR = cur[0]

        def sbuf(off, shape, dt, parts=128):
            return abuf(SCR + off, shape, dt, parts)

        xT = sbuf(0, [8, 512], BF16)
        xb = sbuf(8192, [4, 1024], BF16)
        ybT = [sbuf(16384 + 4096 * b, [4, 512], BF16) for b in range(4)]
        su = sbuf(32768, [4, 512], F32)
        sgv = sbuf(40960, [4, 512], F32)
        sq = sbuf(49152, [4, 512], BF16)
        lx_ext = sbuf(53248, [4, 516], F32)
        gg = sbuf(61504, [4, 512], F32)
        rv = sbuf(69696, [4, 512], BF16)
        rgs = sbuf(73792, [4, 512], F32)
        rq = sbuf(81984, [4, 512], BF16)
        rk = sbuf(86080, [4, 512], BF16)
        LOC_D = 90176
        rot_t = [sbuf(8192 + 2048 * i, [512], F32) for i in range(4)]
        rtmp = [sbuf(16384 + 2048 * i, [512], F32) for i in range(4)]
        sv = sbuf(24576, [4, 512], BF16)
        sk = sbuf(28672, [4, 512], BF16)
        bslot = [sbuf(LOC_D + 17600 + 1024 * i, [512], BF16, parts=1) for i in range(2)]
        qd = sbuf(LOC_D, [4, 512], BF16)
        rkt = sbuf(LOC_D + 4096, [4, 512], BF16)
        vdec = sbuf(LOC_D + 8192, [4, 512], BF16)
        sd_ = [sbuf(LOC_D + 12288 + 1024 * i, [4, 128], BF16) for i in range(2)]
        yn = sbuf(LOC_D + 14336, [512], F32)
        ya = sbuf(LOC_D + 16384, [512], BF16)
        stats = sbuf(LOC_D + 17408, [4, 6], F32)
        mv = sbuf(LOC_D + 17408 + 96, [4, 2], F32)
        rs4 = sbuf(LOC_D + 17408 + 128, [4], F32)
        END_D = LOC_D + 17408 + 160
        LOC_E = 69696
        e_xc = sbuf(LOC_E, [512], F32)
        e_xcb = sbuf(LOC_E + 2048, [512], BF16)
        e_tr = sbuf(LOC_E + 3072, [512], F32)
        e_ti = sbuf(LOC_E + 5120, [512], F32)
        e_a = sbuf(LOC_E + 7168, [512], F32)
        e_s = sbuf(LOC_E + 9216, [512], F32)
        e_u = sbuf(LOC_E + 11264, [512], F32)
        e_h = [sbuf(LOC_E + 13312 + 2048 * i, [512], F32) for i in range(2)]
        LOC_F = 53248
        f_k = [sbuf(LOC_F + 4096 * i, [2048], BF16) for i in range(2)]
        f_v = [sbuf(LOC_F + 8192 + 4096 * i, [16, 128], BF16) for i in range(2)]
        f_e = [sbuf(LOC_F + 16384 + 2048 * i, [512], F32) for i in range(2)]
        f_lp = [sbuf(LOC_F + 20480 + 1024 * i, [512], BF16) for i in range(2)]
        f_w = [sbuf(LOC_F + 22528 + 1024 * i, [512], BF16) for i in range(2)]
        f_r = sbuf(LOC_F + 24576, [512], F32)
        f_rb = [sbuf(LOC_F + 26624 + 1024 * i, [512], BF16) for i in range(3)]
        g_tab = sbuf(8192, [2, 512], F32)
        g_vn = sbuf(12288, [512], F32)
        g_vl = sbuf(14336, [512], BF16)
        g_yd = sbuf(15360, [512], BF16)
        g_st = sbuf(LOC_D + 19648, [6], F32)
        g_mv = sbuf(LOC_D + 19648 + 32, [2], F32)
        g_rs = sbuf(LOC_D + 19648 + 64, [1], F32)
        mergedT = sbuf(32768, [8, 512], BF16)
        h_g = [sbuf(40960 + 2048 * i, [512], F32) for i in range(2)]
        h_acc = sbuf(40960 + 4096, [512], F32)
        h_tmp = [sbuf(40960 + 6144 + 2048 * i, [512], F32) for i in range(2)]
        LOC_I = 53248
        i_tab = sbuf(LOC_I, [2, 1024], F32)
        i_st = sbuf(LOC_I + 8192, [4, 2, 6], F32)
        i_mv = sbuf(LOC_I + 8192 + 192, [4, 2], F32)
        i_rs = sbuf(LOC_I + 8192 + 224, [4], F32)
        hT = sbuf(16384, [32, 512], BF16)
        j_r = [sbuf(49152 + 2048 * i, [512], F32) for i in range(2)]
        assert LOC_D + 19648 + 96 <= ARENA_BYTES - SCR, (END_D, ARENA_BYTES - SCR)

        def vw(x):
            return x.v() if isinstance(x, Buf) else x

        def mm(out, lhsT, rhs, start, stop, sig=None):
            S.op("pe", lambda h: h.matmul(out.ap, lhsT=lhsT.ap, rhs=rhs.ap, start=start, stop=stop),
                 reads=[lhsT, rhs], writes=[out], sig=(stop if sig is None else sig))

        def tr(out, in_):
            idv = identb.v()
            S.op("pe", lambda h: h.transpose(out=out.ap, in_=in_.ap, identity=idv.ap), reads=[in_, idv], writes=[out])

        def act(out, in_, func, bias=None, scale=None):
            reads = [in_]
            kw = {}
            if bias is not None:
                if isinstance(bias, View):
                    reads.append(bias)
                    kw["bias"] = bias.ap
                else:
                    kw["bias"] = float(bias)
            if scale is not None:
                if isinstance(scale, View):
                    reads.append(scale)
                    kw["scale"] = scale.ap
                else:
                    kw["scale"] = float(scale)
            S.op("act", lambda h: h.activation(out=out.ap, in_=in_.ap, func=func, **kw), reads=reads, writes=[out])

        def tt(eng, out, a, b, op):
            S.op(eng, lambda h: h.tensor_tensor(out=out.ap, in0=a.ap, in1=b.ap, op=op), reads=[a, b], writes=[out])

        def ts(eng, out, a, s1, s2, op0, op1=None):
            reads = [a]
            k1 = s1.ap if isinstance(s1, View) else float(s1)
            if isinstance(s1, View):
                reads.append(s1)
            if op1 is None:
                S.op(eng, lambda h: h.tensor_scalar(out=out.ap, in0=a.ap, scalar1=k1, scalar2=None, op0=op0),
                     reads=reads, writes=[out])
                return
            k2 = s2.ap if isinstance(s2, View) else float(s2)
            if isinstance(s2, View):
                reads.append(s2)
            S.op(eng, lambda h: h.tensor_scalar(out=out.ap, in0=a.ap, scalar1=k1, scalar2=k2, op0=op0, op1=op1),
                 reads=reads, writes=[out])

        def stt(eng, out, in0, sc, in1, op0, op1):
            reads = [in0, in1]
            k = sc.ap if isinstance(sc, View) else float(sc)
            if isinstance(sc, View):
                reads.append(sc)
            S.op(eng, lambda h: h.scalar_tensor_tensor(out=out.ap, in0=in0.ap, scalar=k, in1=in1.ap, op0=op0, op1=op1),
                 reads=reads, writes=[out])

        def cp(eng, out, in_):
            if eng == "act":
                S.op("act", lambda h: h.copy(out=out.ap, in_=in_.ap), reads=[in_], writes=[out])
            else:
                S.op(eng, lambda h: h.tensor_copy(out=out.ap, in_=in_.ap), reads=[in_], writes=[out])

        _tt0, _cp0 = tt, cp

        def tt(eng, out, a, b, op):
            _tt0(eng, out, a, b, op)
            if eng == "pool":
                issue_cast()

        def cp(eng, out, in_):
            _cp0(eng, out, in_)
            if eng == "pool":
                issue_cast()

        def mset(eng, out, val):
            S.op(eng, lambda h: h.memset(out.ap, val), writes=[out])

        def dma(eng, out_ap, in_ap, reads=(), writes=(), **kw):
            S.op(eng, lambda h: h.dma_start(out=out_ap, in_=in_ap, **kw), reads=reads, writes=writes, dma=True)

        bank_rr = [0]

        def nbank():
            b = banks[bank_rr[0] % 6]
            bank_rr[0] += 1
            return b

        def nbank3():
            b = banks[bank_rr[0] % 3]
            bank_rr[0] += 1
            return b

        bs_rr = [0]

        def bias_row(l, c0):
            b = bslot[bs_rr[0] % 2]
            bs_rr[0] += 1
            dma("pool", b.v().ap, browd_d[l][:, c0:c0 + 512], reads=[BROWD[l:l + 1, :, :]], writes=[b.v()])
            return b

        wr_rr = [0]
        cast_upto = [0]
        pending_casts = []

        def wload(l, u):
            w = WR[wr_rr[0] % 3]
            wr_rr[0] += 1
            r0 = (l * NUNIT + u) * 256
            while cast_upto[0] < r0 + 256:
                issue_cast()
            src = WS[r0:r0 + 256, :]
            dma("sp", w.v().ap, ws_d[r0:r0 + 256, :].rearrange("(p a) c -> p (a c)", a=2), reads=[src], writes=[w.v()])
            return w

        tr_rr = [0]

        def trhalf():
            hf = tr_rr[0] % 2
            tr_rr[0] += 1
            return PT[hf]

        csl = lambda a, n: consts_in[:, a:a + n]
        dma("pool", identb.v().ap, csl(C_ID, 128), writes=[identb.v()])
        dma("pool", trineg.v().ap, csl(C_TRI, 128), writes=[trineg.v()])
        dma("pool", negones.v().ap, csl(C_NONE, 128), writes=[negones.v()])
        dma("pool", negm.v().ap, csl(C_NEGM, 2048).rearrange("p (a b) -> p a b", a=4), writes=[negm.v()])
        dma("sp", DTt.v().ap, csl(C_DT, 512), writes=[DTt.v()])
        dma("sp", QDt.v().ap, csl(C_QD, 512).rearrange("p (a b) -> p a b", a=4), writes=[QDt.v()])
        dma("sp", KDTt.v().ap, csl(C_KDT, 512), writes=[KDTt.v()])
        dma("sp", tril.v().ap, csl(C_TRIL, 128), writes=[tril.v()])
        dma("sp", smalls.v().ap, smalls_in.rearrange("l p c -> p l c"), writes=[smalls.v()])
        dma("pool", wabd.v().ap, wabd_in.rearrange("l p (a b c) -> p l a b c", a=2, b=4), writes=[wabd.v()])
        for l in range(NL):
            dma("pool", browd_d[l], brow_in[l], writes=[BROWD[l:l + 1, :, :]])
        CH = 512
        for r0 in range(0, NWROWS, CH):
            pending_casts.append((r0, min(NWROWS, r0 + CH)))

        def issue_cast():
            if pending_casts:
                r0, r1 = pending_casts.pop(0)
                cast_upto[0] = r1
                dst = WS[r0:r1, :]
                dma("pool", dst.ap, wpack[r0:r1, :], writes=[dst])

        for _ in range(DMA_RING - 2):
            issue_cast()
        mset("dve", ones_row.v(), 1.0)
        mset("dve", st.v(), 0.0)
        mset("dve", stbf.v(), 0.0)
        mset("dve", hstate.v(), 0.0)
        mset("dve", lxtail.v(), 0.0)
        for l in range(NL):
            lam = smalls[:, l, SM_LAM:SM_LAM + 4]
            act(lrc[:, l, :, 0], lam, AF.Exp, scale=-1.0)
            act(lrc[:, l, :, 0], lrc[:, l, :, 0], AF.Ln, bias=1.0)
            ts("dve", lrc[:, l, :, 1], lrc[:, l, :, 0], -8.0, None, ALU.mult)
            ts("dve", lrc[:, l, :, 0], lrc[:, l, :, 0], -4.0, None, ALU.mult)
            ts("dve", lrc[:, l, :, 2], smalls[:, l, SM_BA:SM_BA + 4], 0.5, None, ALU.mult)
            ts("dve", lrc[:, l, :, 3], smalls[:, l, SM_BX:SM_BX + 4], 0.5, None, ALU.mult)
            wtmp = sbuf(0, [4, 128], F32)
            wtb = sbuf(2048, [4, 128], BF16)
            dma("sp", wtmp.v().ap, sgws_in[l].rearrange("p (a b) -> p a b", a=4), writes=[wtmp.v()])
            for g in range(4):
                tt("dve", wtb[:, g, :], wtmp[:, g, :], tril.v(), ALU.mult)
            for g in range(4):
                tr(PT[0][:, g * 128:(g + 1) * 128], wtb[:, g, :])
            cp("dve", wsT[:, l, :, :], Buf(PT[0].ap[:, 0:512].rearrange("p (a b) -> p a b", a=4), PT[0].key, [128, 4, 128], whole=True).v())

        def ln_tile(xbuf, gtab, btab):
            for blk in range(4):
                for c in range(2):
                    sc = xbuf[:, blk, c * 512:(c + 1) * 512]
                    S.op("dve", lambda h, c=c, sc=sc, blk=blk: h.bn_stats(out=i_st[:, blk, c, :].ap, in_=sc.ap),
                         reads=[sc], writes=[i_st[:, blk, c, :]])
            for blk in range(4):
                S.op("dve", lambda h, blk=blk: h.bn_aggr(out=i_mv[:, blk, :].ap, in_=i_st[:, blk, :, :].ap),
                     reads=[i_st[:, blk, :, :]], writes=[i_mv[:, blk, :]])
            ts("dve", i_rs.v(), i_mv[:, :, 1], LN_EPS, None, ALU.add)
            act(i_rs.v(), i_rs.v(), AF.Sqrt)
            S.op("dve", lambda h: h.reciprocal(out=i_rs.v().ap, in_=i_rs.v().ap), reads=[i_rs.v()], writes=[i_rs.v()])
            for blk in range(4):
                src = xbuf[:, blk, :]
                stt("dve", src, src, i_mv[:, blk, 0:1], gtab, ALU.subtract, ALU.mult)
                stt("dve", src, src, i_rs[:, blk:blk + 1], btab, ALU.mult, ALU.add)

        def ln_blk(xbuf, blk, gtab, btab):
            for c in range(2):
                sc = xbuf[:, blk, c * 512:(c + 1) * 512]
                S.op("dve", lambda h, c=c, sc=sc: h.bn_stats(out=i_st[:, blk, c, :].ap, in_=sc.ap),
                     reads=[sc], writes=[i_st[:, blk, c, :]])
            S.op("dve", lambda h: h.bn_aggr(out=i_mv[:, blk, :].ap, in_=i_st[:, blk, :, :].ap),
                 reads=[i_st[:, blk, :, :]], writes=[i_mv[:, blk, :]])
            rsv = i_rs[:, blk:blk + 1]
            ts("dve", rsv, i_mv[:, blk, 1:2], LN_EPS, None, ALU.add)
            act(rsv, rsv, AF.Sqrt)
            S.op("dve", lambda h: h.reciprocal(out=rsv.ap, in_=rsv.ap), reads=[rsv], writes=[rsv])
            src = xbuf[:, blk, :]
            stt("dve", src, src, i_mv[:, blk, 0:1], gtab, ALU.subtract, ALU.mult)
            stt("dve", src, src, rsv, btab, ALU.mult, ALU.add)

        def transpose_blk(src_f32, blk, dst):
            cp("pool", xb[:, blk, :], src_f32[:, blk, :])
            hf = trhalf()
            for dc in range(8):
                tr(hf[:, dc * 128:(dc + 1) * 128], xb[:, blk, dc * 128:(dc + 1) * 128])
            cp("act" if blk % 2 == 0 else "dve", dst[:, :, blk * 128:(blk + 1) * 128],
               Buf(hf.ap.rearrange("p (a b) -> p a b", a=8), hf.key, [128, 8, 128], whole=True).v())

        def transpose_tile(src_f32, dst):
            for blk in range(4):
                cp("pool", xb[:, blk, :], src_f32[:, blk, :])
                hf = trhalf()
                for dc in range(8):
                    tr(hf[:, dc * 128:(dc + 1) * 128], xb[:, blk, dc * 128:(dc + 1) * 128])
                cp("act" if blk % 2 == 0 else "dve", dst[:, :, blk * 128:(blk + 1) * 128],
                   Buf(hf.ap.rearrange("p (a b) -> p a b", a=8), hf.key, [128, 8, 128], whole=True).v())

        def tile_layer(t, l, last):
            xt_, x1_ = (XA, XB) if t % 2 == 0 else (XB, XA)
            bc = lambda j: smalls[:, l, SM_BC + j:SM_BC + j + 1]
            if l == 0:
                transpose_tile(xt_, xT)
            def tm_unit(g, bankf):
                w = wload(l, U_TM + g)
                br = bias_row(l, g * 512)
                for blk in range(4):
                    pb = bankf()
                    for kc in range(8):
                        mm(pb.v(), xT[:, kc, blk * 128:(blk + 1) * 128], w[:, kc * 512:(kc + 1) * 512], kc == 0, False)
                    mm(pb.v(), ones_row.v(), br.v(), False, True)
                    if g == 0:
                        cp("act", rv[:, blk, :], pb.v())
                    elif g == 1:
                        act(rgs[:, blk, :], pb.v(), AF.Silu)
                    elif g == 2:
                        cp("act", sv[:, blk, :], pb.v())
                    elif g == 3:
                        act(su[:, blk, :], pb.v(), AF.Gelu_apprx_tanh)
                    else:
                        act(sgv[:, blk, :], pb.v(), AF.Gelu_apprx_tanh)
                if g == 2:
                    r0 = l * NBT + t * 4
                    dst = VH[r0:r0 + 4, :, :]
                    dma("pool", vh_d[r0:r0 + 4].rearrange("b p c -> p b c"), sv.v().ap, reads=[sv.v()], writes=[dst])

            def fm_unit(k, bankf):
                w = wload(l, U_FM + 4 + k)
                for c4 in range(4):
                    pb = bankf()
                    for kc in range(8):
                        mm(pb.v(), w[:, kc * 512 + c4 * 128: kc * 512 + (c4 + 1) * 128], xT[:, kc, :], kc == 0, kc == 7)
                    if k == 0:
                        act(lx_ext[:, c4, 3:515], pb.v(), AF.Identity, bias=bc(16 + c4))
                    elif k == 1:
                        act(gg[:, c4, :], pb.v(), AF.Gelu_apprx_tanh, bias=bc(20 + c4))
                    elif k == 2:
                        ts("dve", sq[:, c4, :], pb.v(), bc(24 + c4), 128.0 ** -0.5, ALU.add, ALU.mult)
                    else:
                        act(sk[:, c4, :], pb.v(), AF.Identity, bias=bc(28 + c4))
                if k == 3:
                    for h in range(4):
                        r = (l * 4 + h) * NT + t
                        dma("pool", kh_d[r], sk[:, h, :].ap, reads=[sk[:, h, :]], writes=[KH[r:r + 1, :, :]])

            tm_unit(0, nbank)
            tm_unit(1, nbank)
            for i in range(4):
                dma("pool", rot_t[i].v().ap, rot_in[i, :, t * T:(t + 1) * T], writes=[rot_t[i].v()])
            for qk in range(2):
                w0 = wload(l, U_FM + 2 * qk)
                w1 = wload(l, U_FM + 2 * qk + 1)
                dstb = rq if qk == 0 else rk
                for h in range(4):
                    pa = nbank()
                    pbk = nbank()
                    for kc in range(8):
                        mm(pa.v(), w0[:, kc * 512 + h * 128: kc * 512 + (h + 1) * 128], xT[:, kc, :], kc == 0, kc == 7)
                    for kc in range(8):
                        mm(pbk.v(), w1[:, kc * 512 + h * 128: kc * 512 + (h + 1) * 128], xT[:, kc, :], kc == 0, kc == 7)
                    j0 = (2 * qk) * 4 + h
                    j1 = (2 * qk + 1) * 4 + h
                    ta = rtmp[(h % 2) * 2]
                    tb = rtmp[(h % 2) * 2 + 1]
                    stt("dve", ta.v(), pa.v(), bc(j0), rot_t[2 * qk].v(), ALU.add, ALU.mult)
                    stt("dve", tb.v(), pbk.v(), bc(j1), rot_t[2 * qk + 1].v(), ALU.add, ALU.mult)
                    tt("pool", dstb[:, h, :], ta.v(), tb.v(), ALU.add)
            deferred = [lambda: fm_unit(0, nbank3), lambda: fm_unit(1, nbank3), lambda: tm_unit(2, nbank3),
                        lambda: fm_unit(3, nbank3), lambda: fm_unit(2, nbank3), lambda: tm_unit(3, nbank3),
                        lambda: tm_unit(4, nbank3)]

            for c in range(4):
                cs = slice(c * 128, (c + 1) * 128)
                tt("pool", qd[:, :, cs], rq[:, :, cs], QDt.v(), ALU.mult)
                tt("pool", vdec[:, c, :], rv[:, c, :], KDTt.v(), ALU.mult)
            for c in range(4):
                cs = slice(c * 128, (c + 1) * 128)
                hf = trhalf()
                for h in range(4):
                    tr(hf[:, h * 128:(h + 1) * 128], rk[:, h, cs])
                cp("dve", rkt[:, c, :], hf[:, 0:512])
                sb_ = banks[3]
                for h in range(4):
                    mm(sb_[:, h * 128:(h + 1) * 128], rk[:, h, cs], rq[:, h, cs], True, True, sig=(h == 3))
                sdc = sd_[c % 2]
                tt("dve", sdc.v(), Buf(sb_.ap.rearrange("p (a b) -> p a b", a=4), sb_.key, [128, 4, 128], whole=True).v(),
                   Buf(DTt.ap.rearrange("p (a b) -> p a b", a=4), "AR", [128, 4, 128], base=DTt.base, esz=4).v(), ALU.mult)
                yb_ = banks[4]
                for h in range(4):
                    hs = slice(h * 128, (h + 1) * 128)
                    mm(yb_[:, hs], sdc[:, h, :], rv[:, c, hs], True, False, sig=False)
                    mm(yb_[:, hs], qd[:, h, cs], stbf[:, l, h, :], False, True, sig=(h == 3))
                kvb = banks[5]
                for h in range(4):
                    hs = slice(h * 128, (h + 1) * 128)
                    mm(kvb[:, hs], rkt[:, c, hs], vdec[:, c, hs], True, True, sig=(h == 3))
                for h in range(4):
                    hs = slice(h * 128, (h + 1) * 128)
                    cdk = float(np.exp(np.float64(128.0) * np.log1p(-(2.0 ** (-5.0 - h)))))
                    stt("dve", st[:, l, h, :], st[:, l, h, :], cdk, kvb[:, hs], ALU.mult, ALU.add)
                cp("act", stbf[:, l, :, :], st[:, l, :, :])
                for h in range(4):
                    hs = slice(h * 128, (h + 1) * 128)
                    S.op("dve", lambda hh, h=h, hs=hs: hh.bn_stats(out=stats[:, h, :].ap, in_=yb_[:, hs].ap),
                         reads=[yb_[:, hs]], writes=[stats[:, h, :]])
                    S.op("dve", lambda hh, h=h: hh.bn_aggr(out=mv[:, h, :].ap, in_=stats[:, h, :].ap),
                         reads=[stats[:, h, :]], writes=[mv[:, h, :]])
                ts("dve", rs4.v(), mv[:, :, 1], LN_EPS, None, ALU.add)
                act(rs4.v(), rs4.v(), AF.Sqrt)
                S.op("dve", lambda hh: hh.reciprocal(out=rs4.v().ap, in_=rs4.v().ap), reads=[rs4.v()], writes=[rs4.v()])
                for h in range(4):
                    hs = slice(h * 128, (h + 1) * 128)
                    ts("dve", yn[:, hs], yb_[:, hs], mv[:, h, 0:1], rs4[:, h:h + 1], ALU.subtract, ALU.mult)
                tt("pool", ya.v(), yn.v(), rgs[:, c, :], ALU.mult)
                hf = trhalf()
                for wc in range(4):
                    tr(hf[:, wc * 128:(wc + 1) * 128], ya[:, wc * 128:(wc + 1) * 128])
                cp("act", ybT[0][:, :, cs], Buf(hf.ap[:, 0:512].rearrange("p (a b) -> p a b", a=4), hf.key, [128, 4, 128], whole=True).v())
                for _ in range(2):
                    if deferred:
                        deferred.pop(0)()

            for cc in range(4):
                cp("pool", lx_ext[:, cc, 0:3], lxtail[:, l, cc, :])
            for cc in range(4):
                cw = lambda j: smalls[:, l, SM_CW + cc * 4 + j: SM_CW + cc * 4 + j + 1]
                ts("dve", e_xc.v(), lx_ext[:, cc, 0:512], cw(0), smalls[:, l, SM_CB + cc:SM_CB + cc + 1], ALU.mult, ALU.add)
                for j in range(1, 4):
                    stt("dve", e_xc.v(), lx_ext[:, cc, j:j + 512], cw(j), e_xc.v(), ALU.mult, ALU.add)
                cp("pool", lxtail[:, l, cc, :], lx_ext[:, cc, 512:515])
                cp("pool", e_xcb.v(), e_xc.v())
                pr = nbank()
                pi = nbank()
                mm(pr.v(), wabd[:, l, 0, cc, :], e_xcb.v(), True, True)
                mm(pi.v(), wabd[:, l, 1, cc, :], e_xcb.v(), True, True)
                act(e_tr.v(), pr.v(), AF.Tanh, bias=lrc[:, l, cc, 2:3], scale=0.5)
                act(e_ti.v(), pi.v(), AF.Tanh, bias=lrc[:, l, cc, 3:4], scale=0.5)
                act(e_a.v(), e_tr.v(), AF.Exp, bias=lrc[:, l, cc, 0:1], scale=lrc[:, l, cc, 0:1])
                act(e_s.v(), e_tr.v(), AF.Exp, bias=lrc[:, l, cc, 1:2], scale=lrc[:, l, cc, 1:2])
                act(e_s.v(), e_s.v(), AF.Sqrt, bias=0.25, scale=-0.25)
                tt("dve", e_u.v(), e_s.v(), e_xc.v(), ALU.mult)
                stt("dve", e_u.v(), e_ti.v(), 1.0, e_u.v(), ALU.add, ALU.mult)
                eh = e_h[cc % 2]
                S.op("dve", lambda hh, eh=eh, cc=cc: hh.tensor_tensor_scan(out=eh.v().ap, data0=e_a.v().ap, data1=e_u.v().ap,
                                                                          initial=hstate[:, l, cc:cc + 1].ap,
                                                                          op0=ALU.mult, op1=ALU.add),
                     reads=[e_a.v(), e_u.v(), hstate[:, l, cc:cc + 1]], writes=[eh.v()])
                cp("dve", hstate[:, l, cc:cc + 1], eh[:, 511:512])
                tt("pool", ybT[1][:, cc, :], eh.v(), gg[:, cc, :], ALU.mult)

            nkb = 4 * (t + 1)
            nch = (t + 4) // 4
            steps = []
            for h in range(4):
                kbi = 0
                for ch in range(nch - 1, -1, -1):
                    t0 = ch * 4
                    ntl = min(t + 1, t0 + 4) - t0
                    for kl in range(ntl * 4 - 1, -1, -1):
                        steps.append((h, ch, t0, ntl, kl, kbi, kl == ntl * 4 - 1))
                        kbi += 1

            def kv_bufs(h, ch):
                return f_k[(h * nch + ch) % 2], f_v[(h * nch + ch) % 2]

            def s1(i):
                h, ch, t0, ntl, kl, kbi, first = steps[i]
                fk, fv = kv_bufs(h, ch)
                if first:
                    r = (l * 4 + h) * NT + t0
                    dma("sp", fk[:, 0:ntl * 512].ap.rearrange("p (a b) -> p a b", a=ntl),
                        kh_d[r:r + ntl].rearrange("a p c -> p a c"), reads=[KH[r:r + ntl, :, :]], writes=[fk[:, 0:ntl * 512]])
                    rb = l * NBT + t0 * 4
                    dma("sp", fv[:, 0:ntl * 4, :].ap, vh_d[rb:rb + ntl * 4, :, h * 128:(h + 1) * 128].rearrange("b p c -> p b c"),
                        reads=[VH[rb:rb + ntl * 4, :, :]], writes=[fv[:, 0:ntl * 4, :]])
                kb = t0 * 4 + kl
                diag = kb >= 4 * t
                a_ = kb - 4 * t
                par = i % 2
                kT = fk[:, kl * 128:(kl + 1) * 128]
                pa = banks[par]
                mm(pa.v(), kT, sq[:, h, :], True, not diag)
                if diag:
                    mm(pa.v(), identb.v(), negm[:, a_, :], False, True)
                act(f_e[par].v(), pa.v(), AF.Exp)
                act(f_lp[par].v(), f_e[par].v(), AF.Ln, bias=1.0)
                if kbi < nkb - 1:
                    if kbi == 0:
                        cp("dve", f_r.v(), f_lp[par].v())
                    else:
                        tt("dve", f_r.v(), f_r.v(), f_lp[par].v(), ALU.add)
                    cp("dve", f_rb[(i + 1) % 3].v(), f_r.v())

            def s2(i):
                h, ch, t0, ntl, kl, kbi, first = steps[i]
                fk, fv = kv_bufs(h, ch)
                kb = t0 * 4 + kl
                diag = kb >= 4 * t
                a_ = kb - 4 * t
                par = i % 2
                kT = fk[:, kl * 128:(kl + 1) * 128]
                pbk = banks[2 + par]
                mm(pbk.v(), kT, sq[:, h, :], True, False, sig=False)
                lastmm = "tri"
                if kbi > 0:
                    lastmm = "ones"
                if diag:
                    lastmm = "neg"
                mm(pbk.v(), trineg.v(), f_lp[par].v(), False, lastmm == "tri", sig=(lastmm == "tri"))
                if kbi > 0:
                    mm(pbk.v(), negones.v(), f_rb[i % 3].v(), False, lastmm == "ones", sig=(lastmm == "ones"))
                if diag:
                    mm(pbk.v(), identb.v(), negm[:, a_, :], False, True)
                act(f_w[par].v(), pbk.v(), AF.Exp)

            def s3(i):
                h, ch, t0, ntl, kl, kbi, first = steps[i]
                fk, fv = kv_bufs(h, ch)
                par = i % 2
                ob = banks[4]
                mm(ob.v(), fv[:, kl, :], f_w[par].v(), kbi == 0, kbi == nkb - 1)
                if kbi == nkb - 1:
                    cp("dve", ybT[2][:, h, :], ob.v())

            g_ops = []
            g_ops.append(lambda: dma("pool", g_tab.v().ap, rowtab_in[l, :, 0:1024].rearrange("p (a b) -> p a b", a=2), writes=[g_tab.v()]))

            def g_blk(blk):
                bs_ = slice(blk * 128, (blk + 1) * 128)
                src = sgv[:, blk, :]
                pbg = banks[5]

                def t1():
                    S.op("dve", lambda hh: hh.bn_stats(out=g_st.v().ap, in_=src.ap), reads=[src], writes=[g_st.v()])
                    S.op("dve", lambda hh: hh.bn_aggr(out=g_mv.v().ap, in_=g_st.v().ap), reads=[g_st.v()], writes=[g_mv.v()])
                    ts("dve", g_rs.v(), g_mv[:, 1:2], LN_EPS, None, ALU.add)

                def t2():
                    act(g_rs.v(), g_rs.v(), AF.Ln)
                    act(g_rs.v(), g_rs.v(), AF.Exp, scale=-0.5)

                def t3():
                    stt("dve", g_vn.v(), src, g_mv[:, 0:1], g_tab[:, 0, :], ALU.subtract, ALU.mult)
                    stt("dve", g_vl.v(), g_vn.v(), g_rs.v(), g_tab[:, 1, :], ALU.mult, ALU.add)

                def t4():
                    for g in range(4):
                        gs = slice(g * 128, (g + 1) * 128)
                        mm(pbg[:, gs], wsT[:, l, g, :], g_vl[:, gs], True, True, sig=(g == 3))
                    for g in range(4):
                        gs = slice(g * 128, (g + 1) * 128)
                        stt("dve", g_yd[:, gs], pbg[:, gs], smalls[:, l, SM_BS + g:SM_BS + g + 1], su[:, blk, gs], ALU.add, ALU.mult)

                def t5():
                    hf = trhalf()
                    for wc in range(4):
                        tr(hf[:, wc * 128:(wc + 1) * 128], g_yd[:, wc * 128:(wc + 1) * 128])
                    cp("dve", ybT[3][:, :, bs_], Buf(hf.ap[:, 0:512].rearrange("p (a b) -> p a b", a=4), hf.key, [128, 4, 128], whole=True).v())
                return [t1, t2, t3, t4, t5]

            for blk in range(4):
                g_ops.extend(g_blk(blk))

            s1(0)
            for i in range(len(steps)):
                if i + 1 < len(steps):
                    s1(i + 1)
                s2(i)
                if i >= 1:
                    s3(i - 1)
                if g_ops:
                    g_ops.pop(0)()
            s3(len(steps) - 1)
            while g_ops:
                g_ops.pop(0)()

            for dc in range(8):
                wg = wload(l, U_GT + dc)
                if dc % 2 == 0:
                    wbr = wload(l, U_BR + dc // 2)
                dcl = dc % 2
                for b in range(4):
                    pp = nbank()
                    pg = nbank()
                    for wc in range(4):
                        o_ = dcl * 2048 + (b * 4 + wc) * 128
                        mm(pp.v(), wbr[:, o_:o_ + 128], ybT[b][:, wc, :], wc == 0, wc == 3)
                    for kc in range(8):
                        mm(pg.v(), wg[:, kc * 512 + b * 128: kc * 512 + (b + 1) * 128], xT[:, kc, :], kc == 0, kc == 7)
                    gb = h_g[b % 2]
                    act(gb.v(), pg.v(), AF.Sigmoid, bias=bc(32 + dc * 4 + b))
                    if b == 0:
                        tt("dve", h_acc.v(), gb.v(), pp.v(), ALU.mult)
                    else:
                        tmp = h_tmp[b % 2]
                        tt("dve", tmp.v(), gb.v(), pp.v(), ALU.mult)
                        if b < 3:
                            tt("pool", h_acc.v(), h_acc.v(), tmp.v(), ALU.add)
                        else:
                            tt("pool", mergedT[:, dc, :], h_acc.v(), tmp.v(), ALU.add)

            dma("pool", i_tab.v().ap, rowtab_in[l, :, 1024:3072].rearrange("p (a b) -> p a b", a=2), writes=[i_tab.v()])
            wos = [wload(l, U_WO), wload(l, U_WO + 1)]
            for blk in range(4):
                for half in range(2):
                    bk = banks[(blk % 2) * 2 + half]
                    w = wos[half]
                    hsl = slice(half * 512, (half + 1) * 512)
                    for dc in range(8):
                        mm(bk.v(), mergedT[:, dc, blk * 128:(blk + 1) * 128], w[:, dc * 512:(dc + 1) * 512], dc == 0, dc == 7)
                    stt("dve", x1_[:, blk, hsl], xt_[:, blk, hsl], ALPHA, bk.v(), ALU.mult, ALU.add)
                ln_blk(x1_, blk, i_tab[:, 0, :], i_tab[:, 1, :])
                transpose_blk(x1_, blk, xT)

            dma("pool", i_tab.v().ap, rowtab_in[l, :, 3072:5120].rearrange("p (a b) -> p a b", a=2), writes=[i_tab.v()])
            for u in range(8):
                w = wload(l, U_W1 + u)
                for f in range(4):
                    fc = u * 4 + f
                    pb = nbank()
                    for kc in range(8):
                        mm(pb.v(), w[:, kc * 512 + f * 128: kc * 512 + (f + 1) * 128], xT[:, kc, :], kc == 0, kc == 7)
                    jr = j_r[fc % 2]
                    act(jr.v(), pb.v(), AF.Relu, bias=smalls[:, l, SM_B1 + fc:SM_B1 + fc + 1])
                    tt("pool", hT[:, fc, :], jr.v(), jr.v(), ALU.mult)
            for half in range(2):
                for u4 in range(4):
                    w = wload(l, U_W2 + half * 4 + u4)
                    for j in range(8):
                        fc = u4 * 8 + j
                        for blk in range(4):
                            mm(banks[blk].v(), hT[:, fc, blk * 128:(blk + 1) * 128], w[:, j * 512:(j + 1) * 512], fc == 0, False, sig=False)
                br2 = bias_row(l, 2560 + half * 512)
                for blk in range(4):
                    mm(banks[blk].v(), ones_row.v(), br2.v(), False, True)
                for blk in range(4):
                    hsl = slice(half * 512, (half + 1) * 512)
                    stt("dve", xt_[:, blk, hsl], x1_[:, blk, hsl], ALPHA, banks[blk].v(), ALU.mult, ALU.add)
            if last:
                if t + 1 < NT:
                    dma("pool", x1_.v().ap, x_in[(t + 1) * T:(t + 2) * T, :].rearrange("(b p) c -> p b c", p=128), writes=[x1_.v()])
                ln_tile(xt_, i_tab[:, 0, :], i_tab[:, 1, :])
                dma("sp", y_out[t * T:(t + 1) * T, :].rearrange("(b p) c -> p b c", p=128), xt_.v().ap, reads=[xt_.v()])
            else:
                for blk in range(4):
                    ln_blk(xt_, blk, i_tab[:, 0, :], i_tab[:, 1, :])
                    transpose_blk(xt_, blk, xT)

        dma("sp", XA.v().ap, x_in[0:T, :].rearrange("(b p) c -> p b c", p=128), writes=[XA.v()])
        for t in range(NT):
            for l in range(NL):
                tile_layer(t, l, l == NL - 1)

        S.emit({"pe": block.tensor, "act": block.scalar, "dve": block.vector, "pool": block.gpsimd, "sp": block.sync},
               csem, dsem)
    return nc, S


def _unit_cols(Wc):
    return np.ascontiguousarray(Wc.reshape(8, 128, 512).transpose(1, 0, 2)).reshape(128, 4096)


def _swap_half(a):
    s = a.reshape(a.shape[:-1] + (4, 2, 64))
    return np.ascontiguousarray(s[..., ::-1, :]).reshape(a.shape)


def pack_layer(inp, l):
    w_in = inp["w_in"][l]
    b_in = inp["b_in"][l]
    sec = lambda i, n=512: slice(i, i + n)
    RQ, RK, RV, RG, LX, LG, SQ, SK, SV, SU, SGV, GT = 0, 512, 1024, 1536, 2048, 2560, 3072, 3584, 4096, 4608, 5120, 5632
    units = []
    for c0 in (RV, RG, SV, SU, SGV):
        units.append(_unit_cols(w_in[:, sec(c0)]))
    units.append(_unit_cols(w_in[:, sec(RQ)]))
    units.append(_unit_cols(_swap_half(w_in[:, sec(RQ)])))
    units.append(_unit_cols(w_in[:, sec(RK)]))
    units.append(_unit_cols(_swap_half(w_in[:, sec(RK)])))
    for c0 in (LX, LG, SQ, SK):
        units.append(_unit_cols(w_in[:, sec(c0)]))
    for dc in range(8):
        cols = np.concatenate([w_in[:, GT + b * 1024 + dc * 128: GT + b * 1024 + (dc + 1) * 128] for b in range(4)], axis=1)
        units.append(_unit_cols(cols))
    wb = inp["w_branch"][l].reshape(4, 4, 128, 8, 128).transpose(2, 3, 0, 1, 4).reshape(128, 8, 16, 128)
    for j in range(4):
        units.append(np.ascontiguousarray(wb[:, 2 * j:2 * j + 2]).reshape(128, 4096))
    for half in range(2):
        units.append(_unit_cols(inp["w_out"][l][:, half * 512:(half + 1) * 512]))
    for u in range(8):
        units.append(_unit_cols(inp["w1"][l][:, u * 512:(u + 1) * 512]))
    w2 = inp["w2"][l]
    for half in range(2):
        wh = w2[:, half * 512:(half + 1) * 512].reshape(32, 128, 512)
        for u4 in range(4):
            units.append(np.ascontiguousarray(wh[8 * u4:8 * u4 + 8].transpose(1, 0, 2)).reshape(128, 4096))
    assert len(units) == NUNIT
    wp = np.stack(units, 0).astype(np.float32)
    sm = np.zeros((128, NSM), np.float32)
    colv = lambda v: np.ascontiguousarray(v.reshape(-1, 128).T)
    fm_b = [b_in[sec(RQ)], _swap_half(b_in[sec(RQ)]), b_in[sec(RK)], _swap_half(b_in[sec(RK)]),
            b_in[sec(LX)], b_in[sec(LG)], b_in[sec(SQ)], b_in[sec(SK)]]
    sm[:, SM_BC:SM_BC + 32] = np.concatenate([colv(v) for v in fm_b], axis=1)
    gcols = []
    for dc in range(8):
        for b in range(4):
            gcols.append(b_in[GT + b * 1024 + dc * 128: GT + b * 1024 + (dc + 1) * 128][:, None])
    sm[:, SM_BC + 32:SM_BC + 64] = np.concatenate(gcols, axis=1)
    cw = inp["conv_w"][l]
    sm[:, SM_CW:SM_CW + 16] = cw.reshape(4, 4, 128).transpose(2, 1, 0).reshape(128, 16)
    sm[:, SM_CB:SM_CB + 4] = colv(inp["conv_b"][l])
    sm[:, SM_BA:SM_BA + 4] = colv(inp["lru_ba"][l])
    sm[:, SM_BX:SM_BX + 4] = colv(inp["lru_bx"][l])
    sm[:, SM_LAM:SM_LAM + 4] = colv(inp["lru_lambda"][l])
    sm[:, SM_BS:SM_BS + 4] = inp["sg_bs"][l].T
    sm[:, SM_B1:SM_B1 + 32] = colv(inp["b1"][l])
    rep = lambda v: np.broadcast_to(v[None, :], (128, v.shape[0]))
    rowtab = np.concatenate([rep(inp["sg_ln_g"][l]), rep(inp["sg_ln_b"][l]), rep(inp["ln1_g"][l]), rep(inp["ln1_b"][l]),
                             rep(inp["ln2_g"][l]), rep(inp["ln2_b"][l])], axis=1).astype(np.float32)
    wabd = np.zeros((128, 2, 4, 128), np.float32)
    for k, nm in enumerate(("lru_wa", "lru_wx")):
        wm = inp[nm][l]
        for cc in range(4):
            wabd[0:64, k, cc, 0:64] = wm[2 * cc]
            wabd[64:128, k, cc, 64:128] = wm[2 * cc + 1]
    sgws = np.ascontiguousarray(inp["sg_ws"][l].transpose(1, 0, 2)).reshape(128, 512)
    brow = np.concatenate([b_in[sec(RV)], b_in[sec(RG)], b_in[sec(SV)], b_in[sec(SU)], b_in[sec(SGV)], inp["b2"][l]])[None, :]
    return wp, sm, rowtab, wabd.reshape(128, 1024), sgws, brow.astype(np.float32)


def make_consts(S_LEN):
    c = np.zeros((128, NCONST), np.float32)
    idx = np.arange(128)
    c[:, C_ID:C_ID + 128] = np.eye(128)
    c[:, C_TRI:C_TRI + 128] = -(idx[:, None] >= idx[None, :]).astype(np.float32)
    c[:, C_NONE:C_NONE + 128] = -1.0
    for a in range(4):
        key = a * 128 + idx[:, None]
        qry = np.arange(512)[None, :]
        c[:, C_NEGM + a * 512:C_NEGM + (a + 1) * 512] = np.where(key >= qry, NEG, 0.0)
    log_g = np.log1p(-(2.0 ** (-5.0 - np.arange(4, dtype=np.float64))))
    for h in range(4):
        diff = idx[None, :] - idx[:, None]
        c[:, C_DT + h * 128:C_DT + (h + 1) * 128] = np.where(diff >= 0, np.exp(log_g[h] * np.maximum(diff, 0)), 0.0)
        c[:, C_QD + h * 128:C_QD + (h + 1) * 128] = np.exp(log_g[h] * (idx + 1.0))[None, :]
        c[:, C_KDT + h * 128:C_KDT + (h + 1) * 128] = np.exp(log_g[h] * (127.0 - idx))[:, None]
    c[:, C_TRIL:C_TRIL + 128] = (idx[None, :] <= idx[:, None]).astype(np.float32)
    inv_freq = (np.float32(10000.0) ** (-np.arange(64, dtype=np.float32) / np.float32(64))).astype(np.float32)
    pos = np.arange(S_LEN, dtype=np.float32)
    ang = (pos[None, :] * inv_freq[:, None]).astype(np.float32).astype(np.float64)
    cos = np.cos(ang)
    sin = np.sin(ang)
    C = np.concatenate([cos, cos], 0)
    Sg = np.concatenate([-sin, sin], 0)
    ks = 128.0 ** -0.5
    rot = np.stack([C, Sg, C * ks, Sg * ks], 0).astype(np.float32)
    return c, rot


_CACHE = {}


def run_model(inputs, S_LEN, NL, n_cores, batch_of_core):
    key = (S_LEN, NL)
    if key not in _CACHE:
        _CACHE[key] = build_program(S_LEN, NL)
    nc, _ = _CACHE[key]
    inp = {k: np.asarray(v) for k, v in inputs.items()}
    packs = [pack_layer(inp, l) for l in range(NL)]
    wpack = np.concatenate([p[0].reshape(NUNIT * 256, 2048) for p in packs], 0)
    smalls = np.stack([p[1] for p in packs], 0)
    rowtab = np.stack([p[2] for p in packs], 0)
    wabd = np.stack([p[3] for p in packs], 0)
    sgws = np.stack([p[4] for p in packs], 0)
    brow = np.stack([p[5] for p in packs], 0)
    consts, rot = make_consts(S_LEN)
    shared = {"wpack": wpack, "smalls": smalls, "rowtab": rowtab, "wabd": wabd, "sgws": sgws,
              "consts": consts, "rot": rot, "brow": brow}
    in_maps = []
    for c in range(n_cores):
        m = dict(shared)
        m["x"] = np.ascontiguousarray(inp["x"][batch_of_core[c]], dtype=np.float32)
        in_maps.append(m)
    res = run_bass_kernel_spmd(nc, in_maps, core_ids=list(range(n_cores)))
    return [r["y"] for r in res.results]


def kernel(**inputs):
    x = np.asarray(inputs["x"])
    B, S_LEN, _ = x.shape
    boc = [c % B for c in range(8)]
    outs = run_model(inputs, S_LEN, DEPTH, 8, boc)
    return np.stack([outs[b] for b in range(B)], 0).astype(np.float32)
```
